# Optimizing a Trainium2 kernel written in Bass

```python
import math
import jax
import jax.numpy as jnp
from jax import lax
import numpy as np

D_MODEL = 1024
BATCH = 16
SEQ = 2048
DEPTH = 4

MIX_WIDTH = D_MODEL
N_MIXERS = 4
GROUP_WIDTH = MIX_WIDTH // N_MIXERS
HEAD_DIM = 64
CONV_WIDTH = 4
CHUNK = 128

LRU_BLOCKS = GROUP_WIDTH // HEAD_DIM
LRU_C = 8.0

RET_HEADS = GROUP_WIDTH // HEAD_DIM
ROPE_BASE = 10000.0
GN_EPS = 1e-5

RWKV_HEADS = GROUP_WIDTH // HEAD_DIM
RWKV_DECAY_LORA = 64
RWKV_ICLR_LORA = 64
RWKV_GATE_LORA = 128
RWKV_GN_EPS = HEAD_DIM * 1e-5

SSD_HEADS = GROUP_WIDTH // HEAD_DIM
SSD_GROUPS = 2
SSD_STATE = 128
SSD_XBC = GROUP_WIDTH + 2 * SSD_GROUPS * SSD_STATE

MEM_TOKENS = 256
MEM_HEADS = 4
MEM_HEAD_DIM = D_MODEL // MEM_HEADS

FFN_HIDDEN = ((8 * D_MODEL + 3 * 256 - 1) // (3 * 256)) * 256
NORM_EPS = 1e-6

A_COLS = 2 * GROUP_WIDTH
B_COLS = 4 * GROUP_WIDTH
C_COLS = 3 * GROUP_WIDTH + RWKV_DECAY_LORA + RWKV_ICLR_LORA + RWKV_GATE_LORA
D_COLS = GROUP_WIDTH + SSD_XBC + SSD_HEADS
IN_COLS = A_COLS + B_COLS + C_COLS + D_COLS

kernel_name = 'hybrid_parallel_heads_rglru_retnet_rwkv7_ssd'


def rms_norm(x, g):
    xf = x.astype(jnp.float32)
    y = xf * lax.rsqrt(jnp.mean(xf * xf, axis=-1, keepdims=True) + NORM_EPS)
    return (y * g.astype(jnp.float32)).astype(x.dtype)


def head_group_norm(y, w, b, eps):
    bsz, s, h, n = y.shape
    yf = y.astype(jnp.float32)
    mu = jnp.mean(yf, axis=-1, keepdims=True)
    var = jnp.mean(jnp.square(yf - mu), axis=-1, keepdims=True)
    yn = ((yf - mu) * lax.rsqrt(var + eps)).reshape(bsz, s, h * n)
    return yn * w.astype(jnp.float32) + b.astype(jnp.float32)


def causal_conv(x, w, b):
    k, c = w.shape
    y = lax.conv_general_dilated(
        x, w.astype(x.dtype)[:, None, :], window_strides=(1,), padding=[(k - 1, 0)],
        dimension_numbers=('NWC', 'WIO', 'NWC'), feature_group_count=c)
    return y + b.astype(x.dtype)


def linear_scan(a, u):
    def combine(c1, c2):
        a1, b1 = c1
        a2, b2 = c2
        return a1 * a2, a2 * b1 + b2
    _, h = lax.associative_scan(combine, (a, u), axis=1)
    return h


def chunked_decay_attention(q, k, v, log_a):
    b, s, h, n = q.shape
    p = v.shape[-1]
    c = s // CHUNK
    q = q.reshape(b, c, CHUNK, h, n)
    k = k.reshape(b, c, CHUNK, h, n)
    v = v.reshape(b, c, CHUNK, h, p)
    cum = jnp.cumsum(log_a.astype(jnp.float32).reshape(b, c, CHUNK, h), axis=2)
    causal = jnp.tril(jnp.ones((CHUNK, CHUNK), dtype=bool))
    seg = cum[:, :, :, None, :] - cum[:, :, None, :, :]
    decay = jnp.exp(jnp.where(causal[None, None, :, :, None], seg, -jnp.inf))
    scores = jnp.einsum('bcthn,bcshn->bctsh', q, k) * decay
    y_intra = jnp.einsum('bctsh,bcshp->bcthp', scores, v)
    total = cum[:, :, -1]
    w_end = jnp.exp(total[:, :, None, :] - cum)
    chunk_kv = jnp.einsum('bclhn,bclh,bclhp->bchnp', k, w_end, v)

    def step(state, inp):
        kv_c, tot_c = inp
        return state * jnp.exp(tot_c)[..., None, None] + kv_c, state

    init = jnp.zeros((b, h, n, p), jnp.float32)
    _, states_in = lax.scan(step, init, (jnp.moveaxis(chunk_kv, 1, 0), jnp.moveaxis(total, 1, 0)))
    states_in = jnp.moveaxis(states_in, 0, 1)
    y_inter = jnp.einsum('bclhn,bchnp->bclhp', q * jnp.exp(cum)[..., None], states_in)
    return (y_intra + y_inter).reshape(b, s, h, p)


def rotary(x, cos, sin):
    half = x.shape[-1] // 2
    x1, x2 = x[..., :half], x[..., half:]
    c = cos[None, :, None, :]
    s = sin[None, :, None, :]
    return jnp.concatenate([x1 * c - x2 * s, x1 * s + x2 * c], axis=-1)


def rglru_group(p, conv_w, conv_b, w_r, b_r, w_i, b_i, lam):
    b, s, _ = p.shape
    gate, xr = jnp.split(p, 2, axis=-1)
    xr = causal_conv(xr, conv_w, conv_b)
    xb = xr.reshape(b, s, LRU_BLOCKS, HEAD_DIM)
    r = jax.nn.sigmoid(jnp.einsum('bshi,hij->bshj', xb, w_r) + b_r).reshape(b, s, GROUP_WIDTH)
    i = jax.nn.sigmoid(jnp.einsum('bshi,hij->bshj', xb, w_i) + b_i).reshape(b, s, GROUP_WIDTH)
    log_a = (-LRU_C * r.astype(jnp.float32)) * jax.nn.softplus(-lam.astype(jnp.float32))
    a = jnp.exp(log_a)
    u = jnp.sqrt(-jnp.expm1(2.0 * log_a)) * (i * xr).astype(jnp.float32)
    h = linear_scan(a, u)
    return (h * jax.nn.gelu(gate.astype(jnp.float32), approximate=True)).astype(p.dtype)


def retention_group(p, gn_w, gn_b):
    b, s, _ = p.shape
    q, k, v, g = jnp.split(p, 4, axis=-1)
    heads = lambda t: t.reshape(b, s, RET_HEADS, HEAD_DIM)
    pos = jnp.arange(s, dtype=jnp.float32)
    inv_freq = ROPE_BASE ** (-jnp.arange(HEAD_DIM // 2, dtype=jnp.float32) / (HEAD_DIM // 2))
    ang = pos[:, None] * inv_freq[None, :]
    cos, sin = jnp.cos(ang).astype(p.dtype), jnp.sin(ang).astype(p.dtype)
    qh = rotary(heads(q), cos, sin)
    kh = rotary(heads(k), cos, sin) * (HEAD_DIM ** -0.5)
    log_gamma = jnp.log1p(-jnp.exp2(-5.0 - jnp.arange(RET_HEADS, dtype=jnp.float32)))
    y = chunked_decay_attention(qh, kh, heads(v), jnp.broadcast_to(log_gamma, (b, s, RET_HEADS)))
    y = head_group_norm(y, gn_w, gn_b, GN_EPS)
    return (jax.nn.silu(g.astype(jnp.float32)) * y).astype(p.dtype)


def rwkv7_scan(r, w, k, v, kk, a):
    b, s, h, n = r.shape
    seq_first = lambda t: jnp.moveaxis(t.astype(jnp.float32), 1, 0)

    def step(state, inp):
        r_t, w_t, k_t, v_t, kk_t, a_t = inp
        sa = jnp.einsum('bhvk,bhk->bhv', state, -kk_t)
        state = (state * w_t[:, :, None, :] + sa[..., None] * (kk_t * a_t)[:, :, None, :]
                 + v_t[..., None] * k_t[:, :, None, :])
        return state, jnp.einsum('bhvk,bhk->bhv', state, r_t)

    init = jnp.zeros((b, h, n, n), jnp.float32)
    _, y = lax.scan(step, init, tuple(seq_first(t) for t in (r, w, k, v, kk, a)))
    return jnp.moveaxis(y, 0, 1)


def rwkv7_group(p, mu, w0, w2, a0, a2, g2, k_k, k_a, r_k, gn_w, gn_b):
    b, s, _ = p.shape
    shifted = jnp.pad(p, ((0, 0), (1, 0), (0, 0)))[:, :-1]
    p = p + (shifted - p) * mu
    o1 = GROUP_WIDTH
    o3 = 3 * GROUP_WIDTH
    r, k, v, w_lo, a_lo, g_lo = jnp.split(
        p, [o1, 2 * o1, o3, o3 + RWKV_DECAY_LORA, o3 + RWKV_DECAY_LORA + RWKV_ICLR_LORA], axis=-1)
    w = -jax.nn.softplus(-(w0 + jnp.tanh(w_lo) @ w2).astype(jnp.float32)) - 0.5
    decay = jnp.exp(-jnp.exp(w))
    a = jax.nn.sigmoid((a0 + a_lo @ a2).astype(jnp.float32))
    g = jax.nn.sigmoid(g_lo) @ g2
    heads = lambda t: t.reshape(b, s, RWKV_HEADS, HEAD_DIM)
    kk = heads(k * k_k).astype(jnp.float32)
    kk = kk * lax.rsqrt(jnp.sum(kk * kk, axis=-1, keepdims=True) + 1e-12)
    k = k * (1.0 + (a - 1.0) * k_a)
    rh, kh, vh = heads(r), heads(k), heads(v)
    y = rwkv7_scan(rh, heads(decay), kh, vh, kk, heads(a))
    y = head_group_norm(y, gn_w, gn_b, RWKV_GN_EPS)
    bonus = jnp.sum(rh * kh * r_k.reshape(RWKV_HEADS, HEAD_DIM), axis=-1, keepdims=True) * vh
    return ((y + bonus.reshape(b, s, GROUP_WIDTH)) * g).astype(p.dtype)


def ssd_group(p, conv_w, conv_b, dt_bias, a_log, d_skip, norm_w):
    b, s, _ = p.shape
    z, xbc, dt = jnp.split(p, [GROUP_WIDTH, GROUP_WIDTH + SSD_XBC], axis=-1)
    xbc = jax.nn.silu(causal_conv(xbc, conv_w, conv_b))
    xs, bm, cm = jnp.split(xbc, [GROUP_WIDTH, GROUP_WIDTH + SSD_GROUPS * SSD_STATE], axis=-1)
    xs = xs.reshape(b, s, SSD_HEADS, HEAD_DIM)
    rep = SSD_HEADS // SSD_GROUPS
    bm = jnp.repeat(bm.reshape(b, s, SSD_GROUPS, SSD_STATE), rep, axis=2)
    cm = jnp.repeat(cm.reshape(b, s, SSD_GROUPS, SSD_STATE), rep, axis=2)
    dt = jax.nn.softplus(dt.astype(jnp.float32) + dt_bias.astype(jnp.float32))
    a = -jnp.exp(a_log.astype(jnp.float32))
    y = chunked_decay_attention(cm, bm, xs * dt[..., None], dt * a) + d_skip[:, None] * xs
    y = y.reshape(b, s, GROUP_WIDTH) * jax.nn.silu(z.astype(jnp.float32))
    yg = y.reshape(b, s, SSD_GROUPS, GROUP_WIDTH // SSD_GROUPS)
    yg = yg * lax.rsqrt(jnp.mean(yg * yg, axis=-1, keepdims=True) + NORM_EPS)
    return (yg.reshape(b, s, GROUP_WIDTH) * norm_w).astype(p.dtype)


def memory_cross_attention(h, m, wq, wk, wv, wo):
    b, s, _ = h.shape
    n_mem = m.shape[1]
    q = (h @ wq).reshape(b, s, MEM_HEADS, MEM_HEAD_DIM)
    k = (m @ wk).reshape(b, n_mem, MEM_HEADS, MEM_HEAD_DIM)
    v = (m @ wv).reshape(b, n_mem, MEM_HEADS, MEM_HEAD_DIM)
    scores = jnp.einsum('bshd,bmhd->bhsm', q, k).astype(jnp.float32) * (MEM_HEAD_DIM ** -0.5)
    probs = jax.nn.softmax(scores, axis=-1).astype(v.dtype)
    o = jnp.einsum('bhsm,bmhd->bshd', probs, v).reshape(b, s, D_MODEL)
    return o @ wo


def swiglu(h, w_in, w_out):
    gate, up = jnp.split(h @ w_in, 2, axis=-1)
    return (jax.nn.silu(gate) * up) @ w_out


def setup_inputs(seed: int = 0) -> dict:
    key = jax.random.key(seed)
    ks = iter(jax.random.split(key, 64))
    L, G = DEPTH, GROUP_WIDTH

    def normal(shape, scale):
        return jax.random.normal(next(ks), shape, jnp.float32) * scale

    def uniform(shape, lo, hi):
        return jax.random.uniform(next(ks), shape, jnp.float32, lo, hi)

    def gain(shape):
        return 1.0 + normal(shape, 0.02)

    a_target = uniform((L, G), 0.9, 0.999) ** (1.0 / LRU_C)
    lru_lambda = jnp.log(a_target) - jnp.log1p(-a_target)
    dt0 = jnp.exp(uniform((L, SSD_HEADS), math.log(1e-3), math.log(1e-1)))
    ssd_dt_bias = dt0 + jnp.log(-jnp.expm1(-dt0))

    return {
        'x': normal((BATCH, SEQ, D_MODEL), 1.0),
        'mem': normal((BATCH, MEM_TOKENS, D_MODEL), 1.0),
        'norm_mix': gain((L, D_MODEL)),
        'w_in': normal((L, D_MODEL, IN_COLS), D_MODEL ** -0.5),
        'lru_conv_w': normal((L, CONV_WIDTH, G), CONV_WIDTH ** -0.5),
        'lru_conv_b': normal((L, G), 0.02),
        'lru_w_r': normal((L, LRU_BLOCKS, HEAD_DIM, HEAD_DIM), HEAD_DIM ** -0.5),
        'lru_b_r': normal((L, LRU_BLOCKS, HEAD_DIM), 0.02),
        'lru_w_i': normal((L, LRU_BLOCKS, HEAD_DIM, HEAD_DIM), HEAD_DIM ** -0.5),
        'lru_b_i': normal((L, LRU_BLOCKS, HEAD_DIM), 0.02),
        'lru_lambda': lru_lambda,
        'ret_gn_w': gain((L, G)),
        'ret_gn_b': normal((L, G), 0.02),
        'rwkv_mu': uniform((L, C_COLS), 0.0, 1.0),
        'rwkv_w0': uniform((L, G), -6.0, 1.0),
        'rwkv_w2': normal((L, RWKV_DECAY_LORA, G), 0.1),
        'rwkv_a0': normal((L, G), 0.1),
        'rwkv_a2': normal((L, RWKV_ICLR_LORA, G), 0.1),
        'rwkv_g2': normal((L, RWKV_GATE_LORA, G), RWKV_GATE_LORA ** -0.5),
        'rwkv_k_k': 0.85 + normal((L, G), 0.02),
        'rwkv_k_a': gain((L, G)),
        'rwkv_r_k': normal((L, G), 0.1),
        'rwkv_gn_w': gain((L, G)),
        'rwkv_gn_b': normal((L, G), 0.02),
        'ssd_conv_w': normal((L, CONV_WIDTH, SSD_XBC), CONV_WIDTH ** -0.5),
        'ssd_conv_b': normal((L, SSD_XBC), 0.02),
        'ssd_dt_bias': ssd_dt_bias,
        'ssd_a_log': jnp.log(uniform((L, SSD_HEADS), 1.0, 16.0)),
        'ssd_d': gain((L, SSD_HEADS)),
        'ssd_norm_w': gain((L, G)),
        'w_out': normal((L, MIX_WIDTH, D_MODEL), MIX_WIDTH ** -0.5),
        'norm_mem_q': gain((L, D_MODEL)),
        'norm_mem_kv': gain((L, D_MODEL)),
        'mem_wq': normal((L, D_MODEL, D_MODEL), D_MODEL ** -0.5),
        'mem_wk': normal((L, D_MODEL, D_MODEL), D_MODEL ** -0.5),
        'mem_wv': normal((L, D_MODEL, D_MODEL), D_MODEL ** -0.5),
        'mem_wo': normal((L, D_MODEL, D_MODEL), D_MODEL ** -0.5),
        'norm_ffn': gain((L, D_MODEL)),
        'ffn_w_in': normal((L, D_MODEL, 2 * FFN_HIDDEN), D_MODEL ** -0.5),
        'ffn_w_out': normal((L, FFN_HIDDEN, D_MODEL), FFN_HIDDEN ** -0.5),
        'norm_final': gain((D_MODEL,)),
    }


def reference(x, mem, norm_mix, w_in, lru_conv_w, lru_conv_b, lru_w_r, lru_b_r, lru_w_i, lru_b_i,
              lru_lambda, ret_gn_w, ret_gn_b, rwkv_mu, rwkv_w0, rwkv_w2, rwkv_a0, rwkv_a2, rwkv_g2,
              rwkv_k_k, rwkv_k_a, rwkv_r_k, rwkv_gn_w, rwkv_gn_b, ssd_conv_w, ssd_conv_b,
              ssd_dt_bias, ssd_a_log, ssd_d, ssd_norm_w, w_out, norm_mem_q, norm_mem_kv,
              mem_wq, mem_wk, mem_wv, mem_wo, norm_ffn, ffn_w_in, ffn_w_out, norm_final):
    splits = [A_COLS, A_COLS + B_COLS, A_COLS + B_COLS + C_COLS]
    for l in range(DEPTH):
        h = rms_norm(x, norm_mix[l])
        p_a, p_b, p_c, p_d = jnp.split(h @ w_in[l], splits, axis=-1)
        y = jnp.concatenate([
            rglru_group(p_a, lru_conv_w[l], lru_conv_b[l], lru_w_r[l], lru_b_r[l],
                        lru_w_i[l], lru_b_i[l], lru_lambda[l]),
            retention_group(p_b, ret_gn_w[l], ret_gn_b[l]),
            rwkv7_group(p_c, rwkv_mu[l], rwkv_w0[l], rwkv_w2[l], rwkv_a0[l], rwkv_a2[l],
                        rwkv_g2[l], rwkv_k_k[l], rwkv_k_a[l], rwkv_r_k[l],
                        rwkv_gn_w[l], rwkv_gn_b[l]),
            ssd_group(p_d, ssd_conv_w[l], ssd_conv_b[l], ssd_dt_bias[l], ssd_a_log[l],
                      ssd_d[l], ssd_norm_w[l]),
        ], axis=-1)
        x = x + (y @ w_out[l]).astype(x.dtype)
        x = x + memory_cross_attention(rms_norm(x, norm_mem_q[l]), rms_norm(mem, norm_mem_kv[l]),
                                       mem_wq[l], mem_wk[l], mem_wv[l], mem_wo[l]).astype(x.dtype)
        x = x + swiglu(rms_norm(x, norm_ffn[l]), ffn_w_in[l], ffn_w_out[l]).astype(x.dtype)
    return rms_norm(x, norm_final)
```

```python
import math
import numpy as np
import concourse.bass as bass
import concourse.mybir as mybir
from concourse.bass_utils import run_bass_kernel_spmd

F32 = mybir.dt.float32
BF16 = mybir.dt.bfloat16
AF = mybir.ActivationFunctionType
ALU = mybir.AluOpType

D = 1024
KC = 8
TT = 512
IN_COLS = 3588
FFH = 2816
NJ = FFH // 128
MEMT = 256
N_CORES = 8
SYNC_MODE = "old"


class Buf:
    __slots__ = ("w", "r", "gen")

    def __init__(self):
        self.w = None
        self.r = {}
        self.gen = 0


class T:
    __slots__ = ("ap", "bufs", "gens")

    def __init__(self, ap, bufs):
        self.ap = ap
        self.bufs = bufs
        self.gens = [b.gen for b in bufs]

    def __getitem__(self, k):
        t = T.__new__(T)
        t.ap = self.ap[k]
        t.bufs = self.bufs
        t.gens = self.gens
        return t

    def v(self, fn):
        t = T.__new__(T)
        t.ap = fn(self.ap)
        t.bufs = self.bufs
        t.gens = self.gens
        return t

    def check(self):
        for b, g in zip(self.bufs, self.gens):
            assert b.gen == g, "stale PSUM tile used"


class Prog:
    def __init__(self, nc, sems):
        self.nc = nc
        self.E = {}
        for name in ("pe", "act", "dve", "pool", "sp"):
            self.E[name] = dict(ops=[], sem=sems[name], cnt=0, seen={})
        self.dq = {"sp": dict(slots=[[s, 0] for s in sems["dsp"]], i=0),
                   "pool": dict(slots=[[s, 0] for s in sems["dpool"]], i=0)}
        self.n_ops = 0

    def _waits(self, E, eng, reads, writes):
        waits = {}
        own = E["sem"]

        def need(tok, kind):
            if tok is None:
                return
            sem, val = tok
            if sem is own:
                if eng == "pe" or (SYNC_MODE == "old" and kind != "raw"):
                    return
            if E["seen"].get(sem, 0) >= val:
                return
            if waits.get(sem, 0) < val:
                waits[sem] = val

        for t in reads:
            t.check()
            for b in t.bufs:
                need(b.w, "raw")
        for t in writes:
            t.check()
            for b in t.bufs:
                need(b.w, "waw")
                for s, v in b.r.items():
                    need((s, v), "war")
        for s, v in waits.items():
            E["seen"][s] = v
        return list(waits.items())

    def op(self, eng, fn, reads=(), writes=()):
        E = self.E[eng]
        waits = self._waits(E, eng, reads, writes)
        E["cnt"] += 1
        c = E["cnt"]
        sem = E["sem"]
        E["ops"].append((waits, fn, sem, 1))
        for t in reads:
            for b in t.bufs:
                if b.r.get(sem, 0) < c:
                    b.r[sem] = c
        for t in writes:
            for b in t.bufs:
                b.w = (sem, c)
                b.r = {}
        self.n_ops += 1

    def dma(self, q, out, in_, reads=(), writes=()):
        E = self.E[q]
        dq = self.dq[q]
        slot = dq["slots"][dq["i"] % len(dq["slots"])]
        dq["i"] += 1
        waits = dict(self._waits(E, q, reads, writes))
        sem, val = slot
        if val > 0 and E["seen"].get(sem, 0) < val:
            waits[sem] = max(waits.get(sem, 0), val)
            E["seen"][sem] = val
        slot[1] = val + 16
        nv = slot[1]
        E["ops"].append((list(waits.items()), lambda e: e.dma_start(out=out, in_=in_), sem, 16))
        for t in reads:
            for b in t.bufs:
                b.r[sem] = nv
        for t in writes:
            for b in t.bufs:
                b.w = (sem, nv)
                b.r = {}
        self.n_ops += 1
        return (sem, nv)

    def barrier(self):
        allt = {}
        for n, E in self.E.items():
            if E["cnt"] > 0:
                allt[E["sem"]] = E["cnt"]
        for q in self.dq.values():
            for s, v in q["slots"]:
                if v > 0:
                    allt[s] = v
        for n, E in self.E.items():
            w = []
            for s, v in allt.items():
                if s is E["sem"]:
                    continue
                if E["seen"].get(s, 0) < v:
                    w.append((s, v))
                    E["seen"][s] = v
            if w:
                E["ops"].append((w, None, None, 0))

    def wait_all_on(self, eng):
        E = self.E[eng]
        w = []
        for n, E2 in self.E.items():
            if E2 is not E and E2["cnt"] > 0:
                w.append((E2["sem"], E2["cnt"]))
        for q in self.dq.values():
            for s, v in q["slots"]:
                if v > 0:
                    w.append((s, v))
        E["ops"].append((w, None, None, 0))

    def replay(self, name, e):
        for waits, fn, sem, inc in self.E[name]["ops"]:
            for s, v in waits:
                e.wait_ge(s, v)
            if fn is not None:
                fn(e).then_inc(sem, inc)


class Arena:
    def __init__(self, handle, nwords):
        self.h = handle
        self.n = nwords
        self.top = 0
        self.peak = 0

    def tile(self, free_shape, dtype=F32, parts=128):
        n = 1
        for s in free_shape:
            n *= s
        words = n if dtype == F32 else (n + 1) // 2
        words = (words + 15) // 16 * 16
        off = self.top
        self.top += words
        self.peak = max(self.peak, self.top)
        assert self.top <= self.n, f"SBUF arena overflow {self.top} > {self.n}"
        ap = self.h[:, off:off + (n if dtype == F32 else (n + 1) // 2)]
        if dtype != F32:
            ap = ap.bitcast(dtype)
            if ap.shape[1] != n:
                ap = ap[:, 0:n]
        if len(free_shape) == 2:
            ap = ap.rearrange("p (a b) -> p a b", a=free_shape[0])
        elif len(free_shape) == 3:
            ap = ap.rearrange("p (a b c) -> p a b c", a=free_shape[0], b=free_shape[1])
        elif len(free_shape) == 4:
            ap = ap.rearrange("p (a b c d) -> p a b c d", a=free_shape[0], b=free_shape[1], c=free_shape[2])
        if parts != 128:
            ap = ap[0:parts]
        return T(ap, [Buf()])

    def mark(self):
        return self.top

    def reset(self, m):
        self.top = m


class Psum:
    def __init__(self, banks):
        self.banks = banks
        self.b = [Buf() for _ in range(8)]
        self.rot = list(range(8))
        self.ptr = 0

    def set_rot(self, banks):
        self.rot = list(banks)
        self.ptr = 0

    def _mk(self, b, nq, dtype):
        self.b[b].gen += 1
        ap = self.banks[b][:, 0:nq * 128]
        if dtype != F32:
            ap = ap.bitcast(dtype)
        return T(ap, [self.b[b]])

    def get(self, nq=4, dtype=F32):
        b = self.rot[self.ptr % len(self.rot)]
        self.ptr += 1
        return self._mk(b, nq, dtype)

    def fixed(self, b, dtype=F32):
        return self._mk(b, 4, dtype)


PV = {}


def _pv_layout():
    if PV:
        return PV["_n"]
    c = 0
    for name, n in [("g_mix", 8), ("lru_cw", 8), ("lru_cb", 2), ("lru_br", 2), ("lru_bi", 2), ("lru_lam", 2),
                    ("ret_gw", 2), ("ret_gb", 2), ("rw_mu", 8), ("rw_w0", 2), ("rw_a0", 2), ("rw_kk", 2),
                    ("rw_ka", 2), ("rw_rk", 2), ("rw_gw", 2), ("rw_gb", 2), ("sd_cw", 24), ("sd_cb", 6),
                    ("sd_d", 2), ("sd_nw", 2), ("g_memq", 8), ("g_memkv", 8), ("g_ffn", 8),
                    ("sd_dtb", 4), ("sd_alog", 4)]:
        PV[name] = (c, n)
        c += n
    PV["_n"] = c
    return c


CC = {}


def _cc_layout():
    if CC:
        return CC["_n"]
    c = 0
    for name, n in [("ident", 128), ("tri", 128), ("mstrT", 128), ("mlow", 128), ("maskM", 512), ("mbias", 128),
                    ("bd", 128), ("swap", 128), ("reset", 512), ("g128", 2), ("eps6", 1), ("eps5", 1),
                    ("eps64", 1), ("eps12", 1), ("one", 1), ("g_final", 8)]:
        CC[name] = (c, n)
        c += n
    CC["_n"] = c
    return c


def _chan(v, n):
    return np.ascontiguousarray(np.asarray(v, np.float32).reshape(n, 128).T)


def make_consts(norm_final):
    n = _cc_layout()
    cb = np.zeros((128, n), np.float32)

    def put(name, arr):
        c0, k = CC[name]
        cb[:, c0:c0 + k] = arr

    i = np.arange(128)
    put("ident", np.eye(128, dtype=np.float32))
    tri = (i[:, None] <= i[None, :]).astype(np.float32)
    mstr = (i[:, None] < i[None, :]).astype(np.float32)
    put("tri", tri)
    put("mstrT", mstr)
    put("mlow", mstr.T)
    put("maskM", np.concatenate([mstr, tri, mstr, tri], axis=1))
    put("mbias", np.where(i[:, None] <= i[None, :], 0.0, -30000.0).astype(np.float32))
    bd = ((i[:, None] // 64) == (i[None, :] // 64)).astype(np.float32)
    put("bd", bd)
    partner = np.where(i % 64 < 32, i + 32, i - 32)
    sw = np.zeros((128, 128), np.float32)
    sw[partner, i] = 1.0
    put("swap", sw)
    rs = np.ones((128, 512), np.float32)
    rs[:, ::128] = 0.0
    put("reset", rs)
    gam = 1.0 - np.exp2(-5.0 - np.arange(4, dtype=np.float64))
    g128 = np.zeros((128, 2), np.float32)
    for c in range(2):
        for p in range(128):
            g128[p, c] = gam[2 * c + p // 64] ** 128
    put("g128", g128)
    put("eps6", 1e-6)
    put("eps5", 1e-5)
    put("eps64", 64e-5)
    put("eps12", 1e-12)
    put("one", 1.0)
    put("g_final", _chan(norm_final, 8))
    return cb


def make_rope(S):
    nt = S // TT
    pos = np.arange(S, dtype=np.float32)
    inv_freq = (10000.0 ** (-np.arange(32, dtype=np.float32) / 32.0)).astype(np.float32)
    ang = (pos[:, None] * inv_freq[None, :]).astype(np.float32)
    cos = np.cos(ang).astype(np.float64)
    sin = np.sin(ang).astype(np.float64)
    gam = 1.0 - np.exp2(-5.0 - np.arange(4, dtype=np.float64))
    tl = (np.arange(S) % 128) + 1
    out = np.zeros((nt, 128, 8, TT), np.float32)
    p = np.arange(128)
    n = p % 64
    j = n % 32
    sgn = np.where(n < 32, -1.0, 1.0)
    for c in range(2):
        h = 2 * c + p // 64
        gq = gam[h][:, None] ** tl[None, :]
        gk = gam[h][:, None] ** (-tl[None, :]) / 8.0
        cq = cos[:, j].T * gq
        sq = sin[:, j].T * sgn[:, None] * gq
        ck = cos[:, j].T * gk
        sk = sin[:, j].T * sgn[:, None] * gk
        for t in range(nt):
            sl = slice(t * TT, (t + 1) * TT)
            out[t, :, 0 + 2 * c, :] = cq[:, sl]
            out[t, :, 1 + 2 * c, :] = sq[:, sl]
            out[t, :, 4 + 2 * c, :] = ck[:, sl]
            out[t, :, 5 + 2 * c, :] = sk[:, sl]
    return out


def make_pvec(inp, L):
    n = _pv_layout()
    pv = np.zeros((L, 128, n), np.float32)

    def put(l, name, arr):
        c0, k = PV[name]
        pv[l, :, c0:c0 + k] = arr

    for l in range(L):
        put(l, "g_mix", _chan(inp["norm_mix"][l], 8))
        put(l, "lru_cw", np.concatenate([_chan(inp["lru_conv_w"][l][k], 2) for k in range(4)], axis=1))
        put(l, "lru_cb", _chan(inp["lru_conv_b"][l], 2))
        put(l, "lru_br", _chan(inp["lru_b_r"][l].reshape(-1), 2))
        put(l, "lru_bi", _chan(inp["lru_b_i"][l].reshape(-1), 2))
        put(l, "lru_lam", _chan(inp["lru_lambda"][l], 2))
        put(l, "ret_gw", _chan(inp["ret_gn_w"][l], 2))
        put(l, "ret_gb", _chan(inp["ret_gn_b"][l], 2))
        put(l, "rw_mu", _chan(inp["rwkv_mu"][l], 8))
        put(l, "rw_w0", _chan(inp["rwkv_w0"][l], 2))
        put(l, "rw_a0", _chan(inp["rwkv_a0"][l], 2))
        put(l, "rw_kk", _chan(inp["rwkv_k_k"][l], 2))
        put(l, "rw_ka", _chan(inp["rwkv_k_a"][l], 2))
        put(l, "rw_rk", _chan(inp["rwkv_r_k"][l], 2))
        put(l, "rw_gw", _chan(inp["rwkv_gn_w"][l], 2))
        put(l, "rw_gb", _chan(inp["rwkv_gn_b"][l], 2))
        put(l, "sd_cw", np.concatenate([_chan(inp["ssd_conv_w"][l][k], 6) for k in range(4)], axis=1))
        put(l, "sd_cb", _chan(inp["ssd_conv_b"][l], 6))
        put(l, "sd_d", _chan(np.repeat(np.asarray(inp["ssd_d"][l], np.float32), 64), 2))
        put(l, "sd_nw", _chan(inp["ssd_norm_w"][l], 2))
        put(l, "g_memq", _chan(inp["norm_mem_q"][l], 8))
        put(l, "g_memkv", _chan(inp["norm_mem_kv"][l], 8))
        put(l, "g_ffn", _chan(inp["norm_ffn"][l], 8))
        put(l, "sd_dtb", np.broadcast_to(np.asarray(inp["ssd_dt_bias"][l], np.float32)[None, :], (128, 4)))
        put(l, "sd_alog", np.broadcast_to(np.asarray(inp["ssd_a_log"][l], np.float32)[None, :], (128, 4)))
    return pv


def make_smallw(inp, L):
    sw = np.zeros((L, 128, 1280), np.float32)
    for l in range(L):
        for c in range(2):
            for hh in range(2):
                blk = slice(hh * 64, hh * 64 + 64)
                sw[l, blk, c * 128 + hh * 64: c * 128 + hh * 64 + 64] = inp["lru_w_r"][l][2 * c + hh]
                sw[l, blk, 256 + c * 128 + hh * 64: 256 + c * 128 + hh * 64 + 64] = inp["lru_w_i"][l][2 * c + hh]
        sw[l, 0:64, 512:768] = inp["rwkv_w2"][l]
        sw[l, 64:128, 768:1024] = inp["rwkv_a2"][l]
        sw[l, :, 1024:1280] = inp["rwkv_g2"][l]
    return sw


def build(L, S, NSEQ, flags=None):
    from contextlib import ExitStack
    flags = flags or {}
    EN = lambda k: flags.get(k, True)
    NT = S // TT
    NCHS = S // 128
    GT = 2 if NT % 2 == 0 else 1
    nc = bass.Bass("TRN2", target_bir_lowering=False)
    NPV = _pv_layout()
    NCC = _cc_layout()

    def dram(name, shape, kind="ExternalInput"):
        return nc.dram_tensor(name, shape, F32, kind=kind).ap()

    x_d = dram("x", [NSEQ * S, D])
    mem_d = dram("mem", [NSEQ * MEMT, D])
    cb_d = dram("cb", [128, NCC])
    rope_d = dram("rope", [NT, 128, 8, TT])
    pv_d = dram("pv", [L, 128, NPV])
    sw_d = dram("smallw", [L, 128, 1280])
    win_d = dram("w_in", [L, D, IN_COLS])
    wout_d = dram("w_out", [L, D, D])
    wq_d = dram("mem_wq", [L, D, D])
    wk_d = dram("mem_wk", [L, D, D])
    wv_d = dram("mem_wv", [L, D, D])
    wo_d = dram("mem_wo", [L, D, D])
    f1_d = dram("ffn_w_in", [L, D, 2 * FFH])
    f2_d = dram("ffn_w_out", [L, FFH, D])
    out_d = dram("out", [NSEQ * S, D], kind="ExternalOutput")

    NW = 52224
    with ExitStack() as es:
        arena_h = es.enter_context(nc.sbuf_tensor("arena", [128, NW], F32))
        banks = [es.enter_context(nc.psum_tensor(f"bank{i}", [128, 512], F32)) for i in range(8)]
        sems = {}
        for n in ("pe", "act", "dve", "pool", "sp"):
            sems[n] = es.enter_context(nc.semaphore("s_" + n))
        sems["dsp"] = [es.enter_context(nc.semaphore(f"dsp{i}")) for i in range(12)]
        sems["dpool"] = [es.enter_context(nc.semaphore(f"dpl{i}")) for i in range(12)]
        Pg = Prog(nc, sems)
        AR = Arena(arena_h, NW)
        PS = Psum([b[:, :] for b in banks])

        def _a(x):
            return x.ap if isinstance(x, T) else x

        def _rd(*xs):
            return [x for x in xs if isinstance(x, T)]

        def mm(out, lhsT, rhs, start=True, stop=True):
            Pg.op("pe", lambda e: e.matmul(out.ap, lhsT.ap, rhs.ap, start=start, stop=stop), [lhsT, rhs], [out])

        def tpose(out, in_, ident):
            Pg.op("pe", lambda e: e.transpose(out.ap, in_.ap, ident.ap), [in_, ident], [out])

        def act(out, in_, func, scale=1.0, bias=0.0):
            Pg.op("act", lambda e: e.activation(out=out.ap, in_=in_.ap, func=func, bias=_a(bias), scale=_a(scale)),
                  _rd(in_, scale, bias), [out])

        def tt(out, a, b, op, eng="dve"):
            Pg.op(eng, lambda e: e.tensor_tensor(out=out.ap, in0=a.ap, in1=b.ap, op=op), [a, b], [out])

        def ts(out, a, s1, op0, s2=None, op1=None, eng="dve"):
            kw = dict(out=out.ap, in0=a.ap, scalar1=_a(s1), scalar2=_a(s2), op0=op0)
            if op1 is not None:
                kw["op1"] = op1
            Pg.op(eng, lambda e: e.tensor_scalar(**kw), _rd(a, s1, s2), [out])

        def stt(out, a, s, b, op0, op1):
            Pg.op("dve", lambda e: e.scalar_tensor_tensor(out=out.ap, in0=a.ap, scalar=_a(s), in1=b.ap,
                                                          op0=op0, op1=op1), _rd(a, s, b), [out])

        def cp(out, a, eng="dve"):
            if eng == "act":
                Pg.op("act", lambda e: e.copy(out=out.ap, in_=a.ap), [a], [out])
            else:
                Pg.op(eng, lambda e: e.tensor_copy(out=out.ap, in_=a.ap), [a], [out])

        def scan(out, d0, d1, init):
            Pg.op("dve", lambda e: e.tensor_tensor_scan(out=out.ap, data0=d0.ap, data1=d1.ap, initial=_a(init),
                                                        op0=ALU.mult, op1=ALU.add), _rd(d0, d1, init), [out])

        def recip(out, a):
            Pg.op("dve", lambda e: e.reciprocal(out=out.ap, in_=a.ap), [a], [out])

        def mset(t, val, eng="dve"):
            Pg.op(eng, lambda e: e.memset(t.ap, val), [], [t])

        def wload(dst, src_ap):
            Pg.dma("pool", dst.ap, src_ap, writes=[dst])

        def ld(dst, src_ap):
            Pg.dma("sp", dst.ap, src_ap, writes=[dst])

        MUL, ADD, SUB = ALU.mult, ALU.add, ALU.subtract
        DBG = {}

        def dbg(name, t):
            if not flags.get("dbg") or name in DBG:
                return
            shp = list(t.ap.shape)
            d = nc.dram_tensor("dbg_" + name, shp, t.ap.dtype, kind="ExternalOutput").ap()
            DBG[name] = shp
            Pg.dma("sp", d, t.ap, reads=[t])

        xT = AR.tile([8, S])
        cb = AR.tile([NCC])
        pv = AR.tile([NPV])
        DVL = {}
        ndv = 0
        for name, n in [("hbr", 2), ("hbi", 2), ("cA", 2), ("hcA", 2), ("sdcwh", 24), ("sdcbh", 6), ("omm", 8),
                        ("hw0", 2), ("ha0", 2), ("omka", 2), ("aneg", 4), ("tmp", 8)]:
            DVL[name] = (ndv, n)
            ndv += n
        dv = AR.tile([ndv])
        smallw = AR.tile([1280], BF16)
        ident_bf = AR.tile([128], BF16)
        ones_bf = AR.tile([128], BF16)
        onesD_bf = AR.tile([128], BF16)
        ones128_bf = AR.tile([128], BF16)
        bd1_bf = AR.tile([128], BF16)
        bd64_bf = AR.tile([128], BF16)
        swap_bf = AR.tile([128], BF16)
        ones_f = AR.tile([128])
        memnT = AR.tile([8, MEMT], BF16)
        KT = AR.tile([8, MEMT], BF16)
        Vm = AR.tile([2, D], BF16)
        Vpad = AR.tile([4, 4, 128], BF16)
        Upad = [[AR.tile([128], BF16) for _ in range(2)] for _ in range(2)]
        SpadD = [[AR.tile([128], BF16) for _ in range(2)] for _ in range(2)]
        SD_f = [AR.tile([128]) for _ in range(2)]
        SB_f = [AR.tile([128]) for _ in range(2)]
        SB_bf = [AR.tile([128], BF16) for _ in range(2)]
        SC_f = [AR.tile([128]) for _ in range(2)]
        SC_bf = [AR.tile([128], BF16) for _ in range(2)]
        histA = [AR.tile([3]) for _ in range(2)]
        histD = [AR.tile([3]) for _ in range(6)]
        histC = [AR.tile([1]) for _ in range(8)]
        hprev = [AR.tile([1]) for _ in range(2)]
        stage = [AR.tile([3 + TT]) for _ in range(2)]
        PH0 = AR.mark()

        def C(name, a=None, b=None):
            c0, n = CC[name]
            if a is None:
                return cb[:, c0:c0 + n]
            return cb[:, c0 + a:c0 + b]

        def PVc(name, a=0, b=None):
            c0, n = PV[name]
            b = n if b is None else b
            return pv[:, c0 + a:c0 + b]

        def DVc(name, a=0, b=None):
            c0, n = DVL[name]
            b = n if b is None else b
            return dv[:, c0 + a:c0 + b]

        ident_f = C("ident")
        eps6, eps5, eps64, eps12, one_c = C("eps6"), C("eps5"), C("eps64"), C("eps12"), C("one")

        ld(cb, cb_d[:, :])
        cp(ident_bf, C("ident"))
        mset(ones_bf, 1.0)
        mset(onesD_bf, 1.0 / 1024.0)
        mset(ones128_bf, 1.0 / 128.0)
        mset(ones_f, 1.0)
        cp(bd1_bf, C("bd"))
        ts(bd64_bf, C("bd"), 1.0 / 64.0, MUL)
        cp(swap_bf, C("swap"))
        mset(Vpad, 0.0)
        for a_ in Upad + SpadD:
            for b_ in a_:
                mset(b_, 0.0)

        def rsqrt(out, in_, scale, eps_t):
            act(out, in_, AF.Ln, scale=scale, bias=eps_t)
            act(out, out, AF.Exp, scale=-0.5)

        def rmsnorm_tile(xsl, gcols, hT, sqt, rstd, n=TT):
            bank = PS.get(4)
            bk = bank[:, 0:n]
            for k in range(8):
                sq = sqt[k % 2]
                act(sq, xsl[:, k, :], AF.Square)
                mm(bk, onesD_bf, sq, start=(k == 0), stop=(k == 7))
            rsqrt(rstd, bk, 1.0, eps6)
            for k in range(8):
                if gcols is None:
                    tt(hT[:, k, :], xsl[:, k, :], rstd, MUL)
                else:
                    stt(hT[:, k, :], xsl[:, k, :], gcols[:, k:k + 1], rstd, MUL, MUL)

        def wsrc(w_d, l, r0, kc, c0, n):
            return w_d[l, r0:r0 + kc * 128, c0:c0 + n].rearrange("(k p) c -> p k c", p=128)

        def v4(t):
            return t.v(lambda a: a.rearrange("p (a b) -> p a b", a=4))

        def phase0(sq):
            m = AR.mark()
            PS.set_rot(range(8))
            xst = [AR.tile([D]) for _ in range(2)]
            memT = AR.tile([8, MEMT])
            sqt = [AR.tile([MEMT], BF16) for _ in range(2)]
            rstd = AR.tile([MEMT])
            for c in range(NCHS):
                st = xst[c % 2]
                ld(st, x_d[sq * S + c * 128: sq * S + (c + 1) * 128, :])
                for half in range(2):
                    bk = PS.get(4)
                    for q in range(4):
                        k = half * 4 + q
                        tpose(bk[:, q * 128:(q + 1) * 128], st[:, k * 128:(k + 1) * 128], ident_f)
                    cp(xT[:, half * 4:(half + 1) * 4, c * 128:(c + 1) * 128], v4(bk), eng=("act" if half else "dve"))
            for mc in range(2):
                st = xst[mc % 2]
                ld(st, mem_d[sq * MEMT + mc * 128: sq * MEMT + (mc + 1) * 128, :])
                for half in range(2):
                    bk = PS.get(4)
                    for q in range(4):
                        k = half * 4 + q
                        tpose(bk[:, q * 128:(q + 1) * 128], st[:, k * 128:(k + 1) * 128], ident_f)
                    cp(memT[:, half * 4:(half + 1) * 4, mc * 128:(mc + 1) * 128], v4(bk), eng=("act" if half else "dve"))
            rmsnorm_tile(memT, None, memnT, sqt, rstd, n=MEMT)
            Pg.barrier()
            AR.reset(m)

        def layer_setup(l):
            ld(pv, pv_d[l])
            wload(smallw, sw_d[l])
            ts(DVc("hbr"), PVc("lru_br"), 0.5, MUL)
            ts(DVc("hbi"), PVc("lru_bi"), 0.5, MUL)
            act(DVc("tmp", 0, 2), PVc("lru_lam"), AF.Exp, scale=-1.0)
            act(DVc("tmp", 0, 2), DVc("tmp", 0, 2), AF.Ln, scale=1.0, bias=one_c)
            ts(DVc("cA"), DVc("tmp", 0, 2), -8.0, MUL)
            ts(DVc("hcA"), DVc("tmp", 0, 2), -4.0, MUL)
            ts(DVc("sdcwh"), PVc("sd_cw"), 0.5, MUL)
            ts(DVc("sdcbh"), PVc("sd_cb"), 0.5, MUL)
            ts(DVc("omm"), PVc("rw_mu"), -1.0, MUL, 1.0, ADD)
            ts(DVc("hw0"), PVc("rw_w0"), 0.5, MUL)
            ts(DVc("ha0"), PVc("rw_a0"), 0.5, MUL)
            ts(DVc("omka"), PVc("rw_ka"), -1.0, MUL, 1.0, ADD)
            act(DVc("tmp", 4, 8), PVc("sd_alog"), AF.Exp)
            ts(DVc("aneg"), DVc("tmp", 4, 8), -1.0, MUL)

        def phase_ffn(l):
            m = AR.mark()
            PS.set_rot(range(8))
            GTOK = GT * TT
            hT2 = AR.tile([8, GTOK], BF16)
            actT = AR.tile([NJ, GTOK], BF16)
            sqt = [AR.tile([TT], BF16) for _ in range(2)]
            rstd = AR.tile([TT])
            tg = [AR.tile([TT]) for _ in range(2)]
            s1 = [AR.tile([TT]) for _ in range(2)]
            wgu = [AR.tile([2, 8, 256], BF16) for _ in range(2)]
            w2s = [AR.tile([NJ, 128], BF16) for _ in range(2)]
            for g in range(NT // GT):
                for ti in range(GT):
                    t0 = (g * GT + ti) * TT
                    rmsnorm_tile(xT[:, :, t0:t0 + TT], PVc("g_ffn"), hT2[:, :, ti * TT:(ti + 1) * TT], sqt, rstd)
                for jb in range(NJ // 2):
                    w = wgu[jb % 2]
                    wload(w[:, 0], wsrc(f1_d, l, 0, 8, jb * 256, 256))
                    wload(w[:, 1], wsrc(f1_d, l, 0, 8, FFH + jb * 256, 256))
                    for jj in range(2):
                        j = jb * 2 + jj
                        for ti in range(GT):
                            pg_ = PS.get(4)
                            pu_ = PS.get(4)
                            for k in range(8):
                                mm(pg_, w[:, 0, k, jj * 128:(jj + 1) * 128], hT2[:, k, ti * TT:(ti + 1) * TT],
                                   start=(k == 0), stop=(k == 7))
                            for k in range(8):
                                mm(pu_, w[:, 1, k, jj * 128:(jj + 1) * 128], hT2[:, k, ti * TT:(ti + 1) * TT],
                                   start=(k == 0), stop=(k == 7))
                            tgi, s1i = tg[(j * GT + ti) % 2], s1[(j * GT + ti) % 2]
                            act(tgi, pg_, AF.Tanh, scale=0.5)
                            stt(s1i, tgi, 1.0, pg_, ADD, MUL)
                            stt(actT[:, j, ti * TT:(ti + 1) * TT], s1i, 0.5, pu_, MUL, MUL)
                for d in range(8):
                    w = w2s[d % 2]
                    wload(w, f2_d[l, :, d * 128:(d + 1) * 128].rearrange("(j p) c -> p j c", p=128))
                    for ti in range(GT):
                        t0 = (g * GT + ti) * TT
                        bk = PS.get(4)
                        for j in range(NJ):
                            mm(bk, w[:, j, :], actT[:, j, ti * TT:(ti + 1) * TT], start=(j == 0), stop=(j == NJ - 1))
                        tt(xT[:, d, t0:t0 + TT], xT[:, d, t0:t0 + TT], bk, ADD)
            Pg.barrier()
            AR.reset(m)

        def phase_attn(l):
            m = AR.mark()
            PS.set_rot(range(8))
            wq = AR.tile([8, D], BF16)
            wo = AR.tile([8, D], BF16)
            wkv = [AR.tile([8, 512], BF16) for _ in range(2)]
            hT = AR.tile([8, TT], BF16)
            qT = AR.tile([8, TT], BF16)
            oT = AR.tile([8, TT], BF16)
            et = [AR.tile([TT], BF16) for _ in range(4)]
            rden = AR.tile([TT])
            sqt = [AR.tile([TT], BF16) for _ in range(2)]
            rstd = AR.tile([TT])
            hmT = AR.tile([8, MEMT], BF16)
            wload(wq, wsrc(wq_d, l, 0, 8, 0, D))
            for k in range(8):
                ts(hmT[:, k, :], memnT[:, k, :], PVc("g_memkv", k, k + 1), MUL)
            for half in range(2):
                w = wkv[half]
                wload(w, wsrc(wk_d, l, 0, 8, half * 512, 512))
                for cc in range(4):
                    bk = PS.get(2)
                    for k in range(8):
                        mm(bk, w[:, k, cc * 128:(cc + 1) * 128], hmT[:, k, :], start=(k == 0), stop=(k == 7))
                    cp(KT[:, half * 4 + cc, :], bk, eng="act")
            for half in range(2):
                w = wkv[half]
                wload(w, wsrc(wv_d, l, 0, 8, half * 512, 512))
                for mc in range(2):
                    bk = PS.get(4)
                    for k in range(8):
                        mm(bk, hmT[:, k, mc * 128:(mc + 1) * 128], w[:, k, :], start=(k == 0), stop=(k == 7))
                    cp(Vm[:, mc, half * 512:(half + 1) * 512], bk, eng="act")
            wload(wo, wsrc(wo_d, l, 0, 8, 0, D))
            for ti in range(NT):
                t0 = ti * TT
                rmsnorm_tile(xT[:, :, t0:t0 + TT], PVc("g_memq"), hT, sqt, rstd)
                for c in range(8):
                    bk = PS.get(4)
                    for k in range(8):
                        mm(bk, wq[:, k, c * 128:(c + 1) * 128], hT[:, k, :], start=(k == 0), stop=(k == 7))
                    cp(qT[:, c, :], bk, eng="act")
                for h in range(4):
                    es_ = []
                    for mc in range(2):
                        bk = PS.get(4)
                        for dc in range(2):
                            mm(bk, KT[:, 2 * h + dc, mc * 128:(mc + 1) * 128], qT[:, 2 * h + dc, :],
                               start=(dc == 0), stop=(dc == 1))
                        e_ = et[(h % 2) * 2 + mc]
                        act(e_, bk, AF.Exp, scale=1.0 / 16.0)
                        es_.append(e_)
                    den = PS.get(4)
                    for mc in range(2):
                        mm(den, ones_bf, es_[mc], start=(mc == 0), stop=(mc == 1))
                    recip(rden, den)
                    for dc in range(2):
                        bo = PS.get(4)
                        for mc in range(2):
                            mm(bo, Vm[:, mc, (2 * h + dc) * 128:(2 * h + dc + 1) * 128], es_[mc],
                               start=(mc == 0), stop=(mc == 1))
                        tt(oT[:, 2 * h + dc, :], bo, rden, MUL)
                for d in range(8):
                    bk = PS.get(4)
                    for k in range(8):
                        mm(bk, wo[:, k, d * 128:(d + 1) * 128], oT[:, k, :], start=(k == 0), stop=(k == 7))
                    tt(xT[:, d, t0:t0 + TT], xT[:, d, t0:t0 + TT], bk, ADD)
            Pg.barrier()
            AR.reset(m)

        def phase_final(sq):
            m = AR.mark()
            PS.set_rot(range(8))
            hF = AR.tile([8, TT])
            sqt = [AR.tile([TT], BF16) for _ in range(2)]
            rstd = AR.tile([TT])
            ost = [AR.tile([D]) for _ in range(2)]
            for ti in range(NT):
                t0 = ti * TT
                rmsnorm_tile(xT[:, :, t0:t0 + TT], C("g_final"), hF, sqt, rstd)
                for tc in range(4):
                    o = ost[tc % 2]
                    for half in range(2):
                        bk = PS.get(4)
                        for q in range(4):
                            k = half * 4 + q
                            tpose(bk[:, q * 128:(q + 1) * 128], hF[:, k, tc * 128:(tc + 1) * 128], ident_f)
                        cp(o[:, half * 512:(half + 1) * 512], bk, eng=("act" if half else "dve"))
                    r0 = sq * S + t0 + tc * 128
                    Pg.dma("sp", out_d[r0:r0 + 128, :], o.ap, reads=[o])
            Pg.barrier()
            AR.reset(m)

        PHASE1 = {}

        class Scratch:
            U = 64

            def __init__(self, nunits):
                self.n = nunits
                self.base = AR.top
                AR.top += nunits * self.U
                AR.peak = max(AR.peak, AR.top)
                assert AR.top <= AR.n, f"scratch overflow {AR.top} > {AR.n}"
                self.bufs = [Buf() for _ in range(nunits)]
                self.used = [False] * nunits
                self.owner = {}
                self.peak = 0

            def _get(self, nwords):
                k = (nwords + self.U - 1) // self.U
                i = 0
                while i + k <= self.n:
                    j = i
                    while j < i + k and not self.used[j]:
                        j += 1
                    if j == i + k:
                        for q in range(i, i + k):
                            self.used[q] = True
                        self.peak = max(self.peak, sum(self.used))
                        return i, k
                    i = j + 1
                raise AssertionError(f"scratch pool exhausted (need {k} units, used {sum(self.used)}/{self.n})")

            def f32(self, n):
                i, k = self._get(n)
                ap = arena_h[:, self.base + i * self.U: self.base + i * self.U + n]
                t = T(ap, self.bufs[i:i + k])
                self.owner[id(t)] = (i, k)
                return t

            def bf(self, n):
                i, k = self._get((n + 1) // 2)
                ap = arena_h[:, self.base + i * self.U: self.base + i * self.U + (n + 1) // 2].bitcast(BF16)
                t = T(ap, self.bufs[i:i + k])
                self.owner[id(t)] = (i, k)
                return t

            def free(self, *ts_):
                for t in ts_:
                    i, k = self.owner.pop(id(t))
                    for q in range(i, i + k):
                        self.used[q] = False

        def bc_mid(t, n):
            return t.v(lambda a: a.unsqueeze(1).broadcast_to([128, n, a.shape[1]]))

        def bc_last(t, n):
            return t.v(lambda a: a.unsqueeze(2).broadcast_to([128, a.shape[1], n]))

        def r3(t, a_):
            return t.v(lambda x: x.rearrange("p (a b) -> p a b", a=a_))

        def r4(t, a_, b_):
            return t.v(lambda x: x.rearrange("p (a b c) -> p a b c", a=a_, b=b_))

        VpadV = Vpad.v(lambda a: a.rearrange("p t (c h) n -> p t c h n", c=2))

        def phase_mix(l):
            m = AR.mark()
            PS.set_rot([2, 3, 4, 5, 6, 7])
            wsl = [AR.tile([8, 512], BF16) for _ in range(3)]
            hT = AR.tile([8, TT], BF16)
            yT = AR.tile([8, TT], BF16)
            sqt = [AR.tile([TT], BF16) for _ in range(2)]
            rstd = AR.tile([TT])
            tri4 = AR.tile([TT])
            for i4 in range(4):
                cp(tri4[:, i4 * 128:(i4 + 1) * 128], C("tri"))
            SC = Scratch((AR.n - AR.top) // Scratch.U)
            for t_ in SB_f + SC_f + SD_f + SB_bf + SC_bf + histA + histD + histC + hprev:
                mset(t_, 0.0)
            for a_ in SpadD:
                for b_ in a_:
                    mset(b_, 0.0)
            if not (EN("A") and EN("B") and EN("C") and EN("D")):
                mset(yT, 0.0)

            GROUPS = [(g * 512, 512) for g in range(7)] + [(3584, 4)]
            seq = []
            for ti in range(NT):
                for g in range(8):
                    seq.append(("in", g))
                seq.append(("o", 0))
                seq.append(("o", 1))
            loaded = {}
            state = dict(nload=0)

            def issue_load():
                i = state["nload"]
                if i >= len(seq):
                    return
                kind, g = seq[i]
                slot = wsl[i % 3]
                if kind == "in":
                    c0, n = GROUPS[g]
                    wload(slot[:, :, 0:n], wsrc(win_d, l, 0, 8, c0, n))
                else:
                    wload(slot, wsrc(wout_d, l, 0, 8, g * 512, 512))
                loaded[i] = slot
                state["nload"] += 1

            issue_load()
            issue_load()
            issue_load()
            cur = dict(i=0)

            def next_slot():
                i = cur["i"]
                cur["i"] += 1
                return loaded.pop(i)

            release = issue_load

            def proj(slot, c0, rows=128):
                bk = PS.get(4)
                for k in range(8):
                    mm(bk[0:rows] if rows != 128 else bk, slot[:, k, c0:c0 + rows], hT[:, k, :],
                       start=(k == 0), stop=(k == 7))
                return bk

            def conv_stage(bk, hist, idx, wk, bias):
                st = stage[idx % 2]
                cp(st[:, 0:3], hist)
                cp(st[:, 3:3 + TT], bk, eng="act")
                cv = SC.f32(TT)
                ts(cv, st[:, 0:TT], wk(0), MUL, bias, ADD)
                for k in range(1, 4):
                    stt(cv, st[:, k:k + TT], wk(k), cv, MUL, ADD)
                cp(hist, st[:, TT:TT + 3])
                return cv

            def group_norm(ybank, eps_t, gw, gb):
                ysb = SC.bf(TT)
                cp(ysb, ybank, eng="act")
                ysq = SC.bf(TT)
                act(ysq, ybank, AF.Square)
                pm = PS.get(4)
                mm(pm, bd64_bf, ysb)
                pq = PS.get(4)
                mm(pq, bd64_bf, ysq)
                mean = SC.f32(TT)
                cp(mean, pm, eng="act")
                var = SC.f32(TT)
                stt(var, mean, -1.0, mean, MUL, MUL)
                tt(var, pq, var, ADD)
                ts(var, var, 0.0, ALU.max)
                rsqrt(var, var, 1.0, eps_t)
                yc = SC.f32(TT)
                tt(yc, ybank, mean, SUB)
                tt(yc, yc, var, MUL)
                ts(yc, yc, gw, MUL, gb, ADD)
                SC.free(ysb, ysq, mean, var)
                return yc

            tri_b4 = bc_mid(C("tri"), 4)
            mbias_b4 = bc_mid(C("mbias"), 4)

            for ti in range(NT):
                t0 = ti * TT
                xsl = xT[:, :, t0:t0 + TT]
                rmsnorm_tile(xsl, PVc("g_mix"), hT, sqt, rstd)
                yb = [PS.fixed(0), PS.fixed(1)]

                slot = next_slot()
                if not EN("A"):
                    release()
                if EN("A"):
                    for c in range(2):
                        pg_ = proj(slot, c * 128)
                        px_ = proj(slot, 256 + c * 128)
                        gt = SC.f32(TT)
                        cp(gt, pg_, eng="act")
                        cv = conv_stage(px_, histA[c], c, lambda k: PVc("lru_cw", k * 2 + c, k * 2 + c + 1),
                                        PVc("lru_cb", c, c + 1))
                        cvb = SC.bf(TT)
                        cp(cvb, cv, eng="act")
                        pr = PS.get(4)
                        mm(pr, smallw[:, c * 128:(c + 1) * 128], cvb)
                        pi_ = PS.get(4)
                        mm(pi_, smallw[:, 256 + c * 128:256 + (c + 1) * 128], cvb)
                        tr = SC.f32(TT)
                        act(tr, pr, AF.Tanh, scale=0.5, bias=DVc("hbr", c, c + 1))
                        ti_ = SC.f32(TT)
                        act(ti_, pi_, AF.Tanh, scale=0.5, bias=DVc("hbi", c, c + 1))
                        a_ = SC.f32(TT)
                        act(a_, tr, AF.Exp, scale=DVc("hcA", c, c + 1), bias=DVc("hcA", c, c + 1))
                        s_ = SC.f32(TT)
                        act(s_, tr, AF.Exp, scale=DVc("cA", c, c + 1), bias=DVc("cA", c, c + 1))
                        act(s_, s_, AF.Ln, scale=-1.0, bias=one_c)
                        act(s_, s_, AF.Exp, scale=0.5)
                        stt(ti_, ti_, 1.0, cv, ADD, MUL)
                        stt(ti_, ti_, 0.5, s_, MUL, MUL)
                        scan(tr, a_, ti_, hprev[c])
                        cp(hprev[c], tr[:, TT - 1:TT])
                        act(s_, gt, AF.Square)
                        ts(s_, s_, 0.044715, MUL, 1.0, ADD)
                        tt(s_, s_, gt, MUL)
                        act(s_, s_, AF.Tanh, scale=0.7978845608028654)
                        stt(s_, s_, 1.0, gt, ADD, MUL)
                        stt(yT[:, c, :], s_, 0.5, tr, MUL, MUL)
                        SC.free(gt, cv, cvb, tr, ti_, a_, s_)
                    release()

                slot1 = next_slot()
                slot2 = next_slot()
                if not EN("B"):
                    release()
                    release()
                if EN("B"):
                    rope = SC.f32(8 * TT)
                    rope3 = r3(rope, 8)
                    for i8 in range(8):
                        ld(rope3[:, i8, :], rope_d[ti, :, i8, :])
                    qk = [[SC.bf(TT) for _ in range(2)] for _ in range(2)]
                    for is_k in range(2 if "Q" not in flags.get("Bskip", "") else 0):
                        for c in range(2):
                            bk = proj(slot1, is_k * 256 + c * 128)
                            xb = SC.bf(TT)
                            cp(xb, bk, eng="act")
                            p2 = PS.get(4)
                            mm(p2, swap_bf, xb)
                            t1 = SC.f32(TT)
                            tt(t1, bk, rope3[:, 4 * is_k + 2 * c + 0, :], MUL)
                            t2 = SC.f32(TT)
                            tt(t2, p2, rope3[:, 4 * is_k + 2 * c + 1, :], MUL)
                            tt(qk[is_k][c], t1, t2, ADD)
                            SC.free(xb, t1, t2)
                    SC.free(rope)
                    release()
                    qT_, kT_ = qk
                    dbg("B_q0", qT_[0]); dbg("B_k0", kT_[0]); dbg("B_hT", hT)
                    ktok = SC.bf(4 * 256)
                    ktok3 = r3(ktok, 4)
                    vt = SC.bf(4 * 256)
                    vt3 = r3(vt, 4)
                    for tc in range(4):
                        tcs = slice(tc * 128, (tc + 1) * 128)
                        if "T" not in flags.get("Bskip", ""):
                            pt = PS.get(1, BF16)
                            for c in range(2):
                                tpose(pt[:, c * 128:(c + 1) * 128], kT_[c][:, tcs], ident_bf)
                            cp(ktok3[:, tc, :], pt, eng="act")
                        if "v" in flags.get("Bskip", ""):
                            continue
                        bv = PS.get(2)
                        for k in range(8):
                            mm(bv, hT[:, k, tcs], slot2[:, k, 0:256], start=(k == 0), stop=(k == 7))
                        cp(vt3[:, tc, :], bv, eng="act")
                        bv4 = r4(vt3[:, tc, :], 2, 2)
                        for hh in range(2 if "V" not in flags.get("Bskip", "") else 0):
                            ts(VpadV[:, tc, :, hh, hh * 64:(hh + 1) * 64], bv4[:, :, hh, :], 1.0, MUL)
                    sg = []
                    for c in range(2 if "G" not in flags.get("Bskip", "") else 0):
                        bk = proj(slot2, 256 + c * 128)
                        tg_ = SC.f32(TT)
                        act(tg_, bk, AF.Tanh, scale=0.5)
                        s = SC.f32(TT)
                        stt(s, tg_, 1.0, bk, ADD, MUL)
                        SC.free(tg_)
                        sg.append(s)
                    release()
                    for tc in range(4 if flags.get("Bstage", 3) >= 2 else 0):
                        tcs = slice(tc * 128, (tc + 1) * 128)
                        sm = SC.bf(TT)
                        sm4 = r4(sm, 2, 2)
                        for hh in range(2):
                            r0 = hh * 64
                            sbh = PS.get(2)
                            for c in range(2):
                                mm(sbh[:, c * 128:(c + 1) * 128], kT_[c][r0:r0 + 64, tcs], qT_[c][r0:r0 + 64, tcs])
                            tt(sm4[:, :, hh, :], r3(sbh, 2), r3(tri4[:, 0:256], 2), MUL)
                        for c in range(2):
                            ycol = yb[c][:, tcs]
                            mm(ycol, SB_bf[c], qT_[c][:, tcs], start=True, stop=False)
                            for hh in range(2):
                                h = 2 * c + hh
                                mm(ycol, Vpad[:, tc, h, :], sm[:, h * 128:(h + 1) * 128], start=False, stop=(hh == 1))
                            pS = PS.get(1)
                            mm(pS, ktok3[:, tc, c * 128:(c + 1) * 128], vt3[:, tc, c * 128:(c + 1) * 128])
                            tmp = SC.f32(128)
                            stt(tmp, pS, C("g128", c, c + 1), C("bd"), MUL, MUL)
                            stt(SB_f[c], SB_f[c], C("g128", c, c + 1), tmp, MUL, ADD)
                            cp(SB_bf[c], SB_f[c], eng="act")
                            SC.free(tmp)
                        SC.free(sm)
                    if flags.get("dbg"):
                        dbg("B_vt", vt); dbg("B_ktok", ktok); dbg("B_sg0", sg[0]); dbg("B_Vpad", Vpad)
                    for c in range(2 if flags.get("Bstage", 3) >= 3 else 0):
                        ydb = SC.f32(TT)
                        cp(ydb, yb[c])
                        dbg("B_y%d" % c, ydb)
                        SC.free(ydb)
                        yc = group_norm(yb[c], eps5, PVc("ret_gw", c, c + 1), PVc("ret_gb", c, c + 1))
                        dbg("B_yn%d" % c, yc)
                        stt(yT[:, 2 + c, :], yc, 0.5, sg[c], MUL, MUL)
                        SC.free(yc)
                    SC.free(ktok, vt, *sg, *qT_, *kT_)
                elif False:
                    pass

                slot3 = next_slot()
                slot4 = next_slot()
                if not EN("C"):
                    release()
                    release()
                if EN("C"):
                    def shiftC(idx, bk):
                        st = stage[idx % 2]
                        cp(st[:, 2:3], histC[idx])
                        cp(st[:, 3:3 + TT], bk, eng="act")
                        o = SC.f32(TT)
                        ts(o, st[:, 3:3 + TT], DVc("omm", idx, idx + 1), MUL)
                        stt(o, st[:, 2:2 + TT], PVc("rw_mu", idx, idx + 1), o, MUL, ADD)
                        cp(histC[idx], st[:, 2 + TT:3 + TT])
                        return o

                    rf = [shiftC(c, proj(slot3, c * 128)) for c in range(2)]
                    kf = [shiftC(2 + c, proj(slot3, 256 + c * 128)) for c in range(2)]
                    release()
                    vf = [shiftC(4 + c, proj(slot4, c * 128)) for c in range(2)]
                    lo6 = shiftC(6, proj(slot4, 256))
                    lo7 = shiftC(7, proj(slot4, 384))
                    release()
                    tw = SC.bf(TT)
                    act(tw[0:64], lo6[0:64], AF.Tanh)
                    al = SC.bf(TT)
                    cp(al[64:128], lo6[64:128], eng="act")
                    sgl = SC.bf(TT)
                    act(lo7, lo7, AF.Tanh, scale=0.5)
                    ts(sgl, lo7, 0.5, MUL, 0.5, ADD)
                    SC.free(lo6, lo7)
                    gT_, bon, Wt, ARt, Ktl, Btl, vb, ARb = [], [], [], [], [], [], [], []
                    for c in range(2):
                        th = SC.f32(TT)
                        pw = PS.get(4)
                        mm(pw, smallw[0:64, 512 + c * 128:512 + (c + 1) * 128], tw[0:64])
                        logw = SC.f32(TT)
                        act(th, pw, AF.Tanh, scale=0.5, bias=DVc("hw0", c, c + 1))
                        ts(logw, th, -0.3032653298563167, MUL, -0.3032653298563167, ADD)
                        pa = PS.get(4)
                        mm(pa, smallw[64:128, 768 + c * 128:768 + (c + 1) * 128], al[64:128])
                        a_ = SC.f32(TT)
                        act(th, pa, AF.Tanh, scale=0.5, bias=DVc("ha0", c, c + 1))
                        ts(a_, th, 0.5, MUL, 0.5, ADD)
                        pg_ = PS.get(4)
                        mm(pg_, smallw[:, 1024 + c * 128:1024 + (c + 1) * 128], sgl)
                        g_ = SC.f32(TT)
                        cp(g_, pg_, eng="act")
                        gT_.append(g_)
                        kq = SC.f32(TT)
                        ts(kq, kf[c], PVc("rw_kk", c, c + 1), MUL)
                        ksq = SC.bf(TT)
                        act(ksq, kq, AF.Square)
                        pk = PS.get(4)
                        mm(pk, bd1_bf, ksq)
                        rsqrt(th, pk, 1.0, eps12)
                        tt(kq, kq, th, MUL)
                        SC.free(ksq)
                        ts(th, a_, PVc("rw_ka", c, c + 1), MUL, DVc("omka", c, c + 1), ADD)
                        kp = SC.f32(TT)
                        tt(kp, kf[c], th, MUL)
                        tt(th, a_, kq, MUL)
                        SC.free(a_, kf[c])
                        rk = SC.bf(TT)
                        stt(rk, rf[c], PVc("rw_rk", c, c + 1), kp, MUL, MUL)
                        pbn = PS.get(4)
                        mm(pbn, bd1_bf, rk)
                        bo = SC.f32(TT)
                        tt(bo, pbn, vf[c], MUL)
                        bon.append(bo)
                        SC.free(rk)
                        cum = SC.f32(TT)
                        scan(cum, C("reset"), logw, 0.0)
                        wt = SC.f32(TT)
                        act(wt, cum, AF.Exp)
                        Wt.append(wt)
                        wi = SC.f32(TT)
                        act(wi, cum, AF.Exp, scale=-1.0)
                        tt(cum, cum, logw, SUB)
                        act(cum, cum, AF.Exp)
                        SC.free(logw)
                        art = SC.bf(2 * TT)
                        art4 = r4(art, 4, 2)
                        ARb.append(art)
                        tt(art4[:, :, 0, :], r3(kq, 4), r3(cum, 4), MUL)
                        tt(art4[:, :, 1, :], r3(rf[c], 4), r3(wt, 4), MUL)
                        ARt.append(art4)
                        kt_ = SC.bf(TT)
                        tt(kt_, kp, wi, MUL)
                        bt_ = SC.bf(TT)
                        tt(bt_, th, wi, MUL)
                        Ktl.append(kt_)
                        Btl.append(bt_)
                        v_ = SC.bf(TT)
                        cp(v_, vf[c], eng="act")
                        vb.append(v_)
                        SC.free(th, kq, kp, cum, wi, rf[c], vf[c])
                    SC.free(tw, al, sgl)
                    Vt = SC.bf(4 * 256)
                    Vt3 = r3(Vt, 4)
                    Ktok = SC.bf(4 * 256)
                    Ktok3 = r3(Ktok, 4)
                    Btok = SC.bf(4 * 256)
                    Btok3 = r3(Btok, 4)
                    for tc in range(4):
                        tcs = slice(tc * 128, (tc + 1) * 128)
                        for src, dst, pad in ((vb, Vt3, True), (Ktl, Ktok3, False), (Btl, Btok3, False)):
                            pt = PS.get(1, BF16)
                            for c in range(2):
                                tpose(pt[:, c * 128:(c + 1) * 128], src[c][:, tcs], ident_bf)
                            cp(dst[:, tc, :], pt, eng="act")
                            if pad:
                                pt4 = r4(dst[:, tc, :], 2, 2)
                                for hh in range(2):
                                    ts(VpadV[:, tc, :, hh, hh * 64:(hh + 1) * 64], pt4[:, :, hh, :], 1.0, MUL)
                    SC.free(*vb)
                    uset = 0
                    for tc in range(4):
                        tcs = slice(tc * 128, (tc + 1) * 128)
                        for c in range(2):
                            Mxs = []
                            up = Upad[uset % 2]
                            uset += 1
                            for hh in range(2):
                                h = 2 * c + hh
                                r0 = hh * 64
                                arh = ARt[c][r0:r0 + 64, tc].v(lambda a: a.rearrange("p a b -> p (a b)"))
                                pM = PS.get(4)
                                mm(pM[:, 0:256], Btl[c][r0:r0 + 64, tcs], arh)
                                mm(pM[:, 256:512], Ktl[c][r0:r0 + 64, tcs], arh)
                                pL = PS.get(1)
                                mm(pL, ARt[c][r0:r0 + 64, tc, 0, :], Btl[c][r0:r0 + 64, tcs])
                                Mx = SC.bf(TT)
                                tt(Mx, pM, C("maskM"), MUL)
                                Lc = SC.bf(128)
                                tt(Lc, pL, C("mlow"), MUL)
                                Pm = SC.bf(128)
                                tt(Pm, ident_bf, Mx[:, 0:128], SUB)
                                Ac = Mx[:, 0:128]
                                Ac_own = None
                                for lev in range(1, 7):
                                    pL2 = PS.get(1)
                                    mm(pL2, Ac, Lc)
                                    Ln_ = SC.bf(128)
                                    cp(Ln_, pL2, eng="act")
                                    An_ = None
                                    if lev < 6:
                                        pA2 = PS.get(1)
                                        mm(pA2, Lc, Ac)
                                        An_ = SC.bf(128)
                                        cp(An_, pA2)
                                    pP = PS.get(1)
                                    mm(pP, Ln_, Pm)
                                    Pn = SC.bf(128)
                                    tt(Pn, pP, Pm, ADD)
                                    SC.free(Pm, Lc)
                                    if Ac_own is not None:
                                        SC.free(Ac_own)
                                    Pm, Lc, Ac, Ac_own = Pn, Ln_, An_, An_
                                SC.free(Lc)
                                pX = PS.get(1)[:, 0:64]
                                mm(pX, ARt[c][:, tc, 0, :], SC_bf[c][:, r0:r0 + 64], start=True, stop=False)
                                mm(pX, Mx[:, 256:384], Vt3[:, tc, h * 64:(h + 1) * 64], start=False, stop=True)
                                Xb = SC.bf(64)
                                cp(Xb, pX, eng="act")
                                pU = PS.get(1)[:, 0:64]
                                mm(pU, Pm, Xb)
                                act(up[hh][:, r0:r0 + 64], pU, AF.Copy, scale=-1.0)
                                SC.free(Pm, Xb)
                                Mxs.append(Mx)
                            ycol = yb[c][:, tcs]
                            mm(ycol, SC_bf[c], ARt[c][:, tc, 1, :], start=True, stop=False)
                            for hh in range(2):
                                h = 2 * c + hh
                                mm(ycol, Vpad[:, tc, h, :], Mxs[hh][:, 384:512], start=False, stop=False)
                                mm(ycol, up[hh], Mxs[hh][:, 128:256], start=False, stop=(hh == 1))
                            pS = PS.get(1)
                            kc_ = Ktok3[:, tc, c * 128:(c + 1) * 128]
                            bc_ = Btok3[:, tc, c * 128:(c + 1) * 128]
                            mm(pS, kc_, Vpad[:, tc, 2 * c, :], start=True, stop=False)
                            mm(pS, kc_, Vpad[:, tc, 2 * c + 1, :], start=False, stop=False)
                            mm(pS, bc_, up[0], start=False, stop=False)
                            mm(pS, bc_, up[1], start=False, stop=True)
                            wc = Wt[c][:, tc * 128 + 127:tc * 128 + 128]
                            tmp = SC.f32(128)
                            stt(tmp, pS, wc, C("bd"), MUL, MUL)
                            stt(SC_f[c], SC_f[c], wc, tmp, MUL, ADD)
                            cp(SC_bf[c], SC_f[c], eng="act")
                            SC.free(tmp, *Mxs)
                    for c in range(2):
                        yc = group_norm(yb[c], eps64, PVc("rw_gw", c, c + 1), PVc("rw_gb", c, c + 1))
                        tt(yc, yc, bon[c], ADD)
                        tt(yT[:, 4 + c, :], yc, gT_[c], MUL)
                        SC.free(yc)
                    for lst in (gT_, bon, Wt, Ktl, Btl):
                        SC.free(*lst)
                    SC.free(Vt, Ktok, Btok)
                    SC.free(*ARb)

                slot5 = next_slot()
                slot6 = next_slot()
                slot7 = next_slot()
                if not EN("D"):
                    release()
                    release()
                    release()
                if EN("D"):
                    sz = []
                    for c in range(2):
                        bk = proj(slot5, c * 128)
                        tg_ = SC.f32(TT)
                        act(tg_, bk, AF.Tanh, scale=0.5)
                        s = SC.f32(TT)
                        stt(s, tg_, 1.0, bk, ADD, MUL)
                        SC.free(tg_)
                        sz.append(s)

                    def convD(idx, bk):
                        cv = conv_stage(bk, histD[idx], idx, lambda k: DVc("sdcwh", k * 6 + idx, k * 6 + idx + 1),
                                        DVc("sdcbh", idx, idx + 1))
                        th = SC.f32(TT)
                        act(th, cv, AF.Tanh)
                        return cv, th

                    xsf, xsb, BT_, CT_ = [], [], [], []
                    for c in range(2):
                        cv, th = convD(c, proj(slot5, 256 + c * 128))
                        stt(cv, th, 1.0, cv, ADD, MUL)
                        b_ = SC.bf(TT)
                        cp(b_, cv, eng="act")
                        xsf.append(cv)
                        xsb.append(b_)
                        SC.free(th)
                    release()
                    for g in range(2):
                        cv, th = convD(2 + g, proj(slot6, g * 128))
                        b_ = SC.bf(TT)
                        stt(b_, th, 1.0, cv, ADD, MUL)
                        BT_.append(b_)
                        SC.free(cv, th)
                    for g in range(2):
                        cv, th = convD(4 + g, proj(slot6, 256 + g * 128))
                        b_ = SC.bf(TT)
                        stt(b_, th, 1.0, cv, ADD, MUL)
                        CT_.append(b_)
                        SC.free(cv, th)
                    release()
                    pdt = PS.get(1)
                    for tc in range(4):
                        for k in range(8):
                            mm(pdt[:, tc * 4:(tc + 1) * 4], hT[:, k, tc * 128:(tc + 1) * 128], slot7[:, k, 0:4],
                               start=(k == 0), stop=(k == 7))
                    release()
                    dt = SC.f32(16)
                    tt(r3(dt, 4), r3(pdt[:, 0:16], 4), bc_mid(PVc("sd_dtb"), 4), ADD)
                    act(dt, dt, AF.Exp)
                    act(dt, dt, AF.Ln, scale=1.0, bias=one_c)
                    la = SC.f32(16)
                    tt(r3(la, 4), r3(dt, 4), bc_mid(DVc("aneg"), 4), MUL)
                    pc = PS.get(1)
                    mm(pc[:, 0:16], C("tri"), la)
                    cum = SC.f32(16)
                    cp(cum, pc[:, 0:16], eng="act")
                    Btok = SC.bf(4 * 256)
                    Btok3 = r3(Btok, 4)
                    for tc in range(4):
                        tcs = slice(tc * 128, (tc + 1) * 128)
                        h4 = slice(tc * 4, (tc + 1) * 4)
                        px = PS.get(1, BF16)
                        for c in range(2):
                            tpose(px[:, c * 128:(c + 1) * 128], xsb[c][:, tcs], ident_bf)
                        pb = PS.get(1, BF16)
                        for g in range(2):
                            tpose(pb[:, g * 128:(g + 1) * 128], BT_[g][:, tcs], ident_bf)
                        cp(Btok3[:, tc, :], pb, eng="act")
                        lat = SC.f32(TT)
                        tt(r3(lat, 4), tri_b4, bc_last(la[:, h4], 128), MUL)
                        pcb = PS.get(4)
                        mm(pcb, ones_f, lat)
                        dif = SC.f32(TT)
                        tt(r3(dif, 4), r3(pcb, 4), bc_last(cum[:, h4], 128), SUB)
                        tt(r3(dif, 4), r3(dif, 4), mbias_b4, ADD)
                        act(dif, dif, AF.Exp)
                        E4 = SC.f32(TT)
                        act(E4, pcb, AF.Exp)
                        E43 = r3(E4, 4)
                        we = SC.f32(4)
                        tt(we, r3(pcb, 4)[:, :, 127], cum[:, h4], SUB)
                        act(we, we, AF.Exp)
                        tt(we, we, dt[:, h4], MUL)
                        px4 = r4(px, 2, 2)
                        dt22 = r3(dt[:, h4], 2)
                        for hh in range(2):
                            tt(VpadV[:, tc, :, hh, hh * 64:(hh + 1) * 64], px4[:, :, hh, :],
                               bc_last(dt22[:, :, hh], 64), MUL)
                        vw = SC.bf(256)
                        tt(r3(vw, 4), r3(px, 4), bc_last(we, 64), MUL)
                        psc = PS.get(2)
                        for g in range(2):
                            mm(psc[:, g * 128:(g + 1) * 128], BT_[g][:, tcs], CT_[g][:, tcs])
                        sm = SC.bf(TT)
                        dif4 = r4(dif, 2, 2)
                        sm4 = r4(sm, 2, 2)
                        Cs = SC.bf(TT)
                        Cs4 = r4(Cs, 2, 2)
                        E44 = r4(E4, 2, 2)
                        for g in range(2):
                            tt(sm4[:, g], bc_mid(psc[:, g * 128:(g + 1) * 128], 2), dif4[:, g], MUL)
                            tt(Cs4[:, g], bc_mid(CT_[g][:, tcs], 2), E44[:, g], MUL)
                        for g in range(2):
                            ycol = yb[g][:, tcs]
                            for hh in range(2):
                                h = 2 * g + hh
                                mm(ycol, SpadD[g][hh], Cs[:, h * 128:(h + 1) * 128], start=(hh == 0), stop=False)
                                mm(ycol, Vpad[:, tc, h, :], sm[:, h * 128:(h + 1) * 128], start=False, stop=(hh == 1))
                            pS = PS.get(1)
                            mm(pS, Btok3[:, tc, g * 128:(g + 1) * 128], vw[:, g * 128:(g + 1) * 128])
                            sd3 = r3(SD_f[g], 2)
                            tt(sd3, sd3, bc_last(E43[:, 2 * g:2 * g + 2, 127], 64), MUL)
                            tt(SD_f[g], SD_f[g], pS, ADD)
                            for hh in range(2):
                                cp(SpadD[g][hh][:, hh * 64:(hh + 1) * 64], SD_f[g][:, hh * 64:(hh + 1) * 64], eng="act")
                        SC.free(lat, dif, E4, we, vw, sm, Cs)
                    for g in range(2):
                        y1 = SC.f32(TT)
                        stt(y1, xsf[g], PVc("sd_d", g, g + 1), yb[g], MUL, ADD)
                        stt(y1, y1, 0.5, sz[g], MUL, MUL)
                        ysq = SC.bf(TT)
                        act(ysq, y1, AF.Square)
                        pm = PS.get(4)
                        mm(pm, ones128_bf, ysq)
                        r_ = SC.f32(TT)
                        rsqrt(r_, pm, 1.0, eps6)
                        stt(yT[:, 6 + g, :], y1, PVc("sd_nw", g, g + 1), r_, MUL, MUL)
                        SC.free(y1, ysq, r_)
                    SC.free(dt, la, cum, Btok, *sz, *xsf, *xsb, *BT_, *CT_)

                so = [next_slot(), next_slot()]
                for d in range(8):
                    bk = PS.get(4)
                    w = so[d // 4]
                    for k in range(8):
                        mm(bk, w[:, k, (d % 4) * 128:(d % 4 + 1) * 128], yT[:, k, :], start=(k == 0), stop=(k == 7))
                    tt(xT[:, d, t0:t0 + TT], xT[:, d, t0:t0 + TT], bk, ADD)
                    if d % 4 == 3:
                        release()
            dbg("yT", yT)
            PHASE1["scratch_peak"] = max(PHASE1.get("scratch_peak", 0), SC.peak)
            PHASE1["scratch_n"] = SC.n
            Pg.barrier()
            AR.reset(m)


        for sq in range(NSEQ):
            phase0(sq)
            for l in range(L):
                layer_setup(l)
                if EN("mix"):
                    phase_mix(l)
                if EN("attn"):
                    phase_attn(l)
                if EN("ffn"):
                    phase_ffn(l)
            phase_final(sq)
        Pg.wait_all_on("sp")
        build.info = dict(n_ops=Pg.n_ops, arena_peak=AR.peak, arena_n=AR.n, **PHASE1)

        with nc.Block() as block:
            @block.tensor
            def _(e):
                Pg.replay("pe", e)

            @block.scalar
            def _(e):
                Pg.replay("act", e)

            @block.vector
            def _(e):
                Pg.replay("dve", e)

            @block.gpsimd
            def _(e):
                Pg.replay("pool", e)

            @block.sync
            def _(e):
                Pg.replay("sp", e)
    return nc


def make_in_maps(inputs, L, S, nseq_per_core, n_cores):
    f = lambda a: np.ascontiguousarray(np.asarray(a, dtype=np.float32))
    shared = {
        "cb": make_consts(f(inputs["norm_final"])),
        "rope": make_rope(S),
        "pv": make_pvec(inputs, L),
        "smallw": make_smallw(inputs, L),
        "w_in": f(inputs["w_in"]),
        "w_out": f(inputs["w_out"]),
        "mem_wq": f(inputs["mem_wq"]),
        "mem_wk": f(inputs["mem_wk"]),
        "mem_wv": f(inputs["mem_wv"]),
        "mem_wo": f(inputs["mem_wo"]),
        "ffn_w_in": f(inputs["ffn_w_in"]),
        "ffn_w_out": f(inputs["ffn_w_out"]),
    }
    x = f(inputs["x"])
    mem = f(inputs["mem"])
    maps = []
    for c in range(n_cores):
        b0 = c * nseq_per_core
        m = dict(shared)
        m["x"] = np.ascontiguousarray(x[b0:b0 + nseq_per_core].reshape(nseq_per_core * S, D))
        m["mem"] = np.ascontiguousarray(mem[b0:b0 + nseq_per_core].reshape(nseq_per_core * MEMT, D))
        maps.append(m)
    return maps


def kernel(**inputs):
    x = np.asarray(inputs["x"])
    B, S, _ = x.shape
    L = np.asarray(inputs["w_in"]).shape[0]
    n_cores = N_CORES if B % N_CORES == 0 else 1
    nseq = B // n_cores
    nc = build(L, S, nseq)
    maps = make_in_maps(inputs, L, S, nseq, n_cores)
    res = run_bass_kernel_spmd(nc, maps, core_ids=list(range(n_cores)))
    outs = [np.asarray(r["out"]).reshape(nseq, S, D) for r in res.results]
    return np.concatenate(outs, axis=0).astype(np.float32)
```

```python
import math
import numpy as np
import concourse.bass as bass
import concourse.mybir as mybir
from concourse.bass_utils import run_bass_kernel_spmd

F32 = mybir.dt.float32
BF16 = mybir.dt.bfloat16
AF = mybir.ActivationFunctionType
ALU = mybir.AluOpType

D = 1024
KC = 8
TT = 512
IN_COLS = 3588
FFH = 2816
NJ = FFH // 128
MEMT = 256
N_CORES = 8
SYNC_MODE = "old"


class Buf:
    __slots__ = ("w", "r", "gen")

    def __init__(self):
        self.w = None
        self.r = {}
        self.gen = 0


class T:
    __slots__ = ("ap", "bufs", "gens")

    def __init__(self, ap, bufs):
        self.ap = ap
        self.bufs = bufs
        self.gens = [b.gen for b in bufs]

    def __getitem__(self, k):
        t = T.__new__(T)
        t.ap = self.ap[k]
        t.bufs = self.bufs
        t.gens = self.gens
        return t

    def v(self, fn):
        t = T.__new__(T)
        t.ap = fn(self.ap)
        t.bufs = self.bufs
        t.gens = self.gens
        return t

    def check(self):
        for b, g in zip(self.bufs, self.gens):
            assert b.gen == g, "stale PSUM tile used"


class Prog:
    def __init__(self, nc, sems):
        self.nc = nc
        self.E = {}
        for name in ("pe", "act", "dve", "pool", "sp"):
            self.E[name] = dict(ops=[], sem=sems[name], cnt=0, seen={})
        self.dq = {"sp": dict(slots=[[s, 0] for s in sems["dsp"]], i=0),
                   "pool": dict(slots=[[s, 0] for s in sems["dpool"]], i=0)}
        self.n_ops = 0

    def _waits(self, E, eng, reads, writes):
        waits = {}
        own = E["sem"]

        def need(tok, kind):
            if tok is None:
                return
            sem, val = tok
            if sem is own:
                if eng == "pe" or (SYNC_MODE == "old" and kind != "raw"):
                    return
            if E["seen"].get(sem, 0) >= val:
                return
            if waits.get(sem, 0) < val:
                waits[sem] = val

        for t in reads:
            t.check()
            for b in t.bufs:
                need(b.w, "raw")
        for t in writes:
            t.check()
            for b in t.bufs:
                need(b.w, "waw")
                for s, v in b.r.items():
                    need((s, v), "war")
        for s, v in waits.items():
            E["seen"][s] = v
        return list(waits.items())

    def op(self, eng, fn, reads=(), writes=()):
        E = self.E[eng]
        waits = self._waits(E, eng, reads, writes)
        E["cnt"] += 1
        c = E["cnt"]
        sem = E["sem"]
        E["ops"].append((waits, fn, sem, 1))
        for t in reads:
            for b in t.bufs:
                if b.r.get(sem, 0) < c:
                    b.r[sem] = c
        for t in writes:
            for b in t.bufs:
                b.w = (sem, c)
                b.r = {}
        self.n_ops += 1

    def dma(self, q, out, in_, reads=(), writes=()):
        E = self.E[q]
        dq = self.dq[q]
        slot = dq["slots"][dq["i"] % len(dq["slots"])]
        dq["i"] += 1
        waits = dict(self._waits(E, q, reads, writes))
        sem, val = slot
        if val > 0 and E["seen"].get(sem, 0) < val:
            waits[sem] = max(waits.get(sem, 0), val)
            E["seen"][sem] = val
        slot[1] = val + 16
        nv = slot[1]
        E["ops"].append((list(waits.items()), lambda e: e.dma_start(out=out, in_=in_), sem, 16))
        for t in reads:
            for b in t.bufs:
                b.r[sem] = nv
        for t in writes:
            for b in t.bufs:
                b.w = (sem, nv)
                b.r = {}
        self.n_ops += 1
        return (sem, nv)

    def barrier(self):
        allt = {}
        for n, E in self.E.items():
            if E["cnt"] > 0:
                allt[E["sem"]] = E["cnt"]
        for q in self.dq.values():
            for s, v in q["slots"]:
                if v > 0:
                    allt[s] = v
        for n, E in self.E.items():
            w = []
            for s, v in allt.items():
                if s is E["sem"]:
                    continue
                if E["seen"].get(s, 0) < v:
                    w.append((s, v))
                    E["seen"][s] = v
            if w:
                E["ops"].append((w, None, None, 0))

    def wait_all_on(self, eng):
        E = self.E[eng]
        w = []
        for n, E2 in self.E.items():
            if E2 is not E and E2["cnt"] > 0:
                w.append((E2["sem"], E2["cnt"]))
        for q in self.dq.values():
            for s, v in q["slots"]:
                if v > 0:
                    w.append((s, v))
        E["ops"].append((w, None, None, 0))

    def replay(self, name, e):
        for waits, fn, sem, inc in self.E[name]["ops"]:
            for s, v in waits:
                e.wait_ge(s, v)
            if fn is not None:
                fn(e).then_inc(sem, inc)


class Arena:
    def __init__(self, handle, nwords):
        self.h = handle
        self.n = nwords
        self.top = 0
        self.peak = 0

    def tile(self, free_shape, dtype=F32, parts=128):
        n = 1
        for s in free_shape:
            n *= s
        words = n if dtype == F32 else (n + 1) // 2
        words = (words + 15) // 16 * 16
        off = self.top
        self.top += words
        self.peak = max(self.peak, self.top)
        assert self.top <= self.n, f"SBUF arena overflow {self.top} > {self.n}"
        ap = self.h[:, off:off + (n if dtype == F32 else (n + 1) // 2)]
        if dtype != F32:
            ap = ap.bitcast(dtype)
            if ap.shape[1] != n:
                ap = ap[:, 0:n]
        if len(free_shape) == 2:
            ap = ap.rearrange("p (a b) -> p a b", a=free_shape[0])
        elif len(free_shape) == 3:
            ap = ap.rearrange("p (a b c) -> p a b c", a=free_shape[0], b=free_shape[1])
        elif len(free_shape) == 4:
            ap = ap.rearrange("p (a b c d) -> p a b c d", a=free_shape[0], b=free_shape[1], c=free_shape[2])
        if parts != 128:
            ap = ap[0:parts]
        return T(ap, [Buf()])

    def mark(self):
        return self.top

    def reset(self, m):
        self.top = m


class Psum:
    def __init__(self, banks):
        self.banks = banks
        self.b = [Buf() for _ in range(8)]
        self.rot = list(range(8))
        self.ptr = 0

    def set_rot(self, banks):
        self.rot = list(banks)
        self.ptr = 0

    def _mk(self, b, nq, dtype):
        self.b[b].gen += 1
        ap = self.banks[b][:, 0:nq * 128]
        if dtype != F32:
            ap = ap.bitcast(dtype)
        return T(ap, [self.b[b]])

    def get(self, nq=4, dtype=F32):
        b = self.rot[self.ptr % len(self.rot)]
        self.ptr += 1
        return self._mk(b, nq, dtype)

    def fixed(self, b, dtype=F32):
        return self._mk(b, 4, dtype)


PV = {}


def _pv_layout():
    if PV:
        return PV["_n"]
    c = 0
    for name, n in [("g_mix", 8), ("lru_cw", 8), ("lru_cb", 2), ("lru_br", 2), ("lru_bi", 2), ("lru_lam", 2),
                    ("ret_gw", 2), ("ret_gb", 2), ("rw_mu", 8), ("rw_w0", 2), ("rw_a0", 2), ("rw_kk", 2),
                    ("rw_ka", 2), ("rw_rk", 2), ("rw_gw", 2), ("rw_gb", 2), ("sd_cw", 24), ("sd_cb", 6),
                    ("sd_d", 2), ("sd_nw", 2), ("g_memq", 8), ("g_memkv", 8), ("g_ffn", 8),
                    ("sd_dtb", 4), ("sd_alog", 4)]:
        PV[name] = (c, n)
        c += n
    PV["_n"] = c
    return c


CC = {}


def _cc_layout():
    if CC:
        return CC["_n"]
    c = 0
    for name, n in [("ident", 128), ("tri", 128), ("mstrT", 128), ("mlow", 128), ("maskM", 512), ("mbias", 128),
                    ("bd", 128), ("swap", 128), ("reset", 512), ("g128", 2), ("eps6", 1), ("eps5", 1),
                    ("eps64", 1), ("eps12", 1), ("one", 1), ("g_final", 8)]:
        CC[name] = (c, n)
        c += n
    CC["_n"] = c
    return c


def _chan(v, n):
    return np.ascontiguousarray(np.asarray(v, np.float32).reshape(n, 128).T)


def make_consts(norm_final):
    n = _cc_layout()
    cb = np.zeros((128, n), np.float32)

    def put(name, arr):
        c0, k = CC[name]
        cb[:, c0:c0 + k] = arr

    i = np.arange(128)
    put("ident", np.eye(128, dtype=np.float32))
    tri = (i[:, None] <= i[None, :]).astype(np.float32)
    mstr = (i[:, None] < i[None, :]).astype(np.float32)
    put("tri", tri)
    put("mstrT", mstr)
    put("mlow", mstr.T)
    put("maskM", np.concatenate([mstr, tri, mstr, tri], axis=1))
    put("mbias", np.where(i[:, None] <= i[None, :], 0.0, -30000.0).astype(np.float32))
    bd = ((i[:, None] // 64) == (i[None, :] // 64)).astype(np.float32)
    put("bd", bd)
    partner = np.where(i % 64 < 32, i + 32, i - 32)
    sw = np.zeros((128, 128), np.float32)
    sw[partner, i] = 1.0
    put("swap", sw)
    rs = np.ones((128, 512), np.float32)
    rs[:, ::128] = 0.0
    put("reset", rs)
    gam = 1.0 - np.exp2(-5.0 - np.arange(4, dtype=np.float64))
    g128 = np.zeros((128, 2), np.float32)
    for c in range(2):
        for p in range(128):
            g128[p, c] = gam[2 * c + p // 64] ** 128
    put("g128", g128)
    put("eps6", 1e-6)
    put("eps5", 1e-5)
    put("eps64", 64e-5)
    put("eps12", 1e-12)
    put("one", 1.0)
    put("g_final", _chan(norm_final, 8))
    return cb


def make_rope(S):
    nt = S // TT
    pos = np.arange(S, dtype=np.float32)
    inv_freq = (10000.0 ** (-np.arange(32, dtype=np.float32) / 32.0)).astype(np.float32)
    ang = (pos[:, None] * inv_freq[None, :]).astype(np.float32)
    cos = np.cos(ang).astype(np.float64)
    sin = np.sin(ang).astype(np.float64)
    gam = 1.0 - np.exp2(-5.0 - np.arange(4, dtype=np.float64))
    tl = (np.arange(S) % 128) + 1
    out = np.zeros((nt, 128, 8, TT), np.float32)
    p = np.arange(128)
    n = p % 64
    j = n % 32
    sgn = np.where(n < 32, -1.0, 1.0)
    for c in range(2):
        h = 2 * c + p // 64
        gq = gam[h][:, None] ** tl[None, :]
        gk = gam[h][:, None] ** (-tl[None, :]) / 8.0
        cq = cos[:, j].T * gq
        sq = sin[:, j].T * sgn[:, None] * gq
        ck = cos[:, j].T * gk
        sk = sin[:, j].T * sgn[:, None] * gk
        for t in range(nt):
            sl = slice(t * TT, (t + 1) * TT)
            out[t, :, 0 + 2 * c, :] = cq[:, sl]
            out[t, :, 1 + 2 * c, :] = sq[:, sl]
            out[t, :, 4 + 2 * c, :] = ck[:, sl]
            out[t, :, 5 + 2 * c, :] = sk[:, sl]
    return out


def make_pvec(inp, L):
    n = _pv_layout()
    pv = np.zeros((L, 128, n), np.float32)

    def put(l, name, arr):
        c0, k = PV[name]
        pv[l, :, c0:c0 + k] = arr

    for l in range(L):
        put(l, "g_mix", _chan(inp["norm_mix"][l], 8))
        put(l, "lru_cw", np.concatenate([_chan(inp["lru_conv_w"][l][k], 2) for k in range(4)], axis=1))
        put(l, "lru_cb", _chan(inp["lru_conv_b"][l], 2))
        put(l, "lru_br", _chan(inp["lru_b_r"][l].reshape(-1), 2))
        put(l, "lru_bi", _chan(inp["lru_b_i"][l].reshape(-1), 2))
        put(l, "lru_lam", _chan(inp["lru_lambda"][l], 2))
        put(l, "ret_gw", _chan(inp["ret_gn_w"][l], 2))
        put(l, "ret_gb", _chan(inp["ret_gn_b"][l], 2))
        put(l, "rw_mu", _chan(inp["rwkv_mu"][l], 8))
        put(l, "rw_w0", _chan(inp["rwkv_w0"][l], 2))
        put(l, "rw_a0", _chan(inp["rwkv_a0"][l], 2))
        put(l, "rw_kk", _chan(inp["rwkv_k_k"][l], 2))
        put(l, "rw_ka", _chan(inp["rwkv_k_a"][l], 2))
        put(l, "rw_rk", _chan(inp["rwkv_r_k"][l], 2))
        put(l, "rw_gw", _chan(inp["rwkv_gn_w"][l], 2))
        put(l, "rw_gb", _chan(inp["rwkv_gn_b"][l], 2))
        put(l, "sd_cw", np.concatenate([_chan(inp["ssd_conv_w"][l][k], 6) for k in range(4)], axis=1))
        put(l, "sd_cb", _chan(inp["ssd_conv_b"][l], 6))
        put(l, "sd_d", _chan(np.repeat(np.asarray(inp["ssd_d"][l], np.float32), 64), 2))
        put(l, "sd_nw", _chan(inp["ssd_norm_w"][l], 2))
        put(l, "g_memq", _chan(inp["norm_mem_q"][l], 8))
        put(l, "g_memkv", _chan(inp["norm_mem_kv"][l], 8))
        put(l, "g_ffn", _chan(inp["norm_ffn"][l], 8))
        put(l, "sd_dtb", np.broadcast_to(np.asarray(inp["ssd_dt_bias"][l], np.float32)[None, :], (128, 4)))
        put(l, "sd_alog", np.broadcast_to(np.asarray(inp["ssd_a_log"][l], np.float32)[None, :], (128, 4)))
    return pv


def make_smallw(inp, L):
    sw = np.zeros((L, 128, 1280), np.float32)
    for l in range(L):
        for c in range(2):
            for hh in range(2):
                blk = slice(hh * 64, hh * 64 + 64)
                sw[l, blk, c * 128 + hh * 64: c * 128 + hh * 64 + 64] = inp["lru_w_r"][l][2 * c + hh]
                sw[l, blk, 256 + c * 128 + hh * 64: 256 + c * 128 + hh * 64 + 64] = inp["lru_w_i"][l][2 * c + hh]
        sw[l, 0:64, 512:768] = inp["rwkv_w2"][l]
        sw[l, 64:128, 768:1024] = inp["rwkv_a2"][l]
        sw[l, :, 1024:1280] = inp["rwkv_g2"][l]
    return sw


def build(L, S, NSEQ, flags=None):
    from contextlib import ExitStack
    flags = flags or {}
    EN = lambda k: flags.get(k, True)
    NT = S // TT
    NCHS = S // 128
    GT = 2 if NT % 2 == 0 else 1
    nc = bass.Bass("TRN2", target_bir_lowering=False)
    NPV = _pv_layout()
    NCC = _cc_layout()

    def dram(name, shape, kind="ExternalInput"):
        return nc.dram_tensor(name, shape, F32, kind=kind).ap()

    x_d = dram("x", [NSEQ * S, D])
    mem_d = dram("mem", [NSEQ * MEMT, D])
    cb_d = dram("cb", [128, NCC])
    rope_d = dram("rope", [NT, 128, 8, TT])
    pv_d = dram("pv", [L, 128, NPV])
    sw_d = dram("smallw", [L, 128, 1280])
    win_d = dram("w_in", [L, D, IN_COLS])
    wout_d = dram("w_out", [L, D, D])
    wq_d = dram("mem_wq", [L, D, D])
    wk_d = dram("mem_wk", [L, D, D])
    wv_d = dram("mem_wv", [L, D, D])
    wo_d = dram("mem_wo", [L, D, D])
    f1_d = dram("ffn_w_in", [L, D, 2 * FFH])
    f2_d = dram("ffn_w_out", [L, FFH, D])
    out_d = dram("out", [NSEQ * S, D], kind="ExternalOutput")

    NW = 52224
    with ExitStack() as es:
        arena_h = es.enter_context(nc.sbuf_tensor("arena", [128, NW], F32))
        banks = [es.enter_context(nc.psum_tensor(f"bank{i}", [128, 512], F32)) for i in range(8)]
        sems = {}
        for n in ("pe", "act", "dve", "pool", "sp"):
            sems[n] = es.enter_context(nc.semaphore("s_" + n))
        sems["dsp"] = [es.enter_context(nc.semaphore(f"dsp{i}")) for i in range(12)]
        sems["dpool"] = [es.enter_context(nc.semaphore(f"dpl{i}")) for i in range(12)]
        Pg = Prog(nc, sems)
        AR = Arena(arena_h, NW)
        PS = Psum([b[:, :] for b in banks])

        def _a(x):
            return x.ap if isinstance(x, T) else x

        def _rd(*xs):
            return [x for x in xs if isinstance(x, T)]

        def mm(out, lhsT, rhs, start=True, stop=True):
            Pg.op("pe", lambda e: e.matmul(out.ap, lhsT.ap, rhs.ap, start=start, stop=stop), [lhsT, rhs], [out])

        def tpose(out, in_, ident):
            Pg.op("pe", lambda e: e.transpose(out.ap, in_.ap, ident.ap), [in_, ident], [out])

        def act(out, in_, func, scale=1.0, bias=0.0):
            Pg.op("act", lambda e: e.activation(out=out.ap, in_=in_.ap, func=func, bias=_a(bias), scale=_a(scale)),
                  _rd(in_, scale, bias), [out])

        def tt(out, a, b, op, eng="dve"):
            Pg.op(eng, lambda e: e.tensor_tensor(out=out.ap, in0=a.ap, in1=b.ap, op=op), [a, b], [out])

        def ts(out, a, s1, op0, s2=None, op1=None, eng="dve"):
            kw = dict(out=out.ap, in0=a.ap, scalar1=_a(s1), scalar2=_a(s2), op0=op0)
            if op1 is not None:
                kw["op1"] = op1
            Pg.op(eng, lambda e: e.tensor_scalar(**kw), _rd(a, s1, s2), [out])

        def stt(out, a, s, b, op0, op1):
            Pg.op("dve", lambda e: e.scalar_tensor_tensor(out=out.ap, in0=a.ap, scalar=_a(s), in1=b.ap,
                                                          op0=op0, op1=op1), _rd(a, s, b), [out])

        def cp(out, a, eng="dve"):
            if eng == "act":
                Pg.op("act", lambda e: e.copy(out=out.ap, in_=a.ap), [a], [out])
            else:
                Pg.op(eng, lambda e: e.tensor_copy(out=out.ap, in_=a.ap), [a], [out])

        def scan(out, d0, d1, init):
            Pg.op("dve", lambda e: e.tensor_tensor_scan(out=out.ap, data0=d0.ap, data1=d1.ap, initial=_a(init),
                                                        op0=ALU.mult, op1=ALU.add), _rd(d0, d1, init), [out])

        def recip(out, a):
            Pg.op("dve", lambda e: e.reciprocal(out=out.ap, in_=a.ap), [a], [out])

        def mset(t, val, eng="dve"):
            Pg.op(eng, lambda e: e.memset(t.ap, val), [], [t])

        def wload(dst, src_ap):
            Pg.dma("pool", dst.ap, src_ap, writes=[dst])

        def ld(dst, src_ap):
            Pg.dma("sp", dst.ap, src_ap, writes=[dst])

        MUL, ADD, SUB = ALU.mult, ALU.add, ALU.subtract
        DBG = {}

        def dbg(name, t):
            if not flags.get("dbg") or name in DBG:
                return
            shp = list(t.ap.shape)
            d = nc.dram_tensor("dbg_" + name, shp, t.ap.dtype, kind="ExternalOutput").ap()
            DBG[name] = shp
            Pg.dma("sp", d, t.ap, reads=[t])

        xT = AR.tile([8, S])
        cb = AR.tile([NCC])
        pv = AR.tile([NPV])
        DVL = {}
        ndv = 0
        for name, n in [("hbr", 2), ("hbi", 2), ("cA", 2), ("hcA", 2), ("sdcwh", 24), ("sdcbh", 6), ("omm", 8),
                        ("hw0", 2), ("ha0", 2), ("omka", 2), ("aneg", 4), ("tmp", 8)]:
            DVL[name] = (ndv, n)
            ndv += n
        dv = AR.tile([ndv])
        smallw = AR.tile([1280], BF16)
        ident_bf = AR.tile([128], BF16)
        ones_bf = AR.tile([128], BF16)
        onesD_bf = AR.tile([128], BF16)
        ones128_bf = AR.tile([128], BF16)
        bd1_bf = AR.tile([128], BF16)
        bd64_bf = AR.tile([128], BF16)
        swap_bf = AR.tile([128], BF16)
        ones_f = AR.tile([128])
        memnT = AR.tile([8, MEMT], BF16)
        KT = AR.tile([8, MEMT], BF16)
        Vm = AR.tile([2, D], BF16)
        Vpad = AR.tile([4, 4, 128], BF16)
        Upad = [[AR.tile([128], BF16) for _ in range(2)] for _ in range(2)]
        SpadD = [[AR.tile([128], BF16) for _ in range(2)] for _ in range(2)]
        SD_f = [AR.tile([128]) for _ in range(2)]
        SB_f = [AR.tile([128]) for _ in range(2)]
        SB_bf = [AR.tile([128], BF16) for _ in range(2)]
        SC_f = [AR.tile([128]) for _ in range(2)]
        SC_bf = [AR.tile([128], BF16) for _ in range(2)]
        histA = [AR.tile([3]) for _ in range(2)]
        histD = [AR.tile([3]) for _ in range(6)]
        histC = [AR.tile([1]) for _ in range(8)]
        hprev = [AR.tile([1]) for _ in range(2)]
        stage = [AR.tile([3 + TT]) for _ in range(2)]
        PH0 = AR.mark()

        def C(name, a=None, b=None):
            c0, n = CC[name]
            if a is None:
                return cb[:, c0:c0 + n]
            return cb[:, c0 + a:c0 + b]

        def PVc(name, a=0, b=None):
            c0, n = PV[name]
            b = n if b is None else b
            return pv[:, c0 + a:c0 + b]

        def DVc(name, a=0, b=None):
            c0, n = DVL[name]
            b = n if b is None else b
            return dv[:, c0 + a:c0 + b]

        ident_f = C("ident")
        eps6, eps5, eps64, eps12, one_c = C("eps6"), C("eps5"), C("eps64"), C("eps12"), C("one")

        ld(cb, cb_d[:, :])
        cp(ident_bf, C("ident"))
        mset(ones_bf, 1.0)
        mset(onesD_bf, 1.0 / 1024.0)
        mset(ones128_bf, 1.0 / 128.0)
        mset(ones_f, 1.0)
        cp(bd1_bf, C("bd"))
        ts(bd64_bf, C("bd"), 1.0 / 64.0, MUL)
        cp(swap_bf, C("swap"))
        mset(Vpad, 0.0)
        for a_ in Upad + SpadD:
            for b_ in a_:
                mset(b_, 0.0)

        def rsqrt(out, in_, scale, eps_t):
            act(out, in_, AF.Ln, scale=scale, bias=eps_t)
            act(out, out, AF.Exp, scale=-0.5)

        def rmsnorm_tile(xsl, gcols, hT, sqt, rstd, n=TT):
            bank = PS.get(4)
            bk = bank[:, 0:n]
            for k in range(8):
                sq = sqt[k % 2]
                act(sq, xsl[:, k, :], AF.Square)
                mm(bk, onesD_bf, sq, start=(k == 0), stop=(k == 7))
            rsqrt(rstd, bk, 1.0, eps6)
            for k in range(8):
                if gcols is None:
                    tt(hT[:, k, :], xsl[:, k, :], rstd, MUL)
                else:
                    stt(hT[:, k, :], xsl[:, k, :], gcols[:, k:k + 1], rstd, MUL, MUL)

        def wsrc(w_d, l, r0, kc, c0, n):
            return w_d[l, r0:r0 + kc * 128, c0:c0 + n].rearrange("(k p) c -> p k c", p=128)

        def v4(t):
            return t.v(lambda a: a.rearrange("p (a b) -> p a b", a=4))

        def phase0(sq):
            m = AR.mark()
            PS.set_rot(range(8))
            xst = [AR.tile([D]) for _ in range(2)]
            memT = AR.tile([8, MEMT])
            sqt = [AR.tile([MEMT], BF16) for _ in range(2)]
            rstd = AR.tile([MEMT])
            for c in range(NCHS):
                st = xst[c % 2]
                ld(st, x_d[sq * S + c * 128: sq * S + (c + 1) * 128, :])
                for half in range(2):
                    bk = PS.get(4)
                    for q in range(4):
                        k = half * 4 + q
                        tpose(bk[:, q * 128:(q + 1) * 128], st[:, k * 128:(k + 1) * 128], ident_f)
                    cp(xT[:, half * 4:(half + 1) * 4, c * 128:(c + 1) * 128], v4(bk), eng=("act" if half else "dve"))
            for mc in range(2):
                st = xst[mc % 2]
                ld(st, mem_d[sq * MEMT + mc * 128: sq * MEMT + (mc + 1) * 128, :])
                for half in range(2):
                    bk = PS.get(4)
                    for q in range(4):
                        k = half * 4 + q
                        tpose(bk[:, q * 128:(q + 1) * 128], st[:, k * 128:(k + 1) * 128], ident_f)
                    cp(memT[:, half * 4:(half + 1) * 4, mc * 128:(mc + 1) * 128], v4(bk), eng=("act" if half else "dve"))
            rmsnorm_tile(memT, None, memnT, sqt, rstd, n=MEMT)
            Pg.barrier()
            AR.reset(m)

        def layer_setup(l):
            ld(pv, pv_d[l])
            wload(smallw, sw_d[l])
            ts(DVc("hbr"), PVc("lru_br"), 0.5, MUL)
            ts(DVc("hbi"), PVc("lru_bi"), 0.5, MUL)
            act(DVc("tmp", 0, 2), PVc("lru_lam"), AF.Exp, scale=-1.0)
            act(DVc("tmp", 0, 2), DVc("tmp", 0, 2), AF.Ln, scale=1.0, bias=one_c)
            ts(DVc("cA"), DVc("tmp", 0, 2), -8.0, MUL)
            ts(DVc("hcA"), DVc("tmp", 0, 2), -4.0, MUL)
            ts(DVc("sdcwh"), PVc("sd_cw"), 0.5, MUL)
            ts(DVc("sdcbh"), PVc("sd_cb"), 0.5, MUL)
            ts(DVc("omm"), PVc("rw_mu"), -1.0, MUL, 1.0, ADD)
            ts(DVc("hw0"), PVc("rw_w0"), 0.5, MUL)
            ts(DVc("ha0"), PVc("rw_a0"), 0.5, MUL)
            ts(DVc("omka"), PVc("rw_ka"), -1.0, MUL, 1.0, ADD)
            act(DVc("tmp", 4, 8), PVc("sd_alog"), AF.Exp)
            ts(DVc("aneg"), DVc("tmp", 4, 8), -1.0, MUL)

        def phase_ffn(l):
            m = AR.mark()
            PS.set_rot(range(8))
            GTOK = GT * TT
            hT2 = AR.tile([8, GTOK], BF16)
            actT = AR.tile([NJ, GTOK], BF16)
            sqt = [AR.tile([TT], BF16) for _ in range(2)]
            rstd = AR.tile([TT])
            tg = [AR.tile([TT]) for _ in range(2)]
            s1 = [AR.tile([TT]) for _ in range(2)]
            wgu = [AR.tile([2, 8, 256], BF16) for _ in range(2)]
            w2s = [AR.tile([NJ, 128], BF16) for _ in range(2)]
            for g in range(NT // GT):
                for ti in range(GT):
                    t0 = (g * GT + ti) * TT
                    rmsnorm_tile(xT[:, :, t0:t0 + TT], PVc("g_ffn"), hT2[:, :, ti * TT:(ti + 1) * TT], sqt, rstd)
                for jb in range(NJ // 2):
                    w = wgu[jb % 2]
                    wload(w[:, 0], wsrc(f1_d, l, 0, 8, jb * 256, 256))
                    wload(w[:, 1], wsrc(f1_d, l, 0, 8, FFH + jb * 256, 256))
                    for jj in range(2):
                        j = jb * 2 + jj
                        for ti in range(GT):
                            pg_ = PS.get(4)
                            pu_ = PS.get(4)
                            for k in range(8):
                                mm(pg_, w[:, 0, k, jj * 128:(jj + 1) * 128], hT2[:, k, ti * TT:(ti + 1) * TT],
                                   start=(k == 0), stop=(k == 7))
                            for k in range(8):
                                mm(pu_, w[:, 1, k, jj * 128:(jj + 1) * 128], hT2[:, k, ti * TT:(ti + 1) * TT],
                                   start=(k == 0), stop=(k == 7))
                            tgi, s1i = tg[(j * GT + ti) % 2], s1[(j * GT + ti) % 2]
                            act(tgi, pg_, AF.Tanh, scale=0.5)
                            stt(s1i, tgi, 1.0, pg_, ADD, MUL)
                            stt(actT[:, j, ti * TT:(ti + 1) * TT], s1i, 0.5, pu_, MUL, MUL)
                for d in range(8):
                    w = w2s[d % 2]
                    wload(w, f2_d[l, :, d * 128:(d + 1) * 128].rearrange("(j p) c -> p j c", p=128))
                    for ti in range(GT):
                        t0 = (g * GT + ti) * TT
                        bk = PS.get(4)
                        for j in range(NJ):
                            mm(bk, w[:, j, :], actT[:, j, ti * TT:(ti + 1) * TT], start=(j == 0), stop=(j == NJ - 1))
                        tt(xT[:, d, t0:t0 + TT], xT[:, d, t0:t0 + TT], bk, ADD)
            Pg.barrier()
            AR.reset(m)

        def phase_attn(l):
            m = AR.mark()
            PS.set_rot(range(8))
            wq = AR.tile([8, D], BF16)
            wo = AR.tile([8, D], BF16)
            wkv = [AR.tile([8, 512], BF16) for _ in range(2)]
            hT = AR.tile([8, TT], BF16)
            qT = AR.tile([8, TT], BF16)
            oT = AR.tile([8, TT], BF16)
            et = [AR.tile([TT], BF16) for _ in range(4)]
            rden = AR.tile([TT])
            sqt = [AR.tile([TT], BF16) for _ in range(2)]
            rstd = AR.tile([TT])
            hmT = AR.tile([8, MEMT], BF16)
            wload(wq, wsrc(wq_d, l, 0, 8, 0, D))
            for k in range(8):
                ts(hmT[:, k, :], memnT[:, k, :], PVc("g_memkv", k, k + 1), MUL)
            for half in range(2):
                w = wkv[half]
                wload(w, wsrc(wk_d, l, 0, 8, half * 512, 512))
                for cc in range(4):
                    bk = PS.get(2)
                    for k in range(8):
                        mm(bk, w[:, k, cc * 128:(cc + 1) * 128], hmT[:, k, :], start=(k == 0), stop=(k == 7))
                    cp(KT[:, half * 4 + cc, :], bk, eng="act")
            for half in range(2):
                w = wkv[half]
                wload(w, wsrc(wv_d, l, 0, 8, half * 512, 512))
                for mc in range(2):
                    bk = PS.get(4)
                    for k in range(8):
                        mm(bk, hmT[:, k, mc * 128:(mc + 1) * 128], w[:, k, :], start=(k == 0), stop=(k == 7))
                    cp(Vm[:, mc, half * 512:(half + 1) * 512], bk, eng="act")
            wload(wo, wsrc(wo_d, l, 0, 8, 0, D))
            for ti in range(NT):
                t0 = ti * TT
                rmsnorm_tile(xT[:, :, t0:t0 + TT], PVc("g_memq"), hT, sqt, rstd)
                for c in range(8):
                    bk = PS.get(4)
                    for k in range(8):
                        mm(bk, wq[:, k, c * 128:(c + 1) * 128], hT[:, k, :], start=(k == 0), stop=(k == 7))
                    cp(qT[:, c, :], bk, eng="act")
                for h in range(4):
                    es_ = []
                    for mc in range(2):
                        bk = PS.get(4)
                        for dc in range(2):
                            mm(bk, KT[:, 2 * h + dc, mc * 128:(mc + 1) * 128], qT[:, 2 * h + dc, :],
                               start=(dc == 0), stop=(dc == 1))
                        e_ = et[(h % 2) * 2 + mc]
                        act(e_, bk, AF.Exp, scale=1.0 / 16.0)
                        es_.append(e_)
                    den = PS.get(4)
                    for mc in range(2):
                        mm(den, ones_bf, es_[mc], start=(mc == 0), stop=(mc == 1))
                    recip(rden, den)
                    for dc in range(2):
                        bo = PS.get(4)
                        for mc in range(2):
                            mm(bo, Vm[:, mc, (2 * h + dc) * 128:(2 * h + dc + 1) * 128], es_[mc],
                               start=(mc == 0), stop=(mc == 1))
                        tt(oT[:, 2 * h + dc, :], bo, rden, MUL)
                for d in range(8):
                    bk = PS.get(4)
                    for k in range(8):
                        mm(bk, wo[:, k, d * 128:(d + 1) * 128], oT[:, k, :], start=(k == 0), stop=(k == 7))
                    tt(xT[:, d, t0:t0 + TT], xT[:, d, t0:t0 + TT], bk, ADD)
            Pg.barrier()
            AR.reset(m)

        def phase_final(sq):
            m = AR.mark()
            PS.set_rot(range(8))
            hF = AR.tile([8, TT])
            sqt = [AR.tile([TT], BF16) for _ in range(2)]
            rstd = AR.tile([TT])
            ost = [AR.tile([D]) for _ in range(2)]
            for ti in range(NT):
                t0 = ti * TT
                rmsnorm_tile(xT[:, :, t0:t0 + TT], C("g_final"), hF, sqt, rstd)
                for tc in range(4):
                    o = ost[tc % 2]
                    for half in range(2):
                        bk = PS.get(4)
                        for q in range(4):
                            k = half * 4 + q
                            tpose(bk[:, q * 128:(q + 1) * 128], hF[:, k, tc * 128:(tc + 1) * 128], ident_f)
                        cp(o[:, half * 512:(half + 1) * 512], bk, eng=("act" if half else "dve"))
                    r0 = sq * S + t0 + tc * 128
                    Pg.dma("sp", out_d[r0:r0 + 128, :], o.ap, reads=[o])
            Pg.barrier()
            AR.reset(m)

        PHASE1 = {}

        class Scratch:
            U = 64

            def __init__(self, nunits):
                self.n = nunits
                self.base = AR.top
                AR.top += nunits * self.U
                AR.peak = max(AR.peak, AR.top)
                assert AR.top <= AR.n, f"scratch overflow {AR.top} > {AR.n}"
                self.bufs = [Buf() for _ in range(nunits)]
                self.used = [False] * nunits
                self.owner = {}
                self.peak = 0

            def _get(self, nwords):
                k = (nwords + self.U - 1) // self.U
                i = 0
                while i + k <= self.n:
                    j = i
                    while j < i + k and not self.used[j]:
                        j += 1
                    if j == i + k:
                        for q in range(i, i + k):
                            self.used[q] = True
                        self.peak = max(self.peak, sum(self.used))
                        return i, k
                    i = j + 1
                raise AssertionError(f"scratch pool exhausted (need {k} units, used {sum(self.used)}/{self.n})")

            def f32(self, n):
                i, k = self._get(n)
                ap = arena_h[:, self.base + i * self.U: self.base + i * self.U + n]
                t = T(ap, self.bufs[i:i + k])
                self.owner[id(t)] = (i, k)
                return t

            def bf(self, n):
                i, k = self._get((n + 1) // 2)
                ap = arena_h[:, self.base + i * self.U: self.base + i * self.U + (n + 1) // 2].bitcast(BF16)
                t = T(ap, self.bufs[i:i + k])
                self.owner[id(t)] = (i, k)
                return t

            def free(self, *ts_):
                for t in ts_:
                    i, k = self.owner.pop(id(t))
                    for q in range(i, i + k):
                        self.used[q] = False

        def bc_mid(t, n):
            return t.v(lambda a: a.unsqueeze(1).broadcast_to([128, n, a.shape[1]]))

        def bc_last(t, n):
            return t.v(lambda a: a.unsqueeze(2).broadcast_to([128, a.shape[1], n]))

        def r3(t, a_):
            return t.v(lambda x: x.rearrange("p (a b) -> p a b", a=a_))

        def r4(t, a_, b_):
            return t.v(lambda x: x.rearrange("p (a b c) -> p a b c", a=a_, b=b_))

        VpadV = Vpad.v(lambda a: a.rearrange("p t (c h) n -> p t c h n", c=2))

        def phase_mix(l):
            m = AR.mark()
            PS.set_rot([2, 3, 4, 5, 6, 7])
            wsl = [AR.tile([8, 512], BF16) for _ in range(3)]
            hT = AR.tile([8, TT], BF16)
            yT = AR.tile([8, TT], BF16)
            sqt = [AR.tile([TT], BF16) for _ in range(2)]
            rstd = AR.tile([TT])
            tri4 = AR.tile([TT])
            for i4 in range(4):
                cp(tri4[:, i4 * 128:(i4 + 1) * 128], C("tri"))
            SC = Scratch((AR.n - AR.top) // Scratch.U)
            for t_ in SB_f + SC_f + SD_f + SB_bf + SC_bf + histA + histD + histC + hprev:
                mset(t_, 0.0)
            for a_ in SpadD:
                for b_ in a_:
                    mset(b_, 0.0)
            if not (EN("A") and EN("B") and EN("C") and EN("D")):
                mset(yT, 0.0)

            GROUPS = [(g * 512, 512) for g in range(7)] + [(3584, 4)]
            seq = []
            for ti in range(NT):
                for g in range(8):
                    seq.append(("in", g))
                seq.append(("o", 0))
                seq.append(("o", 1))
            loaded = {}
            state = dict(nload=0)

            def issue_load():
                i = state["nload"]
                if i >= len(seq):
                    return
                kind, g = seq[i]
                slot = wsl[i % 3]
                if kind == "in":
                    c0, n = GROUPS[g]
                    wload(slot[:, :, 0:n], wsrc(win_d, l, 0, 8, c0, n))
                else:
                    wload(slot, wsrc(wout_d, l, 0, 8, g * 512, 512))
                loaded[i] = slot
                state["nload"] += 1

            issue_load()
            issue_load()
            issue_load()
            cur = dict(i=0)

            def next_slot():
                i = cur["i"]
                cur["i"] += 1
                return loaded.pop(i)

            release = issue_load

            def proj(slot, c0, rows=128):
                bk = PS.get(4)
                for k in range(8):
                    mm(bk[0:rows] if rows != 128 else bk, slot[:, k, c0:c0 + rows], hT[:, k, :],
                       start=(k == 0), stop=(k == 7))
                return bk

            def conv_stage(bk, hist, idx, wk, bias):
                st = stage[idx % 2]
                cp(st[:, 0:3], hist)
                cp(st[:, 3:3 + TT], bk, eng="act")
                cv = SC.f32(TT)
                ts(cv, st[:, 0:TT], wk(0), MUL, bias, ADD)
                for k in range(1, 4):
                    stt(cv, st[:, k:k + TT], wk(k), cv, MUL, ADD)
                cp(hist, st[:, TT:TT + 3])
                return cv

            def group_norm(ybank, eps_t, gw, gb):
                ysb = SC.bf(TT)
                cp(ysb, ybank, eng="act")
                ysq = SC.bf(TT)
                act(ysq, ybank, AF.Square)
                pm = PS.get(4)
                mm(pm, bd64_bf, ysb)
                pq = PS.get(4)
                mm(pq, bd64_bf, ysq)
                mean = SC.f32(TT)
                cp(mean, pm, eng="act")
                var = SC.f32(TT)
                stt(var, mean, -1.0, mean, MUL, MUL)
                tt(var, pq, var, ADD)
                ts(var, var, 0.0, ALU.max)
                rsqrt(var, var, 1.0, eps_t)
                yc = SC.f32(TT)
                tt(yc, ybank, mean, SUB)
                tt(yc, yc, var, MUL)
                ts(yc, yc, gw, MUL, gb, ADD)
                SC.free(ysb, ysq, mean, var)
                return yc

            tri_b4 = bc_mid(C("tri"), 4)
            mbias_b4 = bc_mid(C("mbias"), 4)

            for ti in range(NT):
                t0 = ti * TT
                xsl = xT[:, :, t0:t0 + TT]
                rmsnorm_tile(xsl, PVc("g_mix"), hT, sqt, rstd)
                yb = [PS.fixed(0), PS.fixed(1)]

                slot = next_slot()
                if not EN("A"):
                    release()
                if EN("A"):
                    for c in range(2):
                        pg_ = proj(slot, c * 128)
                        px_ = proj(slot, 256 + c * 128)
                        gt = SC.f32(TT)
                        cp(gt, pg_, eng="act")
                        cv = conv_stage(px_, histA[c], c, lambda k: PVc("lru_cw", k * 2 + c, k * 2 + c + 1),
                                        PVc("lru_cb", c, c + 1))
                        cvb = SC.bf(TT)
                        cp(cvb, cv, eng="act")
                        pr = PS.get(4)
                        mm(pr, smallw[:, c * 128:(c + 1) * 128], cvb)
                        pi_ = PS.get(4)
                        mm(pi_, smallw[:, 256 + c * 128:256 + (c + 1) * 128], cvb)
                        tr = SC.f32(TT)
                        act(tr, pr, AF.Tanh, scale=0.5, bias=DVc("hbr", c, c + 1))
                        ti_ = SC.f32(TT)
                        act(ti_, pi_, AF.Tanh, scale=0.5, bias=DVc("hbi", c, c + 1))
                        a_ = SC.f32(TT)
                        act(a_, tr, AF.Exp, scale=DVc("hcA", c, c + 1), bias=DVc("hcA", c, c + 1))
                        s_ = SC.f32(TT)
                        act(s_, tr, AF.Exp, scale=DVc("cA", c, c + 1), bias=DVc("cA", c, c + 1))
                        act(s_, s_, AF.Ln, scale=-1.0, bias=one_c)
                        act(s_, s_, AF.Exp, scale=0.5)
                        stt(ti_, ti_, 1.0, cv, ADD, MUL)
                        stt(ti_, ti_, 0.5, s_, MUL, MUL)
                        scan(tr, a_, ti_, hprev[c])
                        cp(hprev[c], tr[:, TT - 1:TT])
                        act(s_, gt, AF.Square)
                        ts(s_, s_, 0.044715, MUL, 1.0, ADD)
                        tt(s_, s_, gt, MUL)
                        act(s_, s_, AF.Tanh, scale=0.7978845608028654)
                        stt(s_, s_, 1.0, gt, ADD, MUL)
                        stt(yT[:, c, :], s_, 0.5, tr, MUL, MUL)
                        SC.free(gt, cv, cvb, tr, ti_, a_, s_)
                    release()

                slot1 = next_slot()
                slot2 = next_slot()
                if not EN("B"):
                    release()
                    release()
                if EN("B"):
                    rope = SC.f32(8 * TT)
                    rope3 = r3(rope, 8)
                    for i8 in range(8):
                        ld(rope3[:, i8, :], rope_d[ti, :, i8, :])
                    qk = [[SC.bf(TT) for _ in range(2)] for _ in range(2)]
                    for is_k in range(2 if "Q" not in flags.get("Bskip", "") else 0):
                        for c in range(2):
                            bk = proj(slot1, is_k * 256 + c * 128)
                            xb = SC.bf(TT)
                            cp(xb, bk, eng="act")
                            p2 = PS.get(4)
                            mm(p2, swap_bf, xb)
                            t1 = SC.f32(TT)
                            tt(t1, bk, rope3[:, 4 * is_k + 2 * c + 0, :], MUL)
                            t2 = SC.f32(TT)
                            tt(t2, p2, rope3[:, 4 * is_k + 2 * c + 1, :], MUL)
                            tt(qk[is_k][c], t1, t2, ADD)
                            SC.free(xb, t1, t2)
                    SC.free(rope)
                    release()
                    qT_, kT_ = qk
                    dbg("B_q0", qT_[0]); dbg("B_k0", kT_[0]); dbg("B_hT", hT)
                    ktok = SC.bf(4 * 256)
                    ktok3 = r3(ktok, 4)
                    vt = SC.bf(4 * 256)
                    vt3 = r3(vt, 4)
                    for tc in range(4):
                        tcs = slice(tc * 128, (tc + 1) * 128)
                        if "T" not in flags.get("Bskip", ""):
                            pt = PS.get(1, BF16)
                            for c in range(2):
                                tpose(pt[:, c * 128:(c + 1) * 128], kT_[c][:, tcs], ident_bf)
                            cp(ktok3[:, tc, :], pt, eng="act")
                        if "v" in flags.get("Bskip", ""):
                            continue
                        bv = PS.get(2)
                        for k in range(8):
                            mm(bv, hT[:, k, tcs], slot2[:, k, 0:256], start=(k == 0), stop=(k == 7))
                        cp(vt3[:, tc, :], bv, eng="act")
                        bv4 = r4(vt3[:, tc, :], 2, 2)
                        for hh in range(2 if "V" not in flags.get("Bskip", "") else 0):
                            ts(VpadV[:, tc, :, hh, hh * 64:(hh + 1) * 64], bv4[:, :, hh, :], 1.0, MUL)
                    sg = []
                    for c in range(2 if "G" not in flags.get("Bskip", "") else 0):
                        bk = proj(slot2, 256 + c * 128)
                        tg_ = SC.f32(TT)
                        act(tg_, bk, AF.Tanh, scale=0.5)
                        s = SC.f32(TT)
                        stt(s, tg_, 1.0, bk, ADD, MUL)
                        SC.free(tg_)
                        sg.append(s)
                    release()
                    for tc in range(4 if flags.get("Bstage", 3) >= 2 else 0):
                        tcs = slice(tc * 128, (tc + 1) * 128)
                        sm = SC.bf(TT)
                        sm4 = r4(sm, 2, 2)
                        for hh in range(2):
                            r0 = hh * 64
                            sbh = PS.get(2)
                            for c in range(2):
                                mm(sbh[:, c * 128:(c + 1) * 128], kT_[c][r0:r0 + 64, tcs], qT_[c][r0:r0 + 64, tcs])
                            tt(sm4[:, :, hh, :], r3(sbh, 2), r3(tri4[:, 0:256], 2), MUL)
                        for c in range(2):
                            ycol = yb[c][:, tcs]
                            mm(ycol, SB_bf[c], qT_[c][:, tcs], start=True, stop=False)
                            for hh in range(2):
                                h = 2 * c + hh
                                mm(ycol, Vpad[:, tc, h, :], sm[:, h * 128:(h + 1) * 128], start=False, stop=(hh == 1))
                            pS = PS.get(1)
                            mm(pS, ktok3[:, tc, c * 128:(c + 1) * 128], vt3[:, tc, c * 128:(c + 1) * 128])
                            tmp = SC.f32(128)
                            stt(tmp, pS, C("g128", c, c + 1), C("bd"), MUL, MUL)
                            stt(SB_f[c], SB_f[c], C("g128", c, c + 1), tmp, MUL, ADD)
                            cp(SB_bf[c], SB_f[c], eng="act")
                            SC.free(tmp)
                        SC.free(sm)
                    if flags.get("dbg"):
                        dbg("B_vt", vt); dbg("B_ktok", ktok); dbg("B_sg0", sg[0]); dbg("B_Vpad", Vpad)
                    for c in range(2 if flags.get("Bstage", 3) >= 3 else 0):
                        ydb = SC.f32(TT)
                        cp(ydb, yb[c])
                        dbg("B_y%d" % c, ydb)
                        SC.free(ydb)
                        yc = group_norm(yb[c], eps5, PVc("ret_gw", c, c + 1), PVc("ret_gb", c, c + 1))
                        dbg("B_yn%d" % c, yc)
                        stt(yT[:, 2 + c, :], yc, 0.5, sg[c], MUL, MUL)
                        SC.free(yc)
                    SC.free(ktok, vt, *sg, *qT_, *kT_)
                elif False:
                    pass

                slot3 = next_slot()
                slot4 = next_slot()
                if not EN("C"):
                    release()
                    release()
                if EN("C"):
                    def shiftC(idx, bk):
                        st = stage[idx % 2]
                        cp(st[:, 2:3], histC[idx])
                        cp(st[:, 3:3 + TT], bk, eng="act")
                        o = SC.f32(TT)
                        ts(o, st[:, 3:3 + TT], DVc("omm", idx, idx + 1), MUL)
                        stt(o, st[:, 2:2 + TT], PVc("rw_mu", idx, idx + 1), o, MUL, ADD)
                        cp(histC[idx], st[:, 2 + TT:3 + TT])
                        return o

                    rf = [shiftC(c, proj(slot3, c * 128)) for c in range(2)]
                    kf = [shiftC(2 + c, proj(slot3, 256 + c * 128)) for c in range(2)]
                    release()
                    vf = [shiftC(4 + c, proj(slot4, c * 128)) for c in range(2)]
                    lo6 = shiftC(6, proj(slot4, 256))
                    lo7 = shiftC(7, proj(slot4, 384))
                    release()
                    tw = SC.bf(TT)
                    act(tw[0:64], lo6[0:64], AF.Tanh)
                    al = SC.bf(TT)
                    cp(al[64:128], lo6[64:128], eng="act")
                    sgl = SC.bf(TT)
                    act(lo7, lo7, AF.Tanh, scale=0.5)
                    ts(sgl, lo7, 0.5, MUL, 0.5, ADD)
                    SC.free(lo6, lo7)
                    gT_, bon, Wt, ARt, Ktl, Btl, vb, ARb = [], [], [], [], [], [], [], []
                    for c in range(2):
                        th = SC.f32(TT)
                        pw = PS.get(4)
                        mm(pw, smallw[0:64, 512 + c * 128:512 + (c + 1) * 128], tw[0:64])
                        logw = SC.f32(TT)
                        act(th, pw, AF.Tanh, scale=0.5, bias=DVc("hw0", c, c + 1))
                        ts(logw, th, -0.3032653298563167, MUL, -0.3032653298563167, ADD)
                        pa = PS.get(4)
                        mm(pa, smallw[64:128, 768 + c * 128:768 + (c + 1) * 128], al[64:128])
                        a_ = SC.f32(TT)
                        act(th, pa, AF.Tanh, scale=0.5, bias=DVc("ha0", c, c + 1))
                        ts(a_, th, 0.5, MUL, 0.5, ADD)
                        pg_ = PS.get(4)
                        mm(pg_, smallw[:, 1024 + c * 128:1024 + (c + 1) * 128], sgl)
                        g_ = SC.f32(TT)
                        cp(g_, pg_, eng="act")
                        gT_.append(g_)
                        kq = SC.f32(TT)
                        ts(kq, kf[c], PVc("rw_kk", c, c + 1), MUL)
                        ksq = SC.bf(TT)
                        act(ksq, kq, AF.Square)
                        pk = PS.get(4)
                        mm(pk, bd1_bf, ksq)
                        rsqrt(th, pk, 1.0, eps12)
                        tt(kq, kq, th, MUL)
                        SC.free(ksq)
                        ts(th, a_, PVc("rw_ka", c, c + 1), MUL, DVc("omka", c, c + 1), ADD)
                        kp = SC.f32(TT)
                        tt(kp, kf[c], th, MUL)
                        tt(th, a_, kq, MUL)
                        SC.free(a_, kf[c])
                        rk = SC.bf(TT)
                        stt(rk, rf[c], PVc("rw_rk", c, c + 1), kp, MUL, MUL)
                        pbn = PS.get(4)
                        mm(pbn, bd1_bf, rk)
                        bo = SC.f32(TT)
                        tt(bo, pbn, vf[c], MUL)
                        bon.append(bo)
                        SC.free(rk)
                        cum = SC.f32(TT)
                        scan(cum, C("reset"), logw, 0.0)
                        wt = SC.f32(TT)
                        act(wt, cum, AF.Exp)
                        Wt.append(wt)
                        wi = SC.f32(TT)
                        act(wi, cum, AF.Exp, scale=-1.0)
                        tt(cum, cum, logw, SUB)
                        act(cum, cum, AF.Exp)
                        SC.free(logw)
                        art = SC.bf(2 * TT)
                        art4 = r4(art, 4, 2)
                        ARb.append(art)
                        tt(art4[:, :, 0, :], r3(kq, 4), r3(cum, 4), MUL)
                        tt(art4[:, :, 1, :], r3(rf[c], 4), r3(wt, 4), MUL)
                        ARt.append(art4)
                        kt_ = SC.bf(TT)
                        tt(kt_, kp, wi, MUL)
                        bt_ = SC.bf(TT)
                        tt(bt_, th, wi, MUL)
                        Ktl.append(kt_)
                        Btl.append(bt_)
                        v_ = SC.bf(TT)
                        cp(v_, vf[c], eng="act")
                        vb.append(v_)
                        SC.free(th, kq, kp, cum, wi, rf[c], vf[c])
                    SC.free(tw, al, sgl)
                    Vt = SC.bf(4 * 256)
                    Vt3 = r3(Vt, 4)
                    Ktok = SC.bf(4 * 256)
                    Ktok3 = r3(Ktok, 4)
                    Btok = SC.bf(4 * 256)
                    Btok3 = r3(Btok, 4)
                    for tc in range(4):
                        tcs = slice(tc * 128, (tc + 1) * 128)
                        for src, dst, pad in ((vb, Vt3, True), (Ktl, Ktok3, False), (Btl, Btok3, False)):
                            pt = PS.get(1, BF16)
                            for c in range(2):
                                tpose(pt[:, c * 128:(c + 1) * 128], src[c][:, tcs], ident_bf)
                            cp(dst[:, tc, :], pt, eng="act")
                            if pad:
                                pt4 = r4(dst[:, tc, :], 2, 2)
                                for hh in range(2):
                                    ts(VpadV[:, tc, :, hh, hh * 64:(hh + 1) * 64], pt4[:, :, hh, :], 1.0, MUL)
                    SC.free(*vb)
                    uset = 0
                    for tc in range(4):
                        tcs = slice(tc * 128, (tc + 1) * 128)
                        Mxs = []
                        L4 = SC.bf(TT)
                        P4 = SC.bf(TT)
                        for h in range(4):
                            c, hh = h // 2, h % 2
                            r0 = hh * 64
                            arh = ARt[c][r0:r0 + 64, tc].v(lambda a: a.rearrange("p a b -> p (a b)"))
                            pM = PS.get(4)
                            mm(pM[:, 0:256], Btl[c][r0:r0 + 64, tcs], arh)
                            mm(pM[:, 256:512], Ktl[c][r0:r0 + 64, tcs], arh)
                            pL = PS.get(1)
                            mm(pL, ARt[c][r0:r0 + 64, tc, 0, :], Btl[c][r0:r0 + 64, tcs])
                            Mx = SC.bf(TT)
                            tt(Mx, pM, C("maskM"), MUL)
                            tt(L4[:, h * 128:(h + 1) * 128], pL, C("mlow"), MUL)
                            tt(P4[:, h * 128:(h + 1) * 128], ident_bf, Mx[:, 0:128], SUB)
                            Mxs.append(Mx)
                        A4 = None
                        for lev in range(1, 7):
                            def Aof(h):
                                return Mxs[h][:, 0:128] if A4 is None else A4[:, h * 128:(h + 1) * 128]
                            bankL = PS.get(4)
                            for h in range(4):
                                mm(bankL[:, h * 128:(h + 1) * 128], Aof(h), L4[:, h * 128:(h + 1) * 128])
                            L4n = SC.bf(TT)
                            cp(L4n, bankL, eng="act")
                            A4n = None
                            if lev < 6:
                                bankA = PS.get(4)
                                for h in range(4):
                                    mm(bankA[:, h * 128:(h + 1) * 128], L4[:, h * 128:(h + 1) * 128], Aof(h))
                                A4n = SC.bf(TT)
                                cp(A4n, bankA)
                            bankP = PS.get(4)
                            for h in range(4):
                                mm(bankP[:, h * 128:(h + 1) * 128], L4n[:, h * 128:(h + 1) * 128],
                                   P4[:, h * 128:(h + 1) * 128])
                            P4n = SC.bf(TT)
                            tt(P4n, bankP, P4, ADD)
                            SC.free(P4, L4)
                            if A4 is not None:
                                SC.free(A4)
                            P4, L4, A4 = P4n, L4n, A4n
                        SC.free(L4)
                        for c in range(2):
                            up = Upad[uset % 2]
                            uset += 1
                            for hh in range(2):
                                h = 2 * c + hh
                                r0 = hh * 64
                                Mx = Mxs[h]
                                pX = PS.get(1)[:, 0:64]
                                mm(pX, ARt[c][:, tc, 0, :], SC_bf[c][:, r0:r0 + 64], start=True, stop=False)
                                mm(pX, Mx[:, 256:384], Vt3[:, tc, h * 64:(h + 1) * 64], start=False, stop=True)
                                Xb = SC.bf(64)
                                cp(Xb, pX, eng="act")
                                pU = PS.get(1)[:, 0:64]
                                mm(pU, P4[:, h * 128:(h + 1) * 128], Xb)
                                act(up[hh][:, r0:r0 + 64], pU, AF.Copy, scale=-1.0)
                                SC.free(Xb)
                            ycol = yb[c][:, tcs]
                            mm(ycol, SC_bf[c], ARt[c][:, tc, 1, :], start=True, stop=False)
                            for hh in range(2):
                                h = 2 * c + hh
                                mm(ycol, Vpad[:, tc, h, :], Mxs[h][:, 384:512], start=False, stop=False)
                                mm(ycol, up[hh], Mxs[h][:, 128:256], start=False, stop=(hh == 1))
                            pS = PS.get(1)
                            kc_ = Ktok3[:, tc, c * 128:(c + 1) * 128]
                            bc_ = Btok3[:, tc, c * 128:(c + 1) * 128]
                            mm(pS, kc_, Vpad[:, tc, 2 * c, :], start=True, stop=False)
                            mm(pS, kc_, Vpad[:, tc, 2 * c + 1, :], start=False, stop=False)
                            mm(pS, bc_, up[0], start=False, stop=False)
                            mm(pS, bc_, up[1], start=False, stop=True)
                            wc = Wt[c][:, tc * 128 + 127:tc * 128 + 128]
                            tmp = SC.f32(128)
                            stt(tmp, pS, wc, C("bd"), MUL, MUL)
                            stt(SC_f[c], SC_f[c], wc, tmp, MUL, ADD)
                            cp(SC_bf[c], SC_f[c], eng="act")
                            SC.free(tmp)
                        SC.free(P4, *Mxs)
                    for c in range(2):
                        yc = group_norm(yb[c], eps64, PVc("rw_gw", c, c + 1), PVc("rw_gb", c, c + 1))
                        tt(yc, yc, bon[c], ADD)
                        tt(yT[:, 4 + c, :], yc, gT_[c], MUL)
                        SC.free(yc)
                    for lst in (gT_, bon, Wt, Ktl, Btl):
                        SC.free(*lst)
                    SC.free(Vt, Ktok, Btok)
                    SC.free(*ARb)

                slot5 = next_slot()
                slot6 = next_slot()
                slot7 = next_slot()
                if not EN("D"):
                    release()
                    release()
                    release()
                if EN("D"):
                    sz = []
                    for c in range(2):
                        bk = proj(slot5, c * 128)
                        tg_ = SC.f32(TT)
                        act(tg_, bk, AF.Tanh, scale=0.5)
                        s = SC.f32(TT)
                        stt(s, tg_, 1.0, bk, ADD, MUL)
                        SC.free(tg_)
                        sz.append(s)

                    def convD(idx, bk):
                        cv = conv_stage(bk, histD[idx], idx, lambda k: DVc("sdcwh", k * 6 + idx, k * 6 + idx + 1),
                                        DVc("sdcbh", idx, idx + 1))
                        th = SC.f32(TT)
                        act(th, cv, AF.Tanh)
                        return cv, th

                    xsf, xsb, BT_, CT_ = [], [], [], []
                    for c in range(2):
                        cv, th = convD(c, proj(slot5, 256 + c * 128))
                        stt(cv, th, 1.0, cv, ADD, MUL)
                        b_ = SC.bf(TT)
                        cp(b_, cv, eng="act")
                        xsf.append(cv)
                        xsb.append(b_)
                        SC.free(th)
                    release()
                    for g in range(2):
                        cv, th = convD(2 + g, proj(slot6, g * 128))
                        b_ = SC.bf(TT)
                        stt(b_, th, 1.0, cv, ADD, MUL)
                        BT_.append(b_)
                        SC.free(cv, th)
                    for g in range(2):
                        cv, th = convD(4 + g, proj(slot6, 256 + g * 128))
                        b_ = SC.bf(TT)
                        stt(b_, th, 1.0, cv, ADD, MUL)
                        CT_.append(b_)
                        SC.free(cv, th)
                    release()
                    pdt = PS.get(1)
                    for tc in range(4):
                        for k in range(8):
                            mm(pdt[:, tc * 4:(tc + 1) * 4], hT[:, k, tc * 128:(tc + 1) * 128], slot7[:, k, 0:4],
                               start=(k == 0), stop=(k == 7))
                    release()
                    dt = SC.f32(16)
                    tt(r3(dt, 4), r3(pdt[:, 0:16], 4), bc_mid(PVc("sd_dtb"), 4), ADD)
                    act(dt, dt, AF.Exp)
                    act(dt, dt, AF.Ln, scale=1.0, bias=one_c)
                    la = SC.f32(16)
                    tt(r3(la, 4), r3(dt, 4), bc_mid(DVc("aneg"), 4), MUL)
                    pc = PS.get(1)
                    mm(pc[:, 0:16], C("tri"), la)
                    cum = SC.f32(16)
                    cp(cum, pc[:, 0:16], eng="act")
                    Btok = SC.bf(4 * 256)
                    Btok3 = r3(Btok, 4)
                    for tc in range(4):
                        tcs = slice(tc * 128, (tc + 1) * 128)
                        h4 = slice(tc * 4, (tc + 1) * 4)
                        px = PS.get(1, BF16)
                        for c in range(2):
                            tpose(px[:, c * 128:(c + 1) * 128], xsb[c][:, tcs], ident_bf)
                        pb = PS.get(1, BF16)
                        for g in range(2):
                            tpose(pb[:, g * 128:(g + 1) * 128], BT_[g][:, tcs], ident_bf)
                        cp(Btok3[:, tc, :], pb, eng="act")
                        lat = SC.f32(TT)
                        tt(r3(lat, 4), tri_b4, bc_last(la[:, h4], 128), MUL)
                        pcb = PS.get(4)
                        mm(pcb, ones_f, lat)
                        dif = SC.f32(TT)
                        tt(r3(dif, 4), r3(pcb, 4), bc_last(cum[:, h4], 128), SUB)
                        tt(r3(dif, 4), r3(dif, 4), mbias_b4, ADD)
                        act(dif, dif, AF.Exp)
                        E4 = SC.f32(TT)
                        act(E4, pcb, AF.Exp)
                        E43 = r3(E4, 4)
                        we = SC.f32(4)
                        tt(we, r3(pcb, 4)[:, :, 127], cum[:, h4], SUB)
                        act(we, we, AF.Exp)
                        tt(we, we, dt[:, h4], MUL)
                        px4 = r4(px, 2, 2)
                        dt22 = r3(dt[:, h4], 2)
                        for hh in range(2):
                            tt(VpadV[:, tc, :, hh, hh * 64:(hh + 1) * 64], px4[:, :, hh, :],
                               bc_last(dt22[:, :, hh], 64), MUL)
                        vw = SC.bf(256)
                        tt(r3(vw, 4), r3(px, 4), bc_last(we, 64), MUL)
                        psc = PS.get(2)
                        for g in range(2):
                            mm(psc[:, g * 128:(g + 1) * 128], BT_[g][:, tcs], CT_[g][:, tcs])
                        sm = SC.bf(TT)
                        dif4 = r4(dif, 2, 2)
                        sm4 = r4(sm, 2, 2)
                        Cs = SC.bf(TT)
                        Cs4 = r4(Cs, 2, 2)
                        E44 = r4(E4, 2, 2)
                        for g in range(2):
                            tt(sm4[:, g], bc_mid(psc[:, g * 128:(g + 1) * 128], 2), dif4[:, g], MUL)
                            tt(Cs4[:, g], bc_mid(CT_[g][:, tcs], 2), E44[:, g], MUL)
                        for g in range(2):
                            ycol = yb[g][:, tcs]
                            for hh in range(2):
                                h = 2 * g + hh
                                mm(ycol, SpadD[g][hh], Cs[:, h * 128:(h + 1) * 128], start=(hh == 0), stop=False)
                                mm(ycol, Vpad[:, tc, h, :], sm[:, h * 128:(h + 1) * 128], start=False, stop=(hh == 1))
                            pS = PS.get(1)
                            mm(pS, Btok3[:, tc, g * 128:(g + 1) * 128], vw[:, g * 128:(g + 1) * 128])
                            sd3 = r3(SD_f[g], 2)
                            tt(sd3, sd3, bc_last(E43[:, 2 * g:2 * g + 2, 127], 64), MUL)
                            tt(SD_f[g], SD_f[g], pS, ADD)
                            for hh in range(2):
                                cp(SpadD[g][hh][:, hh * 64:(hh + 1) * 64], SD_f[g][:, hh * 64:(hh + 1) * 64], eng="act")
                        SC.free(lat, dif, E4, we, vw, sm, Cs)
                    for g in range(2):
                        y1 = SC.f32(TT)
                        stt(y1, xsf[g], PVc("sd_d", g, g + 1), yb[g], MUL, ADD)
                        stt(y1, y1, 0.5, sz[g], MUL, MUL)
                        ysq = SC.bf(TT)
                        act(ysq, y1, AF.Square)
                        pm = PS.get(4)
                        mm(pm, ones128_bf, ysq)
                        r_ = SC.f32(TT)
                        rsqrt(r_, pm, 1.0, eps6)
                        stt(yT[:, 6 + g, :], y1, PVc("sd_nw", g, g + 1), r_, MUL, MUL)
                        SC.free(y1, ysq, r_)
                    SC.free(dt, la, cum, Btok, *sz, *xsf, *xsb, *BT_, *CT_)

                so = [next_slot(), next_slot()]
                for d in range(8):
                    bk = PS.get(4)
                    w = so[d // 4]
                    for k in range(8):
                        mm(bk, w[:, k, (d % 4) * 128:(d % 4 + 1) * 128], yT[:, k, :], start=(k == 0), stop=(k == 7))
                    tt(xT[:, d, t0:t0 + TT], xT[:, d, t0:t0 + TT], bk, ADD)
                    if d % 4 == 3:
                        release()
            dbg("yT", yT)
            PHASE1["scratch_peak"] = max(PHASE1.get("scratch_peak", 0), SC.peak)
            PHASE1["scratch_n"] = SC.n
            Pg.barrier()
            AR.reset(m)


        for sq in range(NSEQ):
            phase0(sq)
            for l in range(L):
                layer_setup(l)
                if EN("mix"):
                    phase_mix(l)
                if EN("attn"):
                    phase_attn(l)
                if EN("ffn"):
                    phase_ffn(l)
            phase_final(sq)
        Pg.wait_all_on("sp")
        build.info = dict(n_ops=Pg.n_ops, arena_peak=AR.peak, arena_n=AR.n, **PHASE1)

        with nc.Block() as block:
            @block.tensor
            def _(e):
                Pg.replay("pe", e)

            @block.scalar
            def _(e):
                Pg.replay("act", e)

            @block.vector
            def _(e):
                Pg.replay("dve", e)

            @block.gpsimd
            def _(e):
                Pg.replay("pool", e)

            @block.sync
            def _(e):
                Pg.replay("sp", e)
    return nc


def make_in_maps(inputs, L, S, nseq_per_core, n_cores):
    f = lambda a: np.ascontiguousarray(np.asarray(a, dtype=np.float32))
    shared = {
        "cb": make_consts(f(inputs["norm_final"])),
        "rope": make_rope(S),
        "pv": make_pvec(inputs, L),
        "smallw": make_smallw(inputs, L),
        "w_in": f(inputs["w_in"]),
        "w_out": f(inputs["w_out"]),
        "mem_wq": f(inputs["mem_wq"]),
        "mem_wk": f(inputs["mem_wk"]),
        "mem_wv": f(inputs["mem_wv"]),
        "mem_wo": f(inputs["mem_wo"]),
        "ffn_w_in": f(inputs["ffn_w_in"]),
        "ffn_w_out": f(inputs["ffn_w_out"]),
    }
    x = f(inputs["x"])
    mem = f(inputs["mem"])
    maps = []
    for c in range(n_cores):
        b0 = c * nseq_per_core
        m = dict(shared)
        m["x"] = np.ascontiguousarray(x[b0:b0 + nseq_per_core].reshape(nseq_per_core * S, D))
        m["mem"] = np.ascontiguousarray(mem[b0:b0 + nseq_per_core].reshape(nseq_per_core * MEMT, D))
        maps.append(m)
    return maps


def kernel(**inputs):
    x = np.asarray(inputs["x"])
    B, S, _ = x.shape
    L = np.asarray(inputs["w_in"]).shape[0]
    n_cores = N_CORES if B % N_CORES == 0 else 1
    nseq = B // n_cores
    nc = build(L, S, nseq)
    maps = make_in_maps(inputs, L, S, nseq, n_cores)
    res = run_bass_kernel_spmd(nc, maps, core_ids=list(range(n_cores)))
    outs = [np.asarray(r["out"]).reshape(nseq, S, D) for r in res.results]
    return np.concatenate(outs, axis=0).astype(np.float32)
```

```python
import math
import numpy as np
import concourse.bass as bass
import concourse.mybir as mybir
from concourse.bass_utils import run_bass_kernel_spmd

F32 = mybir.dt.float32
BF16 = mybir.dt.bfloat16
AF = mybir.ActivationFunctionType
ALU = mybir.AluOpType

D = 1024
KC = 8
TT = 512
IN_COLS = 3588
FFH = 2816
NJ = FFH // 128
MEMT = 256
N_CORES = 8
SYNC_MODE = "old"


class Buf:
    __slots__ = ("w", "r", "gen")

    def __init__(self):
        self.w = None
        self.r = {}
        self.gen = 0


class T:
    __slots__ = ("ap", "bufs", "gens")

    def __init__(self, ap, bufs):
        self.ap = ap
        self.bufs = bufs
        self.gens = [b.gen for b in bufs]

    def __getitem__(self, k):
        t = T.__new__(T)
        t.ap = self.ap[k]
        t.bufs = self.bufs
        t.gens = self.gens
        return t

    def v(self, fn):
        t = T.__new__(T)
        t.ap = fn(self.ap)
        t.bufs = self.bufs
        t.gens = self.gens
        return t

    def check(self):
        for b, g in zip(self.bufs, self.gens):
            assert b.gen == g, "stale PSUM tile used"


class Prog:
    def __init__(self, nc, sems):
        self.nc = nc
        self.E = {}
        for name in ("pe", "act", "dve", "pool", "sp"):
            self.E[name] = dict(ops=[], sem=sems[name], cnt=0, seen={})
        self.dq = {"sp": dict(slots=[[s, 0] for s in sems["dsp"]], i=0),
                   "pool": dict(slots=[[s, 0] for s in sems["dpool"]], i=0)}
        self.n_ops = 0

    def _waits(self, E, eng, reads, writes):
        waits = {}
        own = E["sem"]

        def need(tok, kind):
            if tok is None:
                return
            sem, val = tok
            if sem is own:
                if eng == "pe" or (SYNC_MODE == "old" and kind != "raw"):
                    return
            if E["seen"].get(sem, 0) >= val:
                return
            if waits.get(sem, 0) < val:
                waits[sem] = val

        for t in reads:
            t.check()
            for b in t.bufs:
                need(b.w, "raw")
        for t in writes:
            t.check()
            for b in t.bufs:
                need(b.w, "waw")
                for s, v in b.r.items():
                    need((s, v), "war")
        for s, v in waits.items():
            E["seen"][s] = v
        return list(waits.items())

    def op(self, eng, fn, reads=(), writes=()):
        E = self.E[eng]
        waits = self._waits(E, eng, reads, writes)
        E["cnt"] += 1
        c = E["cnt"]
        sem = E["sem"]
        E["ops"].append((waits, fn, sem, 1))
        for t in reads:
            for b in t.bufs:
                if b.r.get(sem, 0) < c:
                    b.r[sem] = c
        for t in writes:
            for b in t.bufs:
                b.w = (sem, c)
                b.r = {}
        self.n_ops += 1

    def dma(self, q, out, in_, reads=(), writes=()):
        E = self.E[q]
        dq = self.dq[q]
        slot = dq["slots"][dq["i"] % len(dq["slots"])]
        dq["i"] += 1
        waits = dict(self._waits(E, q, reads, writes))
        sem, val = slot
        if val > 0 and E["seen"].get(sem, 0) < val:
            waits[sem] = max(waits.get(sem, 0), val)
            E["seen"][sem] = val
        slot[1] = val + 16
        nv = slot[1]
        E["ops"].append((list(waits.items()), lambda e: e.dma_start(out=out, in_=in_), sem, 16))
        for t in reads:
            for b in t.bufs:
                b.r[sem] = nv
        for t in writes:
            for b in t.bufs:
                b.w = (sem, nv)
                b.r = {}
        self.n_ops += 1
        return (sem, nv)

    def barrier(self):
        allt = {}
        for n, E in self.E.items():
            if E["cnt"] > 0:
                allt[E["sem"]] = E["cnt"]
        for q in self.dq.values():
            for s, v in q["slots"]:
                if v > 0:
                    allt[s] = v
        for n, E in self.E.items():
            w = []
            for s, v in allt.items():
                if s is E["sem"]:
                    continue
                if E["seen"].get(s, 0) < v:
                    w.append((s, v))
                    E["seen"][s] = v
            if w:
                E["ops"].append((w, None, None, 0))

    def wait_all_on(self, eng):
        E = self.E[eng]
        w = []
        for n, E2 in self.E.items():
            if E2 is not E and E2["cnt"] > 0:
                w.append((E2["sem"], E2["cnt"]))
        for q in self.dq.values():
            for s, v in q["slots"]:
                if v > 0:
                    w.append((s, v))
        E["ops"].append((w, None, None, 0))

    def replay(self, name, e):
        for waits, fn, sem, inc in self.E[name]["ops"]:
            for s, v in waits:
                e.wait_ge(s, v)
            if fn is not None:
                fn(e).then_inc(sem, inc)


class Arena:
    def __init__(self, handle, nwords):
        self.h = handle
        self.n = nwords
        self.top = 0
        self.peak = 0

    def tile(self, free_shape, dtype=F32, parts=128):
        n = 1
        for s in free_shape:
            n *= s
        words = n if dtype == F32 else (n + 1) // 2
        words = (words + 15) // 16 * 16
        off = self.top
        self.top += words
        self.peak = max(self.peak, self.top)
        assert self.top <= self.n, f"SBUF arena overflow {self.top} > {self.n}"
        ap = self.h[:, off:off + (n if dtype == F32 else (n + 1) // 2)]
        if dtype != F32:
            ap = ap.bitcast(dtype)
            if ap.shape[1] != n:
                ap = ap[:, 0:n]
        if len(free_shape) == 2:
            ap = ap.rearrange("p (a b) -> p a b", a=free_shape[0])
        elif len(free_shape) == 3:
            ap = ap.rearrange("p (a b c) -> p a b c", a=free_shape[0], b=free_shape[1])
        elif len(free_shape) == 4:
            ap = ap.rearrange("p (a b c d) -> p a b c d", a=free_shape[0], b=free_shape[1], c=free_shape[2])
        if parts != 128:
            ap = ap[0:parts]
        return T(ap, [Buf()])

    def mark(self):
        return self.top

    def reset(self, m):
        self.top = m


class Psum:
    def __init__(self, banks):
        self.banks = banks
        self.b = [Buf() for _ in range(8)]
        self.rot = list(range(8))
        self.ptr = 0

    def set_rot(self, banks):
        self.rot = list(banks)
        self.ptr = 0

    def _mk(self, b, nq, dtype):
        self.b[b].gen += 1
        ap = self.banks[b][:, 0:nq * 128]
        if dtype != F32:
            ap = ap.bitcast(dtype)
        return T(ap, [self.b[b]])

    def get(self, nq=4, dtype=F32):
        b = self.rot[self.ptr % len(self.rot)]
        self.ptr += 1
        return self._mk(b, nq, dtype)

    def fixed(self, b, dtype=F32):
        return self._mk(b, 4, dtype)


PV = {}


def _pv_layout():
    if PV:
        return PV["_n"]
    c = 0
    for name, n in [("g_mix", 8), ("lru_cw", 8), ("lru_cb", 2), ("lru_br", 2), ("lru_bi", 2), ("lru_lam", 2),
                    ("ret_gw", 2), ("ret_gb", 2), ("rw_mu", 8), ("rw_w0", 2), ("rw_a0", 2), ("rw_kk", 2),
                    ("rw_ka", 2), ("rw_rk", 2), ("rw_gw", 2), ("rw_gb", 2), ("sd_cw", 24), ("sd_cb", 6),
                    ("sd_d", 2), ("sd_nw", 2), ("g_memq", 8), ("g_memkv", 8), ("g_ffn", 8),
                    ("sd_dtb", 4), ("sd_alog", 4)]:
        PV[name] = (c, n)
        c += n
    PV["_n"] = c
    return c


CC = {}


def _cc_layout():
    if CC:
        return CC["_n"]
    c = 0
    for name, n in [("ident", 128), ("tri", 128), ("mstrT", 128), ("mlow", 128), ("maskM", 512), ("mbias", 128),
                    ("bd", 128), ("swap", 128), ("reset", 512), ("g128", 2), ("eps6", 1), ("eps5", 1),
                    ("eps64", 1), ("eps12", 1), ("one", 1), ("g_final", 8)]:
        CC[name] = (c, n)
        c += n
    CC["_n"] = c
    return c


def _chan(v, n):
    return np.ascontiguousarray(np.asarray(v, np.float32).reshape(n, 128).T)


def make_consts(norm_final):
    n = _cc_layout()
    cb = np.zeros((128, n), np.float32)

    def put(name, arr):
        c0, k = CC[name]
        cb[:, c0:c0 + k] = arr

    i = np.arange(128)
    put("ident", np.eye(128, dtype=np.float32))
    tri = (i[:, None] <= i[None, :]).astype(np.float32)
    mstr = (i[:, None] < i[None, :]).astype(np.float32)
    put("tri", tri)
    put("mstrT", mstr)
    put("mlow", mstr.T)
    put("maskM", np.concatenate([mstr, tri, mstr, tri], axis=1))
    put("mbias", np.where(i[:, None] <= i[None, :], 0.0, -30000.0).astype(np.float32))
    bd = ((i[:, None] // 64) == (i[None, :] // 64)).astype(np.float32)
    put("bd", bd)
    partner = np.where(i % 64 < 32, i + 32, i - 32)
    sw = np.zeros((128, 128), np.float32)
    sw[partner, i] = 1.0
    put("swap", sw)
    rs = np.ones((128, 512), np.float32)
    rs[:, ::128] = 0.0
    put("reset", rs)
    gam = 1.0 - np.exp2(-5.0 - np.arange(4, dtype=np.float64))
    g128 = np.zeros((128, 2), np.float32)
    for c in range(2):
        for p in range(128):
            g128[p, c] = gam[2 * c + p // 64] ** 128
    put("g128", g128)
    put("eps6", 1e-6)
    put("eps5", 1e-5)
    put("eps64", 64e-5)
    put("eps12", 1e-12)
    put("one", 1.0)
    put("g_final", _chan(norm_final, 8))
    return cb


def make_rope(S):
    nt = S // TT
    pos = np.arange(S, dtype=np.float32)
    inv_freq = (10000.0 ** (-np.arange(32, dtype=np.float32) / 32.0)).astype(np.float32)
    ang = (pos[:, None] * inv_freq[None, :]).astype(np.float32)
    cos = np.cos(ang).astype(np.float64)
    sin = np.sin(ang).astype(np.float64)
    gam = 1.0 - np.exp2(-5.0 - np.arange(4, dtype=np.float64))
    tl = (np.arange(S) % 128) + 1
    out = np.zeros((nt, 128, 8, TT), np.float32)
    p = np.arange(128)
    n = p % 64
    j = n % 32
    sgn = np.where(n < 32, -1.0, 1.0)
    for c in range(2):
        h = 2 * c + p // 64
        gq = gam[h][:, None] ** tl[None, :]
        gk = gam[h][:, None] ** (-tl[None, :]) / 8.0
        cq = cos[:, j].T * gq
        sq = sin[:, j].T * sgn[:, None] * gq
        ck = cos[:, j].T * gk
        sk = sin[:, j].T * sgn[:, None] * gk
        for t in range(nt):
            sl = slice(t * TT, (t + 1) * TT)
            out[t, :, 0 + 2 * c, :] = cq[:, sl]
            out[t, :, 1 + 2 * c, :] = sq[:, sl]
            out[t, :, 4 + 2 * c, :] = ck[:, sl]
            out[t, :, 5 + 2 * c, :] = sk[:, sl]
    return out


def make_pvec(inp, L):
    n = _pv_layout()
    pv = np.zeros((L, 128, n), np.float32)

    def put(l, name, arr):
        c0, k = PV[name]
        pv[l, :, c0:c0 + k] = arr

    for l in range(L):
        put(l, "g_mix", _chan(inp["norm_mix"][l], 8))
        put(l, "lru_cw", np.concatenate([_chan(inp["lru_conv_w"][l][k], 2) for k in range(4)], axis=1))
        put(l, "lru_cb", _chan(inp["lru_conv_b"][l], 2))
        put(l, "lru_br", _chan(inp["lru_b_r"][l].reshape(-1), 2))
        put(l, "lru_bi", _chan(inp["lru_b_i"][l].reshape(-1), 2))
        put(l, "lru_lam", _chan(inp["lru_lambda"][l], 2))
        put(l, "ret_gw", _chan(inp["ret_gn_w"][l], 2))
        put(l, "ret_gb", _chan(inp["ret_gn_b"][l], 2))
        put(l, "rw_mu", _chan(inp["rwkv_mu"][l], 8))
        put(l, "rw_w0", _chan(inp["rwkv_w0"][l], 2))
        put(l, "rw_a0", _chan(inp["rwkv_a0"][l], 2))
        put(l, "rw_kk", _chan(inp["rwkv_k_k"][l], 2))
        put(l, "rw_ka", _chan(inp["rwkv_k_a"][l], 2))
        put(l, "rw_rk", _chan(inp["rwkv_r_k"][l], 2))
        put(l, "rw_gw", _chan(inp["rwkv_gn_w"][l], 2))
        put(l, "rw_gb", _chan(inp["rwkv_gn_b"][l], 2))
        put(l, "sd_cw", np.concatenate([_chan(inp["ssd_conv_w"][l][k], 6) for k in range(4)], axis=1))
        put(l, "sd_cb", _chan(inp["ssd_conv_b"][l], 6))
        put(l, "sd_d", _chan(np.repeat(np.asarray(inp["ssd_d"][l], np.float32), 64), 2))
        put(l, "sd_nw", _chan(inp["ssd_norm_w"][l], 2))
        put(l, "g_memq", _chan(inp["norm_mem_q"][l], 8))
        put(l, "g_memkv", _chan(inp["norm_mem_kv"][l], 8))
        put(l, "g_ffn", _chan(inp["norm_ffn"][l], 8))
        put(l, "sd_dtb", np.broadcast_to(np.asarray(inp["ssd_dt_bias"][l], np.float32)[None, :], (128, 4)))
        put(l, "sd_alog", np.broadcast_to(np.asarray(inp["ssd_a_log"][l], np.float32)[None, :], (128, 4)))
    return pv


def make_smallw(inp, L):
    sw = np.zeros((L, 128, 1280), np.float32)
    for l in range(L):
        for c in range(2):
            for hh in range(2):
                blk = slice(hh * 64, hh * 64 + 64)
                sw[l, blk, c * 128 + hh * 64: c * 128 + hh * 64 + 64] = inp["lru_w_r"][l][2 * c + hh]
                sw[l, blk, 256 + c * 128 + hh * 64: 256 + c * 128 + hh * 64 + 64] = inp["lru_w_i"][l][2 * c + hh]
        sw[l, 0:64, 512:768] = inp["rwkv_w2"][l]
        sw[l, 64:128, 768:1024] = inp["rwkv_a2"][l]
        sw[l, :, 1024:1280] = inp["rwkv_g2"][l]
    return sw


def build(L, S, NSEQ, flags=None):
    from contextlib import ExitStack
    flags = flags or {}
    EN = lambda k: flags.get(k, True)
    NT = S // TT
    NCHS = S // 128
    GT = 2 if NT % 2 == 0 else 1
    nc = bass.Bass("TRN2", target_bir_lowering=False)
    NPV = _pv_layout()
    NCC = _cc_layout()

    def dram(name, shape, kind="ExternalInput"):
        return nc.dram_tensor(name, shape, F32, kind=kind).ap()

    x_d = dram("x", [NSEQ * S, D])
    mem_d = dram("mem", [NSEQ * MEMT, D])
    cb_d = dram("cb", [128, NCC])
    rope_d = dram("rope", [NT, 128, 8, TT])
    pv_d = dram("pv", [L, 128, NPV])
    sw_d = dram("smallw", [L, 128, 1280])
    win_d = dram("w_in", [L, D, IN_COLS])
    wout_d = dram("w_out", [L, D, D])
    wq_d = dram("mem_wq", [L, D, D])
    wk_d = dram("mem_wk", [L, D, D])
    wv_d = dram("mem_wv", [L, D, D])
    wo_d = dram("mem_wo", [L, D, D])
    f1_d = dram("ffn_w_in", [L, D, 2 * FFH])
    f2_d = dram("ffn_w_out", [L, FFH, D])
    out_d = dram("out", [NSEQ * S, D], kind="ExternalOutput")

    NW = 52224
    with ExitStack() as es:
        arena_h = es.enter_context(nc.sbuf_tensor("arena", [128, NW], F32))
        banks = [es.enter_context(nc.psum_tensor(f"bank{i}", [128, 512], F32)) for i in range(8)]
        sems = {}
        for n in ("pe", "act", "dve", "pool", "sp"):
            sems[n] = es.enter_context(nc.semaphore("s_" + n))
        sems["dsp"] = [es.enter_context(nc.semaphore(f"dsp{i}")) for i in range(12)]
        sems["dpool"] = [es.enter_context(nc.semaphore(f"dpl{i}")) for i in range(12)]
        Pg = Prog(nc, sems)
        AR = Arena(arena_h, NW)
        PS = Psum([b[:, :] for b in banks])

        def _a(x):
            return x.ap if isinstance(x, T) else x

        def _rd(*xs):
            return [x for x in xs if isinstance(x, T)]

        def mm(out, lhsT, rhs, start=True, stop=True):
            Pg.op("pe", lambda e: e.matmul(out.ap, lhsT.ap, rhs.ap, start=start, stop=stop), [lhsT, rhs], [out])

        def tpose(out, in_, ident):
            Pg.op("pe", lambda e: e.transpose(out.ap, in_.ap, ident.ap), [in_, ident], [out])

        def act(out, in_, func, scale=1.0, bias=0.0):
            Pg.op("act", lambda e: e.activation(out=out.ap, in_=in_.ap, func=func, bias=_a(bias), scale=_a(scale)),
                  _rd(in_, scale, bias), [out])

        def tt(out, a, b, op, eng="dve"):
            Pg.op(eng, lambda e: e.tensor_tensor(out=out.ap, in0=a.ap, in1=b.ap, op=op), [a, b], [out])

        def ts(out, a, s1, op0, s2=None, op1=None, eng="dve"):
            kw = dict(out=out.ap, in0=a.ap, scalar1=_a(s1), scalar2=_a(s2), op0=op0)
            if op1 is not None:
                kw["op1"] = op1
            Pg.op(eng, lambda e: e.tensor_scalar(**kw), _rd(a, s1, s2), [out])

        def stt(out, a, s, b, op0, op1):
            Pg.op("dve", lambda e: e.scalar_tensor_tensor(out=out.ap, in0=a.ap, scalar=_a(s), in1=b.ap,
                                                          op0=op0, op1=op1), _rd(a, s, b), [out])

        def cp(out, a, eng="dve"):
            if eng == "act":
                Pg.op("act", lambda e: e.copy(out=out.ap, in_=a.ap), [a], [out])
            else:
                Pg.op(eng, lambda e: e.tensor_copy(out=out.ap, in_=a.ap), [a], [out])

        def scan(out, d0, d1, init):
            Pg.op("dve", lambda e: e.tensor_tensor_scan(out=out.ap, data0=d0.ap, data1=d1.ap, initial=_a(init),
                                                        op0=ALU.mult, op1=ALU.add), _rd(d0, d1, init), [out])

        def recip(out, a):
            Pg.op("dve", lambda e: e.reciprocal(out=out.ap, in_=a.ap), [a], [out])

        def mset(t, val, eng="dve"):
            Pg.op(eng, lambda e: e.memset(t.ap, val), [], [t])

        def wload(dst, src_ap):
            Pg.dma("pool", dst.ap, src_ap, writes=[dst])

        def ld(dst, src_ap):
            Pg.dma("sp", dst.ap, src_ap, writes=[dst])

        MUL, ADD, SUB = ALU.mult, ALU.add, ALU.subtract
        DBG = {}

        def dbg(name, t):
            if not flags.get("dbg") or name in DBG:
                return
            shp = list(t.ap.shape)
            d = nc.dram_tensor("dbg_" + name, shp, t.ap.dtype, kind="ExternalOutput").ap()
            DBG[name] = shp
            Pg.dma("sp", d, t.ap, reads=[t])

        xT = AR.tile([8, S])
        cb = AR.tile([NCC])
        pv = AR.tile([NPV])
        DVL = {}
        ndv = 0
        for name, n in [("hbr", 2), ("hbi", 2), ("cA", 2), ("hcA", 2), ("sdcwh", 24), ("sdcbh", 6), ("omm", 8),
                        ("hw0", 2), ("ha0", 2), ("omka", 2), ("aneg", 4), ("tmp", 8)]:
            DVL[name] = (ndv, n)
            ndv += n
        dv = AR.tile([ndv])
        smallw = AR.tile([1280], BF16)
        ident_bf = AR.tile([128], BF16)
        ones_bf = AR.tile([128], BF16)
        onesD_bf = AR.tile([128], BF16)
        ones128_bf = AR.tile([128], BF16)
        bd1_bf = AR.tile([128], BF16)
        bd64_bf = AR.tile([128], BF16)
        swap_bf = AR.tile([128], BF16)
        ones_f = AR.tile([128])
        memnT = AR.tile([8, MEMT], BF16)
        KT = AR.tile([8, MEMT], BF16)
        Vm = AR.tile([2, D], BF16)
        Vpad = AR.tile([4, 4, 128], BF16)
        Upad = [[AR.tile([128], BF16) for _ in range(2)] for _ in range(2)]
        SpadD = [[AR.tile([128], BF16) for _ in range(2)] for _ in range(2)]
        SD_f = [AR.tile([128]) for _ in range(2)]
        SB_f = [AR.tile([128]) for _ in range(2)]
        SB_bf = [AR.tile([128], BF16) for _ in range(2)]
        SC_f = [AR.tile([128]) for _ in range(2)]
        SC_bf = [AR.tile([128], BF16) for _ in range(2)]
        histA = [AR.tile([3]) for _ in range(2)]
        histD = [AR.tile([3]) for _ in range(6)]
        histC = [AR.tile([1]) for _ in range(8)]
        hprev = [AR.tile([1]) for _ in range(2)]
        stage = [AR.tile([3 + TT]) for _ in range(2)]
        PH0 = AR.mark()

        def C(name, a=None, b=None):
            c0, n = CC[name]
            if a is None:
                return cb[:, c0:c0 + n]
            return cb[:, c0 + a:c0 + b]

        def PVc(name, a=0, b=None):
            c0, n = PV[name]
            b = n if b is None else b
            return pv[:, c0 + a:c0 + b]

        def DVc(name, a=0, b=None):
            c0, n = DVL[name]
            b = n if b is None else b
            return dv[:, c0 + a:c0 + b]

        ident_f = C("ident")
        eps6, eps5, eps64, eps12, one_c = C("eps6"), C("eps5"), C("eps64"), C("eps12"), C("one")

        ld(cb, cb_d[:, :])
        cp(ident_bf, C("ident"))
        mset(ones_bf, 1.0)
        mset(onesD_bf, 1.0 / 1024.0)
        mset(ones128_bf, 1.0 / 128.0)
        mset(ones_f, 1.0)
        cp(bd1_bf, C("bd"))
        ts(bd64_bf, C("bd"), 1.0 / 64.0, MUL)
        cp(swap_bf, C("swap"))
        mset(Vpad, 0.0)
        for a_ in Upad + SpadD:
            for b_ in a_:
                mset(b_, 0.0)

        def rsqrt(out, in_, scale, eps_t):
            act(out, in_, AF.Ln, scale=scale, bias=eps_t)
            act(out, out, AF.Exp, scale=-0.5)

        def rmsnorm_tile(xsl, gcols, hT, sqt, rstd, n=TT):
            bank = PS.get(4)
            bk = bank[:, 0:n]
            for k in range(8):
                sq = sqt[k % 2]
                act(sq, xsl[:, k, :], AF.Square)
                mm(bk, onesD_bf, sq, start=(k == 0), stop=(k == 7))
            rsqrt(rstd, bk, 1.0, eps6)
            for k in range(8):
                if gcols is None:
                    tt(hT[:, k, :], xsl[:, k, :], rstd, MUL)
                else:
                    stt(hT[:, k, :], xsl[:, k, :], gcols[:, k:k + 1], rstd, MUL, MUL)

        def wsrc(w_d, l, r0, kc, c0, n):
            return w_d[l, r0:r0 + kc * 128, c0:c0 + n].rearrange("(k p) c -> p k c", p=128)

        def v4(t):
            return t.v(lambda a: a.rearrange("p (a b) -> p a b", a=4))

        def phase0(sq):
            m = AR.mark()
            PS.set_rot(range(8))
            xst = [AR.tile([D]) for _ in range(2)]
            memT = AR.tile([8, MEMT])
            sqt = [AR.tile([MEMT], BF16) for _ in range(2)]
            rstd = AR.tile([MEMT])
            for c in range(NCHS):
                st = xst[c % 2]
                ld(st, x_d[sq * S + c * 128: sq * S + (c + 1) * 128, :])
                for half in range(2):
                    bk = PS.get(4)
                    for q in range(4):
                        k = half * 4 + q
                        tpose(bk[:, q * 128:(q + 1) * 128], st[:, k * 128:(k + 1) * 128], ident_f)
                    cp(xT[:, half * 4:(half + 1) * 4, c * 128:(c + 1) * 128], v4(bk), eng=("act" if half else "dve"))
            for mc in range(2):
                st = xst[mc % 2]
                ld(st, mem_d[sq * MEMT + mc * 128: sq * MEMT + (mc + 1) * 128, :])
                for half in range(2):
                    bk = PS.get(4)
                    for q in range(4):
                        k = half * 4 + q
                        tpose(bk[:, q * 128:(q + 1) * 128], st[:, k * 128:(k + 1) * 128], ident_f)
                    cp(memT[:, half * 4:(half + 1) * 4, mc * 128:(mc + 1) * 128], v4(bk), eng=("act" if half else "dve"))
            rmsnorm_tile(memT, None, memnT, sqt, rstd, n=MEMT)
            Pg.barrier()
            AR.reset(m)

        def layer_setup(l):
            ld(pv, pv_d[l])
            wload(smallw, sw_d[l])
            ts(DVc("hbr"), PVc("lru_br"), 0.5, MUL)
            ts(DVc("hbi"), PVc("lru_bi"), 0.5, MUL)
            act(DVc("tmp", 0, 2), PVc("lru_lam"), AF.Exp, scale=-1.0)
            act(DVc("tmp", 0, 2), DVc("tmp", 0, 2), AF.Ln, scale=1.0, bias=one_c)
            ts(DVc("cA"), DVc("tmp", 0, 2), -8.0, MUL)
            ts(DVc("hcA"), DVc("tmp", 0, 2), -4.0, MUL)
            ts(DVc("sdcwh"), PVc("sd_cw"), 0.5, MUL)
            ts(DVc("sdcbh"), PVc("sd_cb"), 0.5, MUL)
            ts(DVc("omm"), PVc("rw_mu"), -1.0, MUL, 1.0, ADD)
            ts(DVc("hw0"), PVc("rw_w0"), 0.5, MUL)
            ts(DVc("ha0"), PVc("rw_a0"), 0.5, MUL)
            ts(DVc("omka"), PVc("rw_ka"), -1.0, MUL, 1.0, ADD)
            act(DVc("tmp", 4, 8), PVc("sd_alog"), AF.Exp)
            ts(DVc("aneg"), DVc("tmp", 4, 8), -1.0, MUL)

        def phase_ffn(l):
            m = AR.mark()
            PS.set_rot(range(8))
            GTOK = GT * TT
            hT2 = AR.tile([8, GTOK], BF16)
            actT = AR.tile([NJ, GTOK], BF16)
            sqt = [AR.tile([TT], BF16) for _ in range(2)]
            rstd = AR.tile([TT])
            tg = [AR.tile([TT]) for _ in range(2)]
            s1 = [AR.tile([TT]) for _ in range(2)]
            wgu = [AR.tile([2, 8, 256], BF16) for _ in range(2)]
            w2s = [AR.tile([NJ, 128], BF16) for _ in range(2)]
            for g in range(NT // GT):
                for ti in range(GT):
                    t0 = (g * GT + ti) * TT
                    rmsnorm_tile(xT[:, :, t0:t0 + TT], PVc("g_ffn"), hT2[:, :, ti * TT:(ti + 1) * TT], sqt, rstd)
                for jb in range(NJ // 2):
                    w = wgu[jb % 2]
                    wload(w[:, 0], wsrc(f1_d, l, 0, 8, jb * 256, 256))
                    wload(w[:, 1], wsrc(f1_d, l, 0, 8, FFH + jb * 256, 256))
                    for jj in range(2):
                        j = jb * 2 + jj
                        for ti in range(GT):
                            pg_ = PS.get(4)
                            pu_ = PS.get(4)
                            for k in range(8):
                                mm(pg_, w[:, 0, k, jj * 128:(jj + 1) * 128], hT2[:, k, ti * TT:(ti + 1) * TT],
                                   start=(k == 0), stop=(k == 7))
                            for k in range(8):
                                mm(pu_, w[:, 1, k, jj * 128:(jj + 1) * 128], hT2[:, k, ti * TT:(ti + 1) * TT],
                                   start=(k == 0), stop=(k == 7))
                            tgi, s1i = tg[(j * GT + ti) % 2], s1[(j * GT + ti) % 2]
                            act(tgi, pg_, AF.Tanh, scale=0.5)
                            stt(s1i, tgi, 1.0, pg_, ADD, MUL)
                            stt(actT[:, j, ti * TT:(ti + 1) * TT], s1i, 0.5, pu_, MUL, MUL)
                for d in range(8):
                    w = w2s[d % 2]
                    wload(w, f2_d[l, :, d * 128:(d + 1) * 128].rearrange("(j p) c -> p j c", p=128))
                    for ti in range(GT):
                        t0 = (g * GT + ti) * TT
                        bk = PS.get(4)
                        for j in range(NJ):
                            mm(bk, w[:, j, :], actT[:, j, ti * TT:(ti + 1) * TT], start=(j == 0), stop=(j == NJ - 1))
                        tt(xT[:, d, t0:t0 + TT], xT[:, d, t0:t0 + TT], bk, ADD)
            Pg.barrier()
            AR.reset(m)

        def phase_attn(l):
            m = AR.mark()
            PS.set_rot(range(8))
            wq = AR.tile([8, D], BF16)
            wo = AR.tile([8, D], BF16)
            wkv = [AR.tile([8, 512], BF16) for _ in range(2)]
            hT = AR.tile([8, TT], BF16)
            qT = AR.tile([8, TT], BF16)
            oT = AR.tile([8, TT], BF16)
            et = [AR.tile([TT], BF16) for _ in range(4)]
            rden = AR.tile([TT])
            sqt = [AR.tile([TT], BF16) for _ in range(2)]
            rstd = AR.tile([TT])
            hmT = AR.tile([8, MEMT], BF16)
            wload(wq, wsrc(wq_d, l, 0, 8, 0, D))
            for k in range(8):
                ts(hmT[:, k, :], memnT[:, k, :], PVc("g_memkv", k, k + 1), MUL)
            for half in range(2):
                w = wkv[half]
                wload(w, wsrc(wk_d, l, 0, 8, half * 512, 512))
                for cc in range(4):
                    bk = PS.get(2)
                    for k in range(8):
                        mm(bk, w[:, k, cc * 128:(cc + 1) * 128], hmT[:, k, :], start=(k == 0), stop=(k == 7))
                    cp(KT[:, half * 4 + cc, :], bk, eng="act")
            for half in range(2):
                w = wkv[half]
                wload(w, wsrc(wv_d, l, 0, 8, half * 512, 512))
                for mc in range(2):
                    bk = PS.get(4)
                    for k in range(8):
                        mm(bk, hmT[:, k, mc * 128:(mc + 1) * 128], w[:, k, :], start=(k == 0), stop=(k == 7))
                    cp(Vm[:, mc, half * 512:(half + 1) * 512], bk, eng="act")
            wload(wo, wsrc(wo_d, l, 0, 8, 0, D))
            for ti in range(NT):
                t0 = ti * TT
                rmsnorm_tile(xT[:, :, t0:t0 + TT], PVc("g_memq"), hT, sqt, rstd)
                for c in range(8):
                    bk = PS.get(4)
                    for k in range(8):
                        mm(bk, wq[:, k, c * 128:(c + 1) * 128], hT[:, k, :], start=(k == 0), stop=(k == 7))
                    cp(qT[:, c, :], bk, eng="act")
                for h in range(4):
                    es_ = []
                    for mc in range(2):
                        bk = PS.get(4)
                        for dc in range(2):
                            mm(bk, KT[:, 2 * h + dc, mc * 128:(mc + 1) * 128], qT[:, 2 * h + dc, :],
                               start=(dc == 0), stop=(dc == 1))
                        e_ = et[(h % 2) * 2 + mc]
                        act(e_, bk, AF.Exp, scale=1.0 / 16.0)
                        es_.append(e_)
                    den = PS.get(4)
                    for mc in range(2):
                        mm(den, ones_bf, es_[mc], start=(mc == 0), stop=(mc == 1))
                    recip(rden, den)
                    for dc in range(2):
                        bo = PS.get(4)
                        for mc in range(2):
                            mm(bo, Vm[:, mc, (2 * h + dc) * 128:(2 * h + dc + 1) * 128], es_[mc],
                               start=(mc == 0), stop=(mc == 1))
                        tt(oT[:, 2 * h + dc, :], bo, rden, MUL)
                for d in range(8):
                    bk = PS.get(4)
                    for k in range(8):
                        mm(bk, wo[:, k, d * 128:(d + 1) * 128], oT[:, k, :], start=(k == 0), stop=(k == 7))
                    tt(xT[:, d, t0:t0 + TT], xT[:, d, t0:t0 + TT], bk, ADD)
            Pg.barrier()
            AR.reset(m)

        def phase_final(sq):
            m = AR.mark()
            PS.set_rot(range(8))
            hF = AR.tile([8, TT])
            sqt = [AR.tile([TT], BF16) for _ in range(2)]
            rstd = AR.tile([TT])
            ost = [AR.tile([D]) for _ in range(2)]
            for ti in range(NT):
                t0 = ti * TT
                rmsnorm_tile(xT[:, :, t0:t0 + TT], C("g_final"), hF, sqt, rstd)
                for tc in range(4):
                    o = ost[tc % 2]
                    for half in range(2):
                        bk = PS.get(4)
                        for q in range(4):
                            k = half * 4 + q
                            tpose(bk[:, q * 128:(q + 1) * 128], hF[:, k, tc * 128:(tc + 1) * 128], ident_f)
                        cp(o[:, half * 512:(half + 1) * 512], bk, eng=("act" if half else "dve"))
                    r0 = sq * S + t0 + tc * 128
                    Pg.dma("sp", out_d[r0:r0 + 128, :], o.ap, reads=[o])
            Pg.barrier()
            AR.reset(m)

        PHASE1 = {}

        class Scratch:
            U = 64

            def __init__(self, nunits):
                self.n = nunits
                self.base = AR.top
                AR.top += nunits * self.U
                AR.peak = max(AR.peak, AR.top)
                assert AR.top <= AR.n, f"scratch overflow {AR.top} > {AR.n}"
                self.bufs = [Buf() for _ in range(nunits)]
                self.used = [False] * nunits
                self.owner = {}
                self.peak = 0

            def _get(self, nwords):
                k = (nwords + self.U - 1) // self.U
                i = 0
                while i + k <= self.n:
                    j = i
                    while j < i + k and not self.used[j]:
                        j += 1
                    if j == i + k:
                        for q in range(i, i + k):
                            self.used[q] = True
                        self.peak = max(self.peak, sum(self.used))
                        return i, k
                    i = j + 1
                raise AssertionError(f"scratch pool exhausted (need {k} units, used {sum(self.used)}/{self.n})")

            def f32(self, n):
                i, k = self._get(n)
                ap = arena_h[:, self.base + i * self.U: self.base + i * self.U + n]
                t = T(ap, self.bufs[i:i + k])
                self.owner[id(t)] = (i, k)
                return t

            def bf(self, n):
                i, k = self._get((n + 1) // 2)
                ap = arena_h[:, self.base + i * self.U: self.base + i * self.U + (n + 1) // 2].bitcast(BF16)
                t = T(ap, self.bufs[i:i + k])
                self.owner[id(t)] = (i, k)
                return t

            def free(self, *ts_):
                for t in ts_:
                    i, k = self.owner.pop(id(t))
                    for q in range(i, i + k):
                        self.used[q] = False

        def bc_mid(t, n):
            return t.v(lambda a: a.unsqueeze(1).broadcast_to([128, n, a.shape[1]]))

        def bc_last(t, n):
            return t.v(lambda a: a.unsqueeze(2).broadcast_to([128, a.shape[1], n]))

        def r3(t, a_):
            return t.v(lambda x: x.rearrange("p (a b) -> p a b", a=a_))

        def r4(t, a_, b_):
            return t.v(lambda x: x.rearrange("p (a b c) -> p a b c", a=a_, b=b_))

        VpadV = Vpad.v(lambda a: a.rearrange("p t (c h) n -> p t c h n", c=2))

        def phase_mix(l):
            m = AR.mark()
            PS.set_rot([2, 3, 4, 5, 6, 7])
            wsl = [AR.tile([8, 512], BF16) for _ in range(3)]
            hT = AR.tile([8, TT], BF16)
            yT = AR.tile([8, TT], BF16)
            sqt = [AR.tile([TT], BF16) for _ in range(2)]
            rstd = AR.tile([TT])
            tri4 = AR.tile([TT])
            for i4 in range(4):
                cp(tri4[:, i4 * 128:(i4 + 1) * 128], C("tri"))
            SC = Scratch((AR.n - AR.top) // Scratch.U)
            for t_ in SB_f + SC_f + SD_f + SB_bf + SC_bf + histA + histD + histC + hprev:
                mset(t_, 0.0)
            for a_ in SpadD:
                for b_ in a_:
                    mset(b_, 0.0)
            if not (EN("A") and EN("B") and EN("C") and EN("D")):
                mset(yT, 0.0)

            GROUPS = [(g * 512, 512) for g in range(7)] + [(3584, 4)]
            seq = []
            for ti in range(NT):
                for g in range(8):
                    seq.append(("in", g))
                seq.append(("o", 0))
                seq.append(("o", 1))
            loaded = {}
            state = dict(nload=0)

            def issue_load():
                i = state["nload"]
                if i >= len(seq):
                    return
                kind, g = seq[i]
                slot = wsl[i % 3]
                if kind == "in":
                    c0, n = GROUPS[g]
                    wload(slot[:, :, 0:n], wsrc(win_d, l, 0, 8, c0, n))
                else:
                    wload(slot, wsrc(wout_d, l, 0, 8, g * 512, 512))
                loaded[i] = slot
                state["nload"] += 1

            issue_load()
            issue_load()
            issue_load()
            cur = dict(i=0)

            def next_slot():
                i = cur["i"]
                cur["i"] += 1
                return loaded.pop(i)

            release = issue_load

            def proj(slot, c0, rows=128):
                bk = PS.get(4)
                for k in range(8):
                    mm(bk[0:rows] if rows != 128 else bk, slot[:, k, c0:c0 + rows], hT[:, k, :],
                       start=(k == 0), stop=(k == 7))
                return bk

            def conv_stage(bk, hist, idx, wk, bias):
                st = stage[idx % 2]
                cp(st[:, 0:3], hist)
                cp(st[:, 3:3 + TT], bk, eng="act")
                cv = SC.f32(TT)
                ts(cv, st[:, 0:TT], wk(0), MUL, bias, ADD)
                for k in range(1, 4):
                    stt(cv, st[:, k:k + TT], wk(k), cv, MUL, ADD)
                cp(hist, st[:, TT:TT + 3])
                return cv

            def group_norm(ybank, eps_t, gw, gb):
                ysb = SC.bf(TT)
                cp(ysb, ybank, eng="act")
                ysq = SC.bf(TT)
                act(ysq, ybank, AF.Square)
                pm = PS.get(4)
                mm(pm, bd64_bf, ysb)
                pq = PS.get(4)
                mm(pq, bd64_bf, ysq)
                mean = SC.f32(TT)
                cp(mean, pm, eng="act")
                var = SC.f32(TT)
                stt(var, mean, -1.0, mean, MUL, MUL)
                tt(var, pq, var, ADD)
                ts(var, var, 0.0, ALU.max)
                rsqrt(var, var, 1.0, eps_t)
                yc = SC.f32(TT)
                tt(yc, ybank, mean, SUB)
                tt(yc, yc, var, MUL)
                ts(yc, yc, gw, MUL, gb, ADD)
                SC.free(ysb, ysq, mean, var)
                return yc

            tri_b4 = bc_mid(C("tri"), 4)
            mbias_b4 = bc_mid(C("mbias"), 4)

            for ti in range(NT):
                t0 = ti * TT
                xsl = xT[:, :, t0:t0 + TT]
                rmsnorm_tile(xsl, PVc("g_mix"), hT, sqt, rstd)
                yb = [PS.fixed(0), PS.fixed(1)]

                slot = next_slot()
                if not EN("A"):
                    release()
                if EN("A"):
                    for c in range(2):
                        pg_ = proj(slot, c * 128)
                        px_ = proj(slot, 256 + c * 128)
                        gt = SC.f32(TT)
                        cp(gt, pg_, eng="act")
                        cv = conv_stage(px_, histA[c], c, lambda k: PVc("lru_cw", k * 2 + c, k * 2 + c + 1),
                                        PVc("lru_cb", c, c + 1))
                        cvb = SC.bf(TT)
                        cp(cvb, cv, eng="act")
                        pr = PS.get(4)
                        mm(pr, smallw[:, c * 128:(c + 1) * 128], cvb)
                        pi_ = PS.get(4)
                        mm(pi_, smallw[:, 256 + c * 128:256 + (c + 1) * 128], cvb)
                        tr = SC.f32(TT)
                        act(tr, pr, AF.Tanh, scale=0.5, bias=DVc("hbr", c, c + 1))
                        ti_ = SC.f32(TT)
                        act(ti_, pi_, AF.Tanh, scale=0.5, bias=DVc("hbi", c, c + 1))
                        a_ = SC.f32(TT)
                        act(a_, tr, AF.Exp, scale=DVc("hcA", c, c + 1), bias=DVc("hcA", c, c + 1))
                        s_ = SC.f32(TT)
                        act(s_, tr, AF.Exp, scale=DVc("cA", c, c + 1), bias=DVc("cA", c, c + 1))
                        act(s_, s_, AF.Ln, scale=-1.0, bias=one_c)
                        act(s_, s_, AF.Exp, scale=0.5)
                        stt(ti_, ti_, 1.0, cv, ADD, MUL)
                        stt(ti_, ti_, 0.5, s_, MUL, MUL)
                        scan(tr, a_, ti_, hprev[c])
                        cp(hprev[c], tr[:, TT - 1:TT])
                        act(s_, gt, AF.Square)
                        ts(s_, s_, 0.044715, MUL, 1.0, ADD)
                        tt(s_, s_, gt, MUL)
                        act(s_, s_, AF.Tanh, scale=0.7978845608028654)
                        stt(s_, s_, 1.0, gt, ADD, MUL)
                        stt(yT[:, c, :], s_, 0.5, tr, MUL, MUL)
                        SC.free(gt, cv, cvb, tr, ti_, a_, s_)
                    release()

                slot1 = next_slot()
                slot2 = next_slot()
                if not EN("B"):
                    release()
                    release()
                if EN("B"):
                    rope = SC.f32(8 * TT)
                    rope3 = r3(rope, 8)
                    for i8 in range(8):
                        ld(rope3[:, i8, :], rope_d[ti, :, i8, :])
                    qk = [[SC.bf(TT) for _ in range(2)] for _ in range(2)]
                    for is_k in range(2 if "Q" not in flags.get("Bskip", "") else 0):
                        for c in range(2):
                            bk = proj(slot1, is_k * 256 + c * 128)
                            xb = SC.bf(TT)
                            cp(xb, bk, eng="act")
                            p2 = PS.get(4)
                            mm(p2, swap_bf, xb)
                            t1 = SC.f32(TT)
                            tt(t1, bk, rope3[:, 4 * is_k + 2 * c + 0, :], MUL)
                            t2 = SC.f32(TT)
                            tt(t2, p2, rope3[:, 4 * is_k + 2 * c + 1, :], MUL)
                            tt(qk[is_k][c], t1, t2, ADD)
                            SC.free(xb, t1, t2)
                    SC.free(rope)
                    release()
                    qT_, kT_ = qk
                    dbg("B_q0", qT_[0]); dbg("B_k0", kT_[0]); dbg("B_hT", hT)
                    ktok = SC.bf(4 * 256)
                    ktok3 = r3(ktok, 4)
                    vt = SC.bf(4 * 256)
                    vt3 = r3(vt, 4)
                    for tc in range(4):
                        tcs = slice(tc * 128, (tc + 1) * 128)
                        if "T" not in flags.get("Bskip", ""):
                            pt = PS.get(1, BF16)
                            for c in range(2):
                                tpose(pt[:, c * 128:(c + 1) * 128], kT_[c][:, tcs], ident_bf)
                            cp(ktok3[:, tc, :], pt, eng="act")
                        if "v" in flags.get("Bskip", ""):
                            continue
                        bv = PS.get(2)
                        for k in range(8):
                            mm(bv, hT[:, k, tcs], slot2[:, k, 0:256], start=(k == 0), stop=(k == 7))
                        cp(vt3[:, tc, :], bv, eng="act")
                        bv4 = r4(vt3[:, tc, :], 2, 2)
                        for hh in range(2 if "V" not in flags.get("Bskip", "") else 0):
                            ts(VpadV[:, tc, :, hh, hh * 64:(hh + 1) * 64], bv4[:, :, hh, :], 1.0, MUL)
                    sg = []
                    for c in range(2 if "G" not in flags.get("Bskip", "") else 0):
                        bk = proj(slot2, 256 + c * 128)
                        tg_ = SC.f32(TT)
                        act(tg_, bk, AF.Tanh, scale=0.5)
                        s = SC.f32(TT)
                        stt(s, tg_, 1.0, bk, ADD, MUL)
                        SC.free(tg_)
                        sg.append(s)
                    release()
                    sms = []
                    for tc in range(4):
                        tcs = slice(tc * 128, (tc + 1) * 128)
                        sm = SC.bf(TT)
                        sm4 = r4(sm, 2, 2)
                        for hh in range(2):
                            r0 = hh * 64
                            sbh = PS.get(2)
                            for c in range(2):
                                mm(sbh[:, c * 128:(c + 1) * 128], kT_[c][r0:r0 + 64, tcs], qT_[c][r0:r0 + 64, tcs])
                            tt(sm4[:, :, hh, :], r3(sbh, 2), r3(tri4[:, 0:256], 2), MUL)
                        sms.append(sm)
                    for tc in range(4):
                        tcs = slice(tc * 128, (tc + 1) * 128)
                        sm = sms[tc]
                        for c in range(2):
                            ycol = yb[c][:, tcs]
                            mm(ycol, SB_bf[c], qT_[c][:, tcs], start=True, stop=False)
                            for hh in range(2):
                                h = 2 * c + hh
                                mm(ycol, Vpad[:, tc, h, :], sm[:, h * 128:(h + 1) * 128], start=False, stop=(hh == 1))
                            pS = PS.get(1)
                            mm(pS, ktok3[:, tc, c * 128:(c + 1) * 128], vt3[:, tc, c * 128:(c + 1) * 128])
                            tmp = SC.f32(128)
                            stt(tmp, pS, C("g128", c, c + 1), C("bd"), MUL, MUL)
                            stt(SB_f[c], SB_f[c], C("g128", c, c + 1), tmp, MUL, ADD)
                            cp(SB_bf[c], SB_f[c], eng="act")
                            SC.free(tmp)
                        SC.free(sm)
                    if flags.get("dbg"):
                        dbg("B_vt", vt); dbg("B_ktok", ktok); dbg("B_sg0", sg[0]); dbg("B_Vpad", Vpad)
                    for c in range(2 if flags.get("Bstage", 3) >= 3 else 0):
                        ydb = SC.f32(TT)
                        cp(ydb, yb[c])
                        dbg("B_y%d" % c, ydb)
                        SC.free(ydb)
                        yc = group_norm(yb[c], eps5, PVc("ret_gw", c, c + 1), PVc("ret_gb", c, c + 1))
                        dbg("B_yn%d" % c, yc)
                        stt(yT[:, 2 + c, :], yc, 0.5, sg[c], MUL, MUL)
                        SC.free(yc)
                    SC.free(ktok, vt, *sg, *qT_, *kT_)
                elif False:
                    pass

                slot3 = next_slot()
                slot4 = next_slot()
                if not EN("C"):
                    release()
                    release()
                if EN("C"):
                    def shiftC(idx, bk):
                        st = stage[idx % 2]
                        cp(st[:, 2:3], histC[idx])
                        cp(st[:, 3:3 + TT], bk, eng="act")
                        o = SC.f32(TT)
                        ts(o, st[:, 3:3 + TT], DVc("omm", idx, idx + 1), MUL)
                        stt(o, st[:, 2:2 + TT], PVc("rw_mu", idx, idx + 1), o, MUL, ADD)
                        cp(histC[idx], st[:, 2 + TT:3 + TT])
                        return o

                    rf = [shiftC(c, proj(slot3, c * 128)) for c in range(2)]
                    kf = [shiftC(2 + c, proj(slot3, 256 + c * 128)) for c in range(2)]
                    release()
                    vf = [shiftC(4 + c, proj(slot4, c * 128)) for c in range(2)]
                    lo6 = shiftC(6, proj(slot4, 256))
                    lo7 = shiftC(7, proj(slot4, 384))
                    release()
                    tw = SC.bf(TT)
                    act(tw[0:64], lo6[0:64], AF.Tanh)
                    al = SC.bf(TT)
                    cp(al[64:128], lo6[64:128], eng="act")
                    sgl = SC.bf(TT)
                    act(lo7, lo7, AF.Tanh, scale=0.5)
                    ts(sgl, lo7, 0.5, MUL, 0.5, ADD)
                    SC.free(lo6, lo7)
                    gT_, bon, Wt, ARt, Ktl, Btl, vb, ARb = [], [], [], [], [], [], [], []
                    for c in range(2):
                        th = SC.f32(TT)
                        pw = PS.get(4)
                        mm(pw, smallw[0:64, 512 + c * 128:512 + (c + 1) * 128], tw[0:64])
                        logw = SC.f32(TT)
                        act(th, pw, AF.Tanh, scale=0.5, bias=DVc("hw0", c, c + 1))
                        ts(logw, th, -0.3032653298563167, MUL, -0.3032653298563167, ADD)
                        pa = PS.get(4)
                        mm(pa, smallw[64:128, 768 + c * 128:768 + (c + 1) * 128], al[64:128])
                        a_ = SC.f32(TT)
                        act(th, pa, AF.Tanh, scale=0.5, bias=DVc("ha0", c, c + 1))
                        ts(a_, th, 0.5, MUL, 0.5, ADD)
                        pg_ = PS.get(4)
                        mm(pg_, smallw[:, 1024 + c * 128:1024 + (c + 1) * 128], sgl)
                        g_ = SC.f32(TT)
                        cp(g_, pg_, eng="act")
                        gT_.append(g_)
                        kq = SC.f32(TT)
                        ts(kq, kf[c], PVc("rw_kk", c, c + 1), MUL)
                        ksq = SC.bf(TT)
                        act(ksq, kq, AF.Square)
                        pk = PS.get(4)
                        mm(pk, bd1_bf, ksq)
                        rsqrt(th, pk, 1.0, eps12)
                        tt(kq, kq, th, MUL)
                        SC.free(ksq)
                        ts(th, a_, PVc("rw_ka", c, c + 1), MUL, DVc("omka", c, c + 1), ADD)
                        kp = SC.f32(TT)
                        tt(kp, kf[c], th, MUL)
                        tt(th, a_, kq, MUL)
                        SC.free(a_, kf[c])
                        rk = SC.bf(TT)
                        stt(rk, rf[c], PVc("rw_rk", c, c + 1), kp, MUL, MUL)
                        pbn = PS.get(4)
                        mm(pbn, bd1_bf, rk)
                        bo = SC.f32(TT)
                        tt(bo, pbn, vf[c], MUL)
                        bon.append(bo)
                        SC.free(rk)
                        cum = SC.f32(TT)
                        scan(cum, C("reset"), logw, 0.0)
                        wt = SC.f32(TT)
                        act(wt, cum, AF.Exp)
                        Wt.append(wt)
                        wi = SC.f32(TT)
                        act(wi, cum, AF.Exp, scale=-1.0)
                        tt(cum, cum, logw, SUB)
                        act(cum, cum, AF.Exp)
                        SC.free(logw)
                        art = SC.bf(2 * TT)
                        art4 = r4(art, 4, 2)
                        ARb.append(art)
                        tt(art4[:, :, 0, :], r3(kq, 4), r3(cum, 4), MUL)
                        tt(art4[:, :, 1, :], r3(rf[c], 4), r3(wt, 4), MUL)
                        ARt.append(art4)
                        kt_ = SC.bf(TT)
                        tt(kt_, kp, wi, MUL)
                        bt_ = SC.bf(TT)
                        tt(bt_, th, wi, MUL)
                        Ktl.append(kt_)
                        Btl.append(bt_)
                        v_ = SC.bf(TT)
                        cp(v_, vf[c], eng="act")
                        vb.append(v_)
                        SC.free(th, kq, kp, cum, wi, rf[c], vf[c])
                    SC.free(tw, al, sgl)
                    Vt = SC.bf(4 * 256)
                    Vt3 = r3(Vt, 4)
                    Ktok = SC.bf(4 * 256)
                    Ktok3 = r3(Ktok, 4)
                    Btok = SC.bf(4 * 256)
                    Btok3 = r3(Btok, 4)
                    for tc in range(4):
                        tcs = slice(tc * 128, (tc + 1) * 128)
                        for src, dst, pad in ((vb, Vt3, True), (Ktl, Ktok3, False), (Btl, Btok3, False)):
                            pt = PS.get(1, BF16)
                            for c in range(2):
                                tpose(pt[:, c * 128:(c + 1) * 128], src[c][:, tcs], ident_bf)
                            cp(dst[:, tc, :], pt, eng="act")
                            if pad:
                                pt4 = r4(dst[:, tc, :], 2, 2)
                                for hh in range(2):
                                    ts(VpadV[:, tc, :, hh, hh * 64:(hh + 1) * 64], pt4[:, :, hh, :], 1.0, MUL)
                    SC.free(*vb)
                    uset = 0
                    Ckeep = []
                    for tc in range(4):
                        tcs = slice(tc * 128, (tc + 1) * 128)
                        Mxs = []
                        L4 = SC.bf(TT)
                        P4 = SC.bf(TT)
                        for h in range(4):
                            c, hh = h // 2, h % 2
                            r0 = hh * 64
                            arh = ARt[c][r0:r0 + 64, tc].v(lambda a: a.rearrange("p a b -> p (a b)"))
                            pM = PS.get(4)
                            mm(pM[:, 0:256], Btl[c][r0:r0 + 64, tcs], arh)
                            mm(pM[:, 256:512], Ktl[c][r0:r0 + 64, tcs], arh)
                            pL = PS.get(1)
                            mm(pL, ARt[c][r0:r0 + 64, tc, 0, :], Btl[c][r0:r0 + 64, tcs])
                            Mx = SC.bf(TT)
                            tt(Mx, pM, C("maskM"), MUL)
                            tt(L4[:, h * 128:(h + 1) * 128], pL, C("mlow"), MUL)
                            tt(P4[:, h * 128:(h + 1) * 128], ident_bf, Mx[:, 0:128], SUB)
                            Mxs.append(Mx)
                        A4 = None
                        for lev in range(1, 7):
                            def Aof(h):
                                return Mxs[h][:, 0:128] if A4 is None else A4[:, h * 128:(h + 1) * 128]
                            bankL = PS.get(4)
                            for h in range(4):
                                mm(bankL[:, h * 128:(h + 1) * 128], Aof(h), L4[:, h * 128:(h + 1) * 128])
                            L4n = SC.bf(TT)
                            cp(L4n, bankL, eng="act")
                            A4n = None
                            if lev < 6:
                                bankA = PS.get(4)
                                for h in range(4):
                                    mm(bankA[:, h * 128:(h + 1) * 128], L4[:, h * 128:(h + 1) * 128], Aof(h))
                                A4n = SC.bf(TT)
                                cp(A4n, bankA)
                            bankP = PS.get(4)
                            for h in range(4):
                                mm(bankP[:, h * 128:(h + 1) * 128], L4n[:, h * 128:(h + 1) * 128],
                                   P4[:, h * 128:(h + 1) * 128])
                            P4n = SC.bf(TT)
                            tt(P4n, bankP, P4, ADD)
                            SC.free(P4, L4)
                            if A4 is not None:
                                SC.free(A4)
                            P4, L4, A4 = P4n, L4n, A4n
                        SC.free(L4)
                        Ckeep.append((Mxs, P4))
                    for tc in range(4):
                        tcs = slice(tc * 128, (tc + 1) * 128)
                        Mxs, P4 = Ckeep[tc]
                        for c in range(2):
                            up = Upad[uset % 2]
                            uset += 1
                            pXs, Xbs = [], []
                            for hh in range(2):
                                h = 2 * c + hh
                                r0 = hh * 64
                                Mx = Mxs[h]
                                pX = PS.get(1)[:, 0:64]
                                mm(pX, ARt[c][:, tc, 0, :], SC_bf[c][:, r0:r0 + 64], start=True, stop=False)
                                mm(pX, Mx[:, 256:384], Vt3[:, tc, h * 64:(h + 1) * 64], start=False, stop=True)
                                pXs.append(pX)
                            for hh in range(2):
                                Xb = SC.bf(64)
                                if hh == 0:
                                    cp(Xb, pXs[hh], eng="act")
                                else:
                                    cp(Xb, pXs[hh])
                                Xbs.append(Xb)
                            pUs = []
                            for hh in range(2):
                                h = 2 * c + hh
                                pU = PS.get(1)[:, 0:64]
                                mm(pU, P4[:, h * 128:(h + 1) * 128], Xbs[hh])
                                pUs.append(pU)
                            for hh in range(2):
                                r0 = hh * 64
                                if hh == 0:
                                    act(up[hh][:, r0:r0 + 64], pUs[hh], AF.Copy, scale=-1.0)
                                else:
                                    ts(up[hh][:, r0:r0 + 64], pUs[hh], -1.0, MUL)
                            SC.free(*Xbs)
                            ycol = yb[c][:, tcs]
                            mm(ycol, SC_bf[c], ARt[c][:, tc, 1, :], start=True, stop=False)
                            for hh in range(2):
                                h = 2 * c + hh
                                mm(ycol, Vpad[:, tc, h, :], Mxs[h][:, 384:512], start=False, stop=False)
                                mm(ycol, up[hh], Mxs[h][:, 128:256], start=False, stop=(hh == 1))
                            pS = PS.get(1)
                            kc_ = Ktok3[:, tc, c * 128:(c + 1) * 128]
                            bc_ = Btok3[:, tc, c * 128:(c + 1) * 128]
                            mm(pS, kc_, Vpad[:, tc, 2 * c, :], start=True, stop=False)
                            mm(pS, kc_, Vpad[:, tc, 2 * c + 1, :], start=False, stop=False)
                            mm(pS, bc_, up[0], start=False, stop=False)
                            mm(pS, bc_, up[1], start=False, stop=True)
                            wc = Wt[c][:, tc * 128 + 127:tc * 128 + 128]
                            tmp = SC.f32(128)
                            stt(tmp, pS, wc, C("bd"), MUL, MUL)
                            stt(SC_f[c], SC_f[c], wc, tmp, MUL, ADD)
                            cp(SC_bf[c], SC_f[c], eng="act")
                            SC.free(tmp)
                        SC.free(P4, *Mxs)
                    for c in range(2):
                        yc = group_norm(yb[c], eps64, PVc("rw_gw", c, c + 1), PVc("rw_gb", c, c + 1))
                        tt(yc, yc, bon[c], ADD)
                        tt(yT[:, 4 + c, :], yc, gT_[c], MUL)
                        SC.free(yc)
                    for lst in (gT_, bon, Wt, Ktl, Btl):
                        SC.free(*lst)
                    SC.free(Vt, Ktok, Btok)
                    SC.free(*ARb)

                slot5 = next_slot()
                slot6 = next_slot()
                slot7 = next_slot()
                if not EN("D"):
                    release()
                    release()
                    release()
                if EN("D"):
                    sz = []
                    for c in range(2):
                        bk = proj(slot5, c * 128)
                        tg_ = SC.f32(TT)
                        act(tg_, bk, AF.Tanh, scale=0.5)
                        s = SC.f32(TT)
                        stt(s, tg_, 1.0, bk, ADD, MUL)
                        SC.free(tg_)
                        sz.append(s)

                    def convD(idx, bk):
                        cv = conv_stage(bk, histD[idx], idx, lambda k: DVc("sdcwh", k * 6 + idx, k * 6 + idx + 1),
                                        DVc("sdcbh", idx, idx + 1))
                        th = SC.f32(TT)
                        act(th, cv, AF.Tanh)
                        return cv, th

                    xsf, xsb, BT_, CT_ = [], [], [], []
                    for c in range(2):
                        cv, th = convD(c, proj(slot5, 256 + c * 128))
                        stt(cv, th, 1.0, cv, ADD, MUL)
                        b_ = SC.bf(TT)
                        cp(b_, cv, eng="act")
                        xsf.append(cv)
                        xsb.append(b_)
                        SC.free(th)
                    release()
                    for g in range(2):
                        cv, th = convD(2 + g, proj(slot6, g * 128))
                        b_ = SC.bf(TT)
                        stt(b_, th, 1.0, cv, ADD, MUL)
                        BT_.append(b_)
                        SC.free(cv, th)
                    for g in range(2):
                        cv, th = convD(4 + g, proj(slot6, 256 + g * 128))
                        b_ = SC.bf(TT)
                        stt(b_, th, 1.0, cv, ADD, MUL)
                        CT_.append(b_)
                        SC.free(cv, th)
                    release()
                    pdt = PS.get(1)
                    for tc in range(4):
                        for k in range(8):
                            mm(pdt[:, tc * 4:(tc + 1) * 4], hT[:, k, tc * 128:(tc + 1) * 128], slot7[:, k, 0:4],
                               start=(k == 0), stop=(k == 7))
                    release()
                    dt = SC.f32(16)
                    tt(r3(dt, 4), r3(pdt[:, 0:16], 4), bc_mid(PVc("sd_dtb"), 4), ADD)
                    act(dt, dt, AF.Exp)
                    act(dt, dt, AF.Ln, scale=1.0, bias=one_c)
                    la = SC.f32(16)
                    tt(r3(la, 4), r3(dt, 4), bc_mid(DVc("aneg"), 4), MUL)
                    pc = PS.get(1)
                    mm(pc[:, 0:16], C("tri"), la)
                    cum = SC.f32(16)
                    cp(cum, pc[:, 0:16], eng="act")
                    Btok = SC.bf(4 * 256)
                    Btok3 = r3(Btok, 4)
                    Dkeep = []
                    for tc in range(4):
                        tcs = slice(tc * 128, (tc + 1) * 128)
                        h4 = slice(tc * 4, (tc + 1) * 4)
                        px = PS.get(1, BF16)
                        for c in range(2):
                            tpose(px[:, c * 128:(c + 1) * 128], xsb[c][:, tcs], ident_bf)
                        pb = PS.get(1, BF16)
                        for g in range(2):
                            tpose(pb[:, g * 128:(g + 1) * 128], BT_[g][:, tcs], ident_bf)
                        cp(Btok3[:, tc, :], pb, eng="act")
                        lat = SC.f32(TT)
                        tt(r3(lat, 4), tri_b4, bc_last(la[:, h4], 128), MUL)
                        pcb = PS.get(4)
                        mm(pcb, ones_f, lat)
                        dif = SC.f32(TT)
                        tt(r3(dif, 4), r3(pcb, 4), bc_last(cum[:, h4], 128), SUB)
                        tt(r3(dif, 4), r3(dif, 4), mbias_b4, ADD)
                        act(dif, dif, AF.Exp)
                        E4 = SC.f32(TT)
                        act(E4, pcb, AF.Exp)
                        E43 = r3(E4, 4)
                        we = SC.f32(4)
                        tt(we, r3(pcb, 4)[:, :, 127], cum[:, h4], SUB)
                        act(we, we, AF.Exp)
                        tt(we, we, dt[:, h4], MUL)
                        px4 = r4(px, 2, 2)
                        dt22 = r3(dt[:, h4], 2)
                        for hh in range(2):
                            tt(VpadV[:, tc, :, hh, hh * 64:(hh + 1) * 64], px4[:, :, hh, :],
                               bc_last(dt22[:, :, hh], 64), MUL)
                        vw = SC.bf(256)
                        tt(r3(vw, 4), r3(px, 4), bc_last(we, 64), MUL)
                        psc = PS.get(2)
                        for g in range(2):
                            mm(psc[:, g * 128:(g + 1) * 128], BT_[g][:, tcs], CT_[g][:, tcs])
                        sm = SC.bf(TT)
                        dif4 = r4(dif, 2, 2)
                        sm4 = r4(sm, 2, 2)
                        Cs = SC.bf(TT)
                        Cs4 = r4(Cs, 2, 2)
                        E44 = r4(E4, 2, 2)
                        for g in range(2):
                            tt(sm4[:, g], bc_mid(psc[:, g * 128:(g + 1) * 128], 2), dif4[:, g], MUL)
                            tt(Cs4[:, g], bc_mid(CT_[g][:, tcs], 2), E44[:, g], MUL)
                        etot = SC.f32(4)
                        cp(etot, E43[:, :, 127], eng="act")
                        Dkeep.append((sm, Cs, vw, etot))
                        SC.free(lat, dif, E4, we)
                    for tc in range(4):
                        tcs = slice(tc * 128, (tc + 1) * 128)
                        sm, Cs, vw, etot = Dkeep[tc]
                        for g in range(2):
                            ycol = yb[g][:, tcs]
                            for hh in range(2):
                                h = 2 * g + hh
                                mm(ycol, SpadD[g][hh], Cs[:, h * 128:(h + 1) * 128], start=(hh == 0), stop=False)
                                mm(ycol, Vpad[:, tc, h, :], sm[:, h * 128:(h + 1) * 128], start=False, stop=(hh == 1))
                            pS = PS.get(1)
                            mm(pS, Btok3[:, tc, g * 128:(g + 1) * 128], vw[:, g * 128:(g + 1) * 128])
                            sd3 = r3(SD_f[g], 2)
                            tt(sd3, sd3, bc_last(etot[:, 2 * g:2 * g + 2], 64), MUL)
                            tt(SD_f[g], SD_f[g], pS, ADD)
                            for hh in range(2):
                                cp(SpadD[g][hh][:, hh * 64:(hh + 1) * 64], SD_f[g][:, hh * 64:(hh + 1) * 64], eng="act")
                        SC.free(vw, sm, Cs, etot)
                    for g in range(2):
                        y1 = SC.f32(TT)
                        stt(y1, xsf[g], PVc("sd_d", g, g + 1), yb[g], MUL, ADD)
                        stt(y1, y1, 0.5, sz[g], MUL, MUL)
                        ysq = SC.bf(TT)
                        act(ysq, y1, AF.Square)
                        pm = PS.get(4)
                        mm(pm, ones128_bf, ysq)
                        r_ = SC.f32(TT)
                        rsqrt(r_, pm, 1.0, eps6)
                        stt(yT[:, 6 + g, :], y1, PVc("sd_nw", g, g + 1), r_, MUL, MUL)
                        SC.free(y1, ysq, r_)
                    SC.free(dt, la, cum, Btok, *sz, *xsf, *xsb, *BT_, *CT_)

                so = [next_slot(), next_slot()]
                for d in range(8):
                    bk = PS.get(4)
                    w = so[d // 4]
                    for k in range(8):
                        mm(bk, w[:, k, (d % 4) * 128:(d % 4 + 1) * 128], yT[:, k, :], start=(k == 0), stop=(k == 7))
                    tt(xT[:, d, t0:t0 + TT], xT[:, d, t0:t0 + TT], bk, ADD)
                    if d % 4 == 3:
                        release()
            dbg("yT", yT)
            PHASE1["scratch_peak"] = max(PHASE1.get("scratch_peak", 0), SC.peak)
            PHASE1["scratch_n"] = SC.n
            Pg.barrier()
            AR.reset(m)


        for sq in range(NSEQ):
            phase0(sq)
            for l in range(L):
                layer_setup(l)
                if EN("mix"):
                    phase_mix(l)
                if EN("attn"):
                    phase_attn(l)
                if EN("ffn"):
                    phase_ffn(l)
            phase_final(sq)
        Pg.wait_all_on("sp")
        build.info = dict(n_ops=Pg.n_ops, arena_peak=AR.peak, arena_n=AR.n, **PHASE1)

        with nc.Block() as block:
            @block.tensor
            def _(e):
                Pg.replay("pe", e)

            @block.scalar
            def _(e):
                Pg.replay("act", e)

            @block.vector
            def _(e):
                Pg.replay("dve", e)

            @block.gpsimd
            def _(e):
                Pg.replay("pool", e)

            @block.sync
            def _(e):
                Pg.replay("sp", e)
    return nc


def make_in_maps(inputs, L, S, nseq_per_core, n_cores):
    f = lambda a: np.ascontiguousarray(np.asarray(a, dtype=np.float32))
    shared = {
        "cb": make_consts(f(inputs["norm_final"])),
        "rope": make_rope(S),
        "pv": make_pvec(inputs, L),
        "smallw": make_smallw(inputs, L),
        "w_in": f(inputs["w_in"]),
        "w_out": f(inputs["w_out"]),
        "mem_wq": f(inputs["mem_wq"]),
        "mem_wk": f(inputs["mem_wk"]),
        "mem_wv": f(inputs["mem_wv"]),
        "mem_wo": f(inputs["mem_wo"]),
        "ffn_w_in": f(inputs["ffn_w_in"]),
        "ffn_w_out": f(inputs["ffn_w_out"]),
    }
    x = f(inputs["x"])
    mem = f(inputs["mem"])
    maps = []
    for c in range(n_cores):
        b0 = c * nseq_per_core
        m = dict(shared)
        m["x"] = np.ascontiguousarray(x[b0:b0 + nseq_per_core].reshape(nseq_per_core * S, D))
        m["mem"] = np.ascontiguousarray(mem[b0:b0 + nseq_per_core].reshape(nseq_per_core * MEMT, D))
        maps.append(m)
    return maps


def kernel(**inputs):
    x = np.asarray(inputs["x"])
    B, S, _ = x.shape
    L = np.asarray(inputs["w_in"]).shape[0]
    n_cores = N_CORES if B % N_CORES == 0 else 1
    nseq = B // n_cores
    nc = build(L, S, nseq)
    maps = make_in_maps(inputs, L, S, nseq, n_cores)
    res = run_bass_kernel_spmd(nc, maps, core_ids=list(range(n_cores)))
    outs = [np.asarray(r["out"]).reshape(nseq, S, D) for r in res.results]
    return np.concatenate(outs, axis=0).astype(np.float32)
```

```python
import math
import numpy as np
import concourse.bass as bass
import concourse.mybir as mybir
from concourse.bass_utils import run_bass_kernel_spmd

F32 = mybir.dt.float32
BF16 = mybir.dt.bfloat16
AF = mybir.ActivationFunctionType
ALU = mybir.AluOpType

D = 1024
KC = 8
TT = 512
IN_COLS = 3588
FFH = 2816
NJ = FFH // 128
MEMT = 256
N_CORES = 8
SYNC_MODE = "old"


class Buf:
    __slots__ = ("w", "r", "gen")

    def __init__(self):
        self.w = None
        self.r = {}
        self.gen = 0


class T:
    __slots__ = ("ap", "bufs", "gens")

    def __init__(self, ap, bufs):
        self.ap = ap
        self.bufs = bufs
        self.gens = [b.gen for b in bufs]

    def __getitem__(self, k):
        t = T.__new__(T)
        t.ap = self.ap[k]
        t.bufs = self.bufs
        t.gens = self.gens
        return t

    def v(self, fn):
        t = T.__new__(T)
        t.ap = fn(self.ap)
        t.bufs = self.bufs
        t.gens = self.gens
        return t

    def check(self):
        for b, g in zip(self.bufs, self.gens):
            assert b.gen == g, "stale PSUM tile used"


class Prog:
    def __init__(self, nc, sems):
        self.nc = nc
        self.E = {}
        for name in ("pe", "act", "dve", "pool", "sp"):
            self.E[name] = dict(ops=[], sem=sems[name], cnt=0, seen={})
        self.dq = {"sp": dict(slots=[[s, 0] for s in sems["dsp"]], i=0),
                   "pool": dict(slots=[[s, 0] for s in sems["dpool"]], i=0)}
        self.n_ops = 0

    def _waits(self, E, eng, reads, writes):
        waits = {}
        own = E["sem"]

        def need(tok, kind):
            if tok is None:
                return
            sem, val = tok
            if sem is own:
                if eng == "pe" or (SYNC_MODE == "old" and kind != "raw"):
                    return
            if E["seen"].get(sem, 0) >= val:
                return
            if waits.get(sem, 0) < val:
                waits[sem] = val

        for t in reads:
            t.check()
            for b in t.bufs:
                need(b.w, "raw")
        for t in writes:
            t.check()
            for b in t.bufs:
                need(b.w, "waw")
                for s, v in b.r.items():
                    need((s, v), "war")
        for s, v in waits.items():
            E["seen"][s] = v
        return list(waits.items())

    def op(self, eng, fn, reads=(), writes=()):
        E = self.E[eng]
        waits = self._waits(E, eng, reads, writes)
        E["cnt"] += 1
        c = E["cnt"]
        sem = E["sem"]
        E["ops"].append((waits, fn, sem, 1))
        for t in reads:
            for b in t.bufs:
                if b.r.get(sem, 0) < c:
                    b.r[sem] = c
        for t in writes:
            for b in t.bufs:
                b.w = (sem, c)
                b.r = {}
        self.n_ops += 1

    def dma(self, q, out, in_, reads=(), writes=()):
        E = self.E[q]
        dq = self.dq[q]
        slot = dq["slots"][dq["i"] % len(dq["slots"])]
        dq["i"] += 1
        waits = dict(self._waits(E, q, reads, writes))
        sem, val = slot
        if val > 0 and E["seen"].get(sem, 0) < val:
            waits[sem] = max(waits.get(sem, 0), val)
            E["seen"][sem] = val
        slot[1] = val + 16
        nv = slot[1]
        E["ops"].append((list(waits.items()), lambda e: e.dma_start(out=out, in_=in_), sem, 16))
        for t in reads:
            for b in t.bufs:
                b.r[sem] = nv
        for t in writes:
            for b in t.bufs:
                b.w = (sem, nv)
                b.r = {}
        self.n_ops += 1
        return (sem, nv)

    def barrier(self):
        allt = {}
        for n, E in self.E.items():
            if E["cnt"] > 0:
                allt[E["sem"]] = E["cnt"]
        for q in self.dq.values():
            for s, v in q["slots"]:
                if v > 0:
                    allt[s] = v
        for n, E in self.E.items():
            w = []
            for s, v in allt.items():
                if s is E["sem"]:
                    continue
                if E["seen"].get(s, 0) < v:
                    w.append((s, v))
                    E["seen"][s] = v
            if w:
                E["ops"].append((w, None, None, 0))

    def wait_all_on(self, eng):
        E = self.E[eng]
        w = []
        for n, E2 in self.E.items():
            if E2 is not E and E2["cnt"] > 0:
                w.append((E2["sem"], E2["cnt"]))
        for q in self.dq.values():
            for s, v in q["slots"]:
                if v > 0:
                    w.append((s, v))
        E["ops"].append((w, None, None, 0))

    def replay(self, name, e):
        for waits, fn, sem, inc in self.E[name]["ops"]:
            for s, v in waits:
                e.wait_ge(s, v)
            if fn is not None:
                fn(e).then_inc(sem, inc)


class Arena:
    def __init__(self, handle, nwords):
        self.h = handle
        self.n = nwords
        self.top = 0
        self.peak = 0

    def tile(self, free_shape, dtype=F32, parts=128):
        n = 1
        for s in free_shape:
            n *= s
        words = n if dtype == F32 else (n + 1) // 2
        words = (words + 15) // 16 * 16
        off = self.top
        self.top += words
        self.peak = max(self.peak, self.top)
        assert self.top <= self.n, f"SBUF arena overflow {self.top} > {self.n}"
        ap = self.h[:, off:off + (n if dtype == F32 else (n + 1) // 2)]
        if dtype != F32:
            ap = ap.bitcast(dtype)
            if ap.shape[1] != n:
                ap = ap[:, 0:n]
        if len(free_shape) == 2:
            ap = ap.rearrange("p (a b) -> p a b", a=free_shape[0])
        elif len(free_shape) == 3:
            ap = ap.rearrange("p (a b c) -> p a b c", a=free_shape[0], b=free_shape[1])
        elif len(free_shape) == 4:
            ap = ap.rearrange("p (a b c d) -> p a b c d", a=free_shape[0], b=free_shape[1], c=free_shape[2])
        if parts != 128:
            ap = ap[0:parts]
        return T(ap, [Buf()])

    def mark(self):
        return self.top

    def reset(self, m):
        self.top = m


class Psum:
    def __init__(self, banks):
        self.banks = banks
        self.b = [Buf() for _ in range(8)]
        self.rot = list(range(8))
        self.ptr = 0

    def set_rot(self, banks):
        self.rot = list(banks)
        self.ptr = 0

    def _mk(self, b, nq, dtype):
        self.b[b].gen += 1
        ap = self.banks[b][:, 0:nq * 128]
        if dtype != F32:
            ap = ap.bitcast(dtype)
        return T(ap, [self.b[b]])

    def get(self, nq=4, dtype=F32):
        b = self.rot[self.ptr % len(self.rot)]
        self.ptr += 1
        return self._mk(b, nq, dtype)

    def fixed(self, b, dtype=F32):
        return self._mk(b, 4, dtype)


PV = {}


def _pv_layout():
    if PV:
        return PV["_n"]
    c = 0
    for name, n in [("g_mix", 8), ("lru_cw", 8), ("lru_cb", 2), ("lru_br", 2), ("lru_bi", 2), ("lru_lam", 2),
                    ("ret_gw", 2), ("ret_gb", 2), ("rw_mu", 8), ("rw_w0", 2), ("rw_a0", 2), ("rw_kk", 2),
                    ("rw_ka", 2), ("rw_rk", 2), ("rw_gw", 2), ("rw_gb", 2), ("sd_cw", 24), ("sd_cb", 6),
                    ("sd_d", 2), ("sd_nw", 2), ("g_memq", 8), ("g_memkv", 8), ("g_ffn", 8),
                    ("sd_dtb", 4), ("sd_alog", 4)]:
        PV[name] = (c, n)
        c += n
    PV["_n"] = c
    return c


CC = {}


def _cc_layout():
    if CC:
        return CC["_n"]
    c = 0
    for name, n in [("ident", 128), ("tri", 128), ("mstrT", 128), ("mlow", 128), ("maskM", 512), ("mbias", 128),
                    ("bd", 128), ("swap", 128), ("reset", 512), ("g128", 2), ("eps6", 1), ("eps5", 1),
                    ("eps64", 1), ("eps12", 1), ("one", 1), ("g_final", 8)]:
        CC[name] = (c, n)
        c += n
    CC["_n"] = c
    return c


def _chan(v, n):
    return np.ascontiguousarray(np.asarray(v, np.float32).reshape(n, 128).T)


def make_consts(norm_final):
    n = _cc_layout()
    cb = np.zeros((128, n), np.float32)

    def put(name, arr):
        c0, k = CC[name]
        cb[:, c0:c0 + k] = arr

    i = np.arange(128)
    put("ident", np.eye(128, dtype=np.float32))
    tri = (i[:, None] <= i[None, :]).astype(np.float32)
    mstr = (i[:, None] < i[None, :]).astype(np.float32)
    put("tri", tri)
    put("mstrT", mstr)
    put("mlow", mstr.T)
    put("maskM", np.concatenate([mstr, tri, mstr, tri], axis=1))
    put("mbias", np.where(i[:, None] <= i[None, :], 0.0, -30000.0).astype(np.float32))
    bd = ((i[:, None] // 64) == (i[None, :] // 64)).astype(np.float32)
    put("bd", bd)
    partner = np.where(i % 64 < 32, i + 32, i - 32)
    sw = np.zeros((128, 128), np.float32)
    sw[partner, i] = 1.0
    put("swap", sw)
    rs = np.ones((128, 512), np.float32)
    rs[:, ::128] = 0.0
    put("reset", rs)
    gam = 1.0 - np.exp2(-5.0 - np.arange(4, dtype=np.float64))
    g128 = np.zeros((128, 2), np.float32)
    for c in range(2):
        for p in range(128):
            g128[p, c] = gam[2 * c + p // 64] ** 128
    put("g128", g128)
    put("eps6", 1e-6)
    put("eps5", 1e-5)
    put("eps64", 64e-5)
    put("eps12", 1e-12)
    put("one", 1.0)
    put("g_final", _chan(norm_final, 8))
    return cb


def make_rope(S):
    nt = S // TT
    pos = np.arange(S, dtype=np.float32)
    inv_freq = (10000.0 ** (-np.arange(32, dtype=np.float32) / 32.0)).astype(np.float32)
    ang = (pos[:, None] * inv_freq[None, :]).astype(np.float32)
    cos = np.cos(ang).astype(np.float64)
    sin = np.sin(ang).astype(np.float64)
    gam = 1.0 - np.exp2(-5.0 - np.arange(4, dtype=np.float64))
    tl = (np.arange(S) % 128) + 1
    out = np.zeros((nt, 128, 8, TT), np.float32)
    p = np.arange(128)
    n = p % 64
    j = n % 32
    sgn = np.where(n < 32, -1.0, 1.0)
    for c in range(2):
        h = 2 * c + p // 64
        gq = gam[h][:, None] ** tl[None, :]
        gk = gam[h][:, None] ** (-tl[None, :]) / 8.0
        cq = cos[:, j].T * gq
        sq = sin[:, j].T * sgn[:, None] * gq
        ck = cos[:, j].T * gk
        sk = sin[:, j].T * sgn[:, None] * gk
        for t in range(nt):
            sl = slice(t * TT, (t + 1) * TT)
            out[t, :, 0 + 2 * c, :] = cq[:, sl]
            out[t, :, 1 + 2 * c, :] = sq[:, sl]
            out[t, :, 4 + 2 * c, :] = ck[:, sl]
            out[t, :, 5 + 2 * c, :] = sk[:, sl]
    return out


def make_pvec(inp, L):
    n = _pv_layout()
    pv = np.zeros((L, 128, n), np.float32)

    def put(l, name, arr):
        c0, k = PV[name]
        pv[l, :, c0:c0 + k] = arr

    for l in range(L):
        put(l, "g_mix", _chan(inp["norm_mix"][l], 8))
        put(l, "lru_cw", np.concatenate([_chan(inp["lru_conv_w"][l][k], 2) for k in range(4)], axis=1))
        put(l, "lru_cb", _chan(inp["lru_conv_b"][l], 2))
        put(l, "lru_br", _chan(inp["lru_b_r"][l].reshape(-1), 2))
        put(l, "lru_bi", _chan(inp["lru_b_i"][l].reshape(-1), 2))
        put(l, "lru_lam", _chan(inp["lru_lambda"][l], 2))
        put(l, "ret_gw", _chan(inp["ret_gn_w"][l], 2))
        put(l, "ret_gb", _chan(inp["ret_gn_b"][l], 2))
        put(l, "rw_mu", _chan(inp["rwkv_mu"][l], 8))
        put(l, "rw_w0", _chan(inp["rwkv_w0"][l], 2))
        put(l, "rw_a0", _chan(inp["rwkv_a0"][l], 2))
        put(l, "rw_kk", _chan(inp["rwkv_k_k"][l], 2))
        put(l, "rw_ka", _chan(inp["rwkv_k_a"][l], 2))
        put(l, "rw_rk", _chan(inp["rwkv_r_k"][l], 2))
        put(l, "rw_gw", _chan(inp["rwkv_gn_w"][l], 2))
        put(l, "rw_gb", _chan(inp["rwkv_gn_b"][l], 2))
        put(l, "sd_cw", np.concatenate([_chan(inp["ssd_conv_w"][l][k], 6) for k in range(4)], axis=1))
        put(l, "sd_cb", _chan(inp["ssd_conv_b"][l], 6))
        put(l, "sd_d", _chan(np.repeat(np.asarray(inp["ssd_d"][l], np.float32), 64), 2))
        put(l, "sd_nw", _chan(inp["ssd_norm_w"][l], 2))
        put(l, "g_memq", _chan(inp["norm_mem_q"][l], 8))
        put(l, "g_memkv", _chan(inp["norm_mem_kv"][l], 8))
        put(l, "g_ffn", _chan(inp["norm_ffn"][l], 8))
        put(l, "sd_dtb", np.broadcast_to(np.asarray(inp["ssd_dt_bias"][l], np.float32)[None, :], (128, 4)))
        put(l, "sd_alog", np.broadcast_to(np.asarray(inp["ssd_a_log"][l], np.float32)[None, :], (128, 4)))
    return pv


def make_smallw(inp, L):
    sw = np.zeros((L, 128, 1280), np.float32)
    for l in range(L):
        for c in range(2):
            for hh in range(2):
                blk = slice(hh * 64, hh * 64 + 64)
                sw[l, blk, c * 128 + hh * 64: c * 128 + hh * 64 + 64] = inp["lru_w_r"][l][2 * c + hh]
                sw[l, blk, 256 + c * 128 + hh * 64: 256 + c * 128 + hh * 64 + 64] = inp["lru_w_i"][l][2 * c + hh]
        sw[l, 0:64, 512:768] = inp["rwkv_w2"][l]
        sw[l, 64:128, 768:1024] = inp["rwkv_a2"][l]
        sw[l, :, 1024:1280] = inp["rwkv_g2"][l]
    return sw


def build(L, S, NSEQ, flags=None):
    from contextlib import ExitStack
    flags = flags or {}
    EN = lambda k: flags.get(k, True)
    NT = S // TT
    NCHS = S // 128
    GT = 2 if NT % 2 == 0 else 1
    nc = bass.Bass("TRN2", target_bir_lowering=False)
    NPV = _pv_layout()
    NCC = _cc_layout()

    def dram(name, shape, kind="ExternalInput"):
        return nc.dram_tensor(name, shape, F32, kind=kind).ap()

    x_d = dram("x", [NSEQ * S, D])
    mem_d = dram("mem", [NSEQ * MEMT, D])
    cb_d = dram("cb", [128, NCC])
    rope_d = dram("rope", [NT, 128, 8, TT])
    pv_d = dram("pv", [L, 128, NPV])
    sw_d = dram("smallw", [L, 128, 1280])
    win_d = dram("w_in", [L, D, IN_COLS])
    wout_d = dram("w_out", [L, D, D])
    wq_d = dram("mem_wq", [L, D, D])
    wk_d = dram("mem_wk", [L, D, D])
    wv_d = dram("mem_wv", [L, D, D])
    wo_d = dram("mem_wo", [L, D, D])
    f1_d = dram("ffn_w_in", [L, D, 2 * FFH])
    f2_d = dram("ffn_w_out", [L, FFH, D])
    out_d = dram("out", [NSEQ * S, D], kind="ExternalOutput")

    NW = 52224
    with ExitStack() as es:
        arena_h = es.enter_context(nc.sbuf_tensor("arena", [128, NW], F32))
        banks = [es.enter_context(nc.psum_tensor(f"bank{i}", [128, 512], F32)) for i in range(8)]
        sems = {}
        for n in ("pe", "act", "dve", "pool", "sp"):
            sems[n] = es.enter_context(nc.semaphore("s_" + n))
        sems["dsp"] = [es.enter_context(nc.semaphore(f"dsp{i}")) for i in range(12)]
        sems["dpool"] = [es.enter_context(nc.semaphore(f"dpl{i}")) for i in range(12)]
        Pg = Prog(nc, sems)
        AR = Arena(arena_h, NW)
        PS = Psum([b[:, :] for b in banks])

        def _a(x):
            return x.ap if isinstance(x, T) else x

        def _rd(*xs):
            return [x for x in xs if isinstance(x, T)]

        def mm(out, lhsT, rhs, start=True, stop=True):
            Pg.op("pe", lambda e: e.matmul(out.ap, lhsT.ap, rhs.ap, start=start, stop=stop), [lhsT, rhs], [out])

        def tpose(out, in_, ident):
            Pg.op("pe", lambda e: e.transpose(out.ap, in_.ap, ident.ap), [in_, ident], [out])

        def act(out, in_, func, scale=1.0, bias=0.0):
            Pg.op("act", lambda e: e.activation(out=out.ap, in_=in_.ap, func=func, bias=_a(bias), scale=_a(scale)),
                  _rd(in_, scale, bias), [out])

        def tt(out, a, b, op, eng="dve"):
            Pg.op(eng, lambda e: e.tensor_tensor(out=out.ap, in0=a.ap, in1=b.ap, op=op), [a, b], [out])

        def ts(out, a, s1, op0, s2=None, op1=None, eng="dve"):
            kw = dict(out=out.ap, in0=a.ap, scalar1=_a(s1), scalar2=_a(s2), op0=op0)
            if op1 is not None:
                kw["op1"] = op1
            Pg.op(eng, lambda e: e.tensor_scalar(**kw), _rd(a, s1, s2), [out])

        def stt(out, a, s, b, op0, op1):
            Pg.op("dve", lambda e: e.scalar_tensor_tensor(out=out.ap, in0=a.ap, scalar=_a(s), in1=b.ap,
                                                          op0=op0, op1=op1), _rd(a, s, b), [out])

        def cp(out, a, eng="dve"):
            if eng == "act":
                Pg.op("act", lambda e: e.copy(out=out.ap, in_=a.ap), [a], [out])
            else:
                Pg.op(eng, lambda e: e.tensor_copy(out=out.ap, in_=a.ap), [a], [out])

        def scan(out, d0, d1, init):
            Pg.op("dve", lambda e: e.tensor_tensor_scan(out=out.ap, data0=d0.ap, data1=d1.ap, initial=_a(init),
                                                        op0=ALU.mult, op1=ALU.add), _rd(d0, d1, init), [out])

        def recip(out, a):
            Pg.op("dve", lambda e: e.reciprocal(out=out.ap, in_=a.ap), [a], [out])

        def mset(t, val, eng="dve"):
            Pg.op(eng, lambda e: e.memset(t.ap, val), [], [t])

        def wload(dst, src_ap):
            Pg.dma("pool", dst.ap, src_ap, writes=[dst])

        def ld(dst, src_ap):
            Pg.dma("sp", dst.ap, src_ap, writes=[dst])

        MUL, ADD, SUB = ALU.mult, ALU.add, ALU.subtract
        DBG = {}

        def dbg(name, t):
            if not flags.get("dbg") or name in DBG:
                return
            shp = list(t.ap.shape)
            d = nc.dram_tensor("dbg_" + name, shp, t.ap.dtype, kind="ExternalOutput").ap()
            DBG[name] = shp
            Pg.dma("sp", d, t.ap, reads=[t])

        xT = AR.tile([8, S])
        cb = AR.tile([NCC])
        pv = AR.tile([NPV])
        DVL = {}
        ndv = 0
        for name, n in [("hbr", 2), ("hbi", 2), ("cA", 2), ("hcA", 2), ("sdcwh", 24), ("sdcbh", 6), ("omm", 8),
                        ("hw0", 2), ("ha0", 2), ("omka", 2), ("aneg", 4), ("tmp", 8)]:
            DVL[name] = (ndv, n)
            ndv += n
        dv = AR.tile([ndv])
        smallw = AR.tile([1280], BF16)
        ident_bf = AR.tile([128], BF16)
        ones_bf = AR.tile([128], BF16)
        onesD_bf = AR.tile([128], BF16)
        ones128_bf = AR.tile([128], BF16)
        bd1_bf = AR.tile([128], BF16)
        bd64_bf = AR.tile([128], BF16)
        swap_bf = AR.tile([128], BF16)
        ones_f = AR.tile([128])
        memnT = AR.tile([8, MEMT], BF16)
        KT = AR.tile([8, MEMT], BF16)
        Vm = AR.tile([2, D], BF16)
        Vpad = AR.tile([4, 4, 128], BF16)
        Upad = [[AR.tile([128], BF16) for _ in range(2)] for _ in range(2)]
        SpadD = [[AR.tile([128], BF16) for _ in range(2)] for _ in range(2)]
        SD_f = [AR.tile([128]) for _ in range(2)]
        SB_f = [AR.tile([128]) for _ in range(2)]
        SB_bf = [AR.tile([128], BF16) for _ in range(2)]
        SC_f = [AR.tile([128]) for _ in range(2)]
        SC_bf = [AR.tile([128], BF16) for _ in range(2)]
        histA = [AR.tile([3]) for _ in range(2)]
        histD = [AR.tile([3]) for _ in range(6)]
        histC = [AR.tile([1]) for _ in range(8)]
        hprev = [AR.tile([1]) for _ in range(2)]
        stage = [AR.tile([3 + TT]) for _ in range(2)]
        PH0 = AR.mark()

        def C(name, a=None, b=None):
            c0, n = CC[name]
            if a is None:
                return cb[:, c0:c0 + n]
            return cb[:, c0 + a:c0 + b]

        def PVc(name, a=0, b=None):
            c0, n = PV[name]
            b = n if b is None else b
            return pv[:, c0 + a:c0 + b]

        def DVc(name, a=0, b=None):
            c0, n = DVL[name]
            b = n if b is None else b
            return dv[:, c0 + a:c0 + b]

        ident_f = C("ident")
        eps6, eps5, eps64, eps12, one_c = C("eps6"), C("eps5"), C("eps64"), C("eps12"), C("one")

        ld(cb, cb_d[:, :])
        cp(ident_bf, C("ident"))
        mset(ones_bf, 1.0)
        mset(onesD_bf, 1.0 / 1024.0)
        mset(ones128_bf, 1.0 / 128.0)
        mset(ones_f, 1.0)
        cp(bd1_bf, C("bd"))
        ts(bd64_bf, C("bd"), 1.0 / 64.0, MUL)
        cp(swap_bf, C("swap"))
        mset(Vpad, 0.0)
        for a_ in Upad + SpadD:
            for b_ in a_:
                mset(b_, 0.0)

        def rsqrt(out, in_, scale, eps_t):
            act(out, in_, AF.Ln, scale=scale, bias=eps_t)
            act(out, out, AF.Exp, scale=-0.5)

        def rmsnorm_tile(xsl, gcols, hT, sqt, rstd, n=TT):
            bank = PS.get(4)
            bk = bank[:, 0:n]
            for k in range(8):
                sq = sqt[k % 2]
                act(sq, xsl[:, k, :], AF.Square)
                mm(bk, onesD_bf, sq, start=(k == 0), stop=(k == 7))
            rsqrt(rstd, bk, 1.0, eps6)
            for k in range(8):
                if gcols is None:
                    tt(hT[:, k, :], xsl[:, k, :], rstd, MUL)
                else:
                    stt(hT[:, k, :], xsl[:, k, :], gcols[:, k:k + 1], rstd, MUL, MUL)

        def wsrc(w_d, l, r0, kc, c0, n):
            return w_d[l, r0:r0 + kc * 128, c0:c0 + n].rearrange("(k p) c -> p k c", p=128)

        def v4(t):
            return t.v(lambda a: a.rearrange("p (a b) -> p a b", a=4))

        def phase0(sq):
            m = AR.mark()
            PS.set_rot(range(8))
            xst = [AR.tile([D]) for _ in range(2)]
            memT = AR.tile([8, MEMT])
            sqt = [AR.tile([MEMT], BF16) for _ in range(2)]
            rstd = AR.tile([MEMT])
            for c in range(NCHS):
                st = xst[c % 2]
                ld(st, x_d[sq * S + c * 128: sq * S + (c + 1) * 128, :])
                for half in range(2):
                    bk = PS.get(4)
                    for q in range(4):
                        k = half * 4 + q
                        tpose(bk[:, q * 128:(q + 1) * 128], st[:, k * 128:(k + 1) * 128], ident_f)
                    cp(xT[:, half * 4:(half + 1) * 4, c * 128:(c + 1) * 128], v4(bk), eng=("act" if half else "dve"))
            for mc in range(2):
                st = xst[mc % 2]
                ld(st, mem_d[sq * MEMT + mc * 128: sq * MEMT + (mc + 1) * 128, :])
                for half in range(2):
                    bk = PS.get(4)
                    for q in range(4):
                        k = half * 4 + q
                        tpose(bk[:, q * 128:(q + 1) * 128], st[:, k * 128:(k + 1) * 128], ident_f)
                    cp(memT[:, half * 4:(half + 1) * 4, mc * 128:(mc + 1) * 128], v4(bk), eng=("act" if half else "dve"))
            rmsnorm_tile(memT, None, memnT, sqt, rstd, n=MEMT)
            Pg.barrier()
            AR.reset(m)

        def layer_setup(l):
            ld(pv, pv_d[l])
            wload(smallw, sw_d[l])
            ts(DVc("hbr"), PVc("lru_br"), 0.5, MUL)
            ts(DVc("hbi"), PVc("lru_bi"), 0.5, MUL)
            act(DVc("tmp", 0, 2), PVc("lru_lam"), AF.Exp, scale=-1.0)
            act(DVc("tmp", 0, 2), DVc("tmp", 0, 2), AF.Ln, scale=1.0, bias=one_c)
            ts(DVc("cA"), DVc("tmp", 0, 2), -8.0, MUL)
            ts(DVc("hcA"), DVc("tmp", 0, 2), -4.0, MUL)
            ts(DVc("sdcwh"), PVc("sd_cw"), 0.5, MUL)
            ts(DVc("sdcbh"), PVc("sd_cb"), 0.5, MUL)
            ts(DVc("omm"), PVc("rw_mu"), -1.0, MUL, 1.0, ADD)
            ts(DVc("hw0"), PVc("rw_w0"), 0.5, MUL)
            ts(DVc("ha0"), PVc("rw_a0"), 0.5, MUL)
            ts(DVc("omka"), PVc("rw_ka"), -1.0, MUL, 1.0, ADD)
            act(DVc("tmp", 4, 8), PVc("sd_alog"), AF.Exp)
            ts(DVc("aneg"), DVc("tmp", 4, 8), -1.0, MUL)

        def phase_ffn(l):
            m = AR.mark()
            PS.set_rot(range(8))
            GTOK = GT * TT
            hT2 = AR.tile([8, GTOK], BF16)
            actT = AR.tile([NJ, GTOK], BF16)
            sqt = [AR.tile([TT], BF16) for _ in range(2)]
            rstd = AR.tile([TT])
            tg = [AR.tile([TT]) for _ in range(2)]
            s1 = [AR.tile([TT]) for _ in range(2)]
            wgu = [AR.tile([2, 8, 256], BF16) for _ in range(2)]
            w2s = [AR.tile([NJ, 128], BF16) for _ in range(2)]
            for g in range(NT // GT):
                for ti in range(GT):
                    t0 = (g * GT + ti) * TT
                    rmsnorm_tile(xT[:, :, t0:t0 + TT], PVc("g_ffn"), hT2[:, :, ti * TT:(ti + 1) * TT], sqt, rstd)
                for jb in range(NJ // 2):
                    w = wgu[jb % 2]
                    wload(w[:, 0], wsrc(f1_d, l, 0, 8, jb * 256, 256))
                    wload(w[:, 1], wsrc(f1_d, l, 0, 8, FFH + jb * 256, 256))
                    for jj in range(2):
                        j = jb * 2 + jj
                        for ti in range(GT):
                            pg_ = PS.get(4)
                            pu_ = PS.get(4)
                            for k in range(8):
                                mm(pg_, w[:, 0, k, jj * 128:(jj + 1) * 128], hT2[:, k, ti * TT:(ti + 1) * TT],
                                   start=(k == 0), stop=(k == 7))
                            for k in range(8):
                                mm(pu_, w[:, 1, k, jj * 128:(jj + 1) * 128], hT2[:, k, ti * TT:(ti + 1) * TT],
                                   start=(k == 0), stop=(k == 7))
                            tgi, s1i = tg[(j * GT + ti) % 2], s1[(j * GT + ti) % 2]
                            act(tgi, pg_, AF.Tanh, scale=0.5)
                            stt(s1i, tgi, 1.0, pg_, ADD, MUL)
                            stt(actT[:, j, ti * TT:(ti + 1) * TT], s1i, 0.5, pu_, MUL, MUL)
                for d in range(8):
                    w = w2s[d % 2]
                    wload(w, f2_d[l, :, d * 128:(d + 1) * 128].rearrange("(j p) c -> p j c", p=128))
                    for ti in range(GT):
                        t0 = (g * GT + ti) * TT
                        bk = PS.get(4)
                        for j in range(NJ):
                            mm(bk, w[:, j, :], actT[:, j, ti * TT:(ti + 1) * TT], start=(j == 0), stop=(j == NJ - 1))
                        tt(xT[:, d, t0:t0 + TT], xT[:, d, t0:t0 + TT], bk, ADD)
            Pg.barrier()
            AR.reset(m)

        def phase_attn(l):
            m = AR.mark()
            PS.set_rot(range(8))
            wq = AR.tile([8, D], BF16)
            wo = AR.tile([8, D], BF16)
            wkv = [AR.tile([8, 512], BF16) for _ in range(2)]
            hT = AR.tile([8, TT], BF16)
            qT = AR.tile([8, TT], BF16)
            oT = AR.tile([8, TT], BF16)
            et = [AR.tile([TT], BF16) for _ in range(4)]
            rden = AR.tile([TT])
            sqt = [AR.tile([TT], BF16) for _ in range(2)]
            rstd = AR.tile([TT])
            hmT = AR.tile([8, MEMT], BF16)
            for k in range(8):
                ts(hmT[:, k, :], memnT[:, k, :], PVc("g_memkv", k, k + 1), MUL)
            for half in range(2):
                w = wkv[half]
                wload(w, wsrc(wk_d, l, 0, 8, half * 512, 512))
                for cc in range(4):
                    bk = PS.get(2)
                    for k in range(8):
                        mm(bk, w[:, k, cc * 128:(cc + 1) * 128], hmT[:, k, :], start=(k == 0), stop=(k == 7))
                    cp(KT[:, half * 4 + cc, :], bk, eng="act")
            for half in range(2):
                w = wkv[half]
                wload(w, wsrc(wv_d, l, 0, 8, half * 512, 512))
                for mc in range(2):
                    bk = PS.get(4)
                    for k in range(8):
                        mm(bk, hmT[:, k, mc * 128:(mc + 1) * 128], w[:, k, :], start=(k == 0), stop=(k == 7))
                    cp(Vm[:, mc, half * 512:(half + 1) * 512], bk, eng="act")
            wload(wq, wsrc(wq_d, l, 0, 8, 0, D))
            wload(wo, wsrc(wo_d, l, 0, 8, 0, D))
            for ti in range(NT):
                t0 = ti * TT
                rmsnorm_tile(xT[:, :, t0:t0 + TT], PVc("g_memq"), hT, sqt, rstd)
                for c in range(8):
                    bk = PS.get(4)
                    for k in range(8):
                        mm(bk, wq[:, k, c * 128:(c + 1) * 128], hT[:, k, :], start=(k == 0), stop=(k == 7))
                    cp(qT[:, c, :], bk, eng="act")
                for h in range(4):
                    es_ = []
                    for mc in range(2):
                        bk = PS.get(4)
                        for dc in range(2):
                            mm(bk, KT[:, 2 * h + dc, mc * 128:(mc + 1) * 128], qT[:, 2 * h + dc, :],
                               start=(dc == 0), stop=(dc == 1))
                        e_ = et[(h % 2) * 2 + mc]
                        act(e_, bk, AF.Exp, scale=1.0 / 16.0)
                        es_.append(e_)
                    den = PS.get(4)
                    for mc in range(2):
                        mm(den, ones_bf, es_[mc], start=(mc == 0), stop=(mc == 1))
                    recip(rden, den)
                    for dc in range(2):
                        bo = PS.get(4)
                        for mc in range(2):
                            mm(bo, Vm[:, mc, (2 * h + dc) * 128:(2 * h + dc + 1) * 128], es_[mc],
                               start=(mc == 0), stop=(mc == 1))
                        tt(oT[:, 2 * h + dc, :], bo, rden, MUL)
                for d in range(8):
                    bk = PS.get(4)
                    for k in range(8):
                        mm(bk, wo[:, k, d * 128:(d + 1) * 128], oT[:, k, :], start=(k == 0), stop=(k == 7))
                    tt(xT[:, d, t0:t0 + TT], xT[:, d, t0:t0 + TT], bk, ADD)
            Pg.barrier()
            AR.reset(m)

        def phase_final(sq):
            m = AR.mark()
            PS.set_rot(range(8))
            hF = AR.tile([8, TT])
            sqt = [AR.tile([TT], BF16) for _ in range(2)]
            rstd = AR.tile([TT])
            ost = [AR.tile([D]) for _ in range(2)]
            for ti in range(NT):
                t0 = ti * TT
                rmsnorm_tile(xT[:, :, t0:t0 + TT], C("g_final"), hF, sqt, rstd)
                for tc in range(4):
                    o = ost[tc % 2]
                    for half in range(2):
                        bk = PS.get(4)
                        for q in range(4):
                            k = half * 4 + q
                            tpose(bk[:, q * 128:(q + 1) * 128], hF[:, k, tc * 128:(tc + 1) * 128], ident_f)
                        cp(o[:, half * 512:(half + 1) * 512], bk, eng=("act" if half else "dve"))
                    r0 = sq * S + t0 + tc * 128
                    Pg.dma("sp", out_d[r0:r0 + 128, :], o.ap, reads=[o])
            Pg.barrier()
            AR.reset(m)

        PHASE1 = {}

        class Scratch:
            U = 64

            def __init__(self, nunits):
                self.n = nunits
                self.base = AR.top
                AR.top += nunits * self.U
                AR.peak = max(AR.peak, AR.top)
                assert AR.top <= AR.n, f"scratch overflow {AR.top} > {AR.n}"
                self.bufs = [Buf() for _ in range(nunits)]
                self.used = [False] * nunits
                self.owner = {}
                self.peak = 0

            def _get(self, nwords):
                k = (nwords + self.U - 1) // self.U
                i = 0
                while i + k <= self.n:
                    j = i
                    while j < i + k and not self.used[j]:
                        j += 1
                    if j == i + k:
                        for q in range(i, i + k):
                            self.used[q] = True
                        self.peak = max(self.peak, sum(self.used))
                        return i, k
                    i = j + 1
                raise AssertionError(f"scratch pool exhausted (need {k} units, used {sum(self.used)}/{self.n})")

            def f32(self, n):
                i, k = self._get(n)
                ap = arena_h[:, self.base + i * self.U: self.base + i * self.U + n]
                t = T(ap, self.bufs[i:i + k])
                self.owner[id(t)] = (i, k)
                return t

            def bf(self, n):
                i, k = self._get((n + 1) // 2)
                ap = arena_h[:, self.base + i * self.U: self.base + i * self.U + (n + 1) // 2].bitcast(BF16)
                t = T(ap, self.bufs[i:i + k])
                self.owner[id(t)] = (i, k)
                return t

            def free(self, *ts_):
                for t in ts_:
                    i, k = self.owner.pop(id(t))
                    for q in range(i, i + k):
                        self.used[q] = False

        def bc_mid(t, n):
            return t.v(lambda a: a.unsqueeze(1).broadcast_to([128, n, a.shape[1]]))

        def bc_last(t, n):
            return t.v(lambda a: a.unsqueeze(2).broadcast_to([128, a.shape[1], n]))

        def r3(t, a_):
            return t.v(lambda x: x.rearrange("p (a b) -> p a b", a=a_))

        def r4(t, a_, b_):
            return t.v(lambda x: x.rearrange("p (a b c) -> p a b c", a=a_, b=b_))

        VpadV = Vpad.v(lambda a: a.rearrange("p t (c h) n -> p t c h n", c=2))

        def phase_mix(l):
            m = AR.mark()
            PS.set_rot([2, 3, 4, 5, 6, 7])
            wsl = [AR.tile([8, 512], BF16) for _ in range(3)]
            hT = AR.tile([8, TT], BF16)
            yT = AR.tile([8, TT], BF16)
            sqt = [AR.tile([TT], BF16) for _ in range(2)]
            rstd = AR.tile([TT])
            tri4 = AR.tile([TT])
            for i4 in range(4):
                cp(tri4[:, i4 * 128:(i4 + 1) * 128], C("tri"))
            SC = Scratch((AR.n - AR.top) // Scratch.U)
            for t_ in SB_f + SC_f + SD_f + SB_bf + SC_bf + histA + histD + histC + hprev:
                mset(t_, 0.0)
            for a_ in SpadD:
                for b_ in a_:
                    mset(b_, 0.0)
            if not (EN("A") and EN("B") and EN("C") and EN("D")):
                mset(yT, 0.0)

            GROUPS = [(g * 512, 512) for g in range(7)] + [(3584, 4)]
            seq = []
            for ti in range(NT):
                for g in range(8):
                    seq.append(("in", g))
                seq.append(("o", 0))
                seq.append(("o", 1))
            loaded = {}
            state = dict(nload=0)

            def issue_load():
                i = state["nload"]
                if i >= len(seq):
                    return
                kind, g = seq[i]
                slot = wsl[i % 3]
                if kind == "in":
                    c0, n = GROUPS[g]
                    wload(slot[:, :, 0:n], wsrc(win_d, l, 0, 8, c0, n))
                else:
                    wload(slot, wsrc(wout_d, l, 0, 8, g * 512, 512))
                loaded[i] = slot
                state["nload"] += 1

            issue_load()
            issue_load()
            issue_load()
            cur = dict(i=0)

            def next_slot():
                i = cur["i"]
                cur["i"] += 1
                return loaded.pop(i)

            release = issue_load

            def proj(slot, c0, rows=128):
                bk = PS.get(4)
                for k in range(8):
                    mm(bk[0:rows] if rows != 128 else bk, slot[:, k, c0:c0 + rows], hT[:, k, :],
                       start=(k == 0), stop=(k == 7))
                return bk

            def conv_stage(bk, hist, idx, wk, bias):
                st = stage[idx % 2]
                cp(st[:, 0:3], hist)
                cp(st[:, 3:3 + TT], bk, eng="act")
                cv = SC.f32(TT)
                ts(cv, st[:, 0:TT], wk(0), MUL, bias, ADD)
                for k in range(1, 4):
                    stt(cv, st[:, k:k + TT], wk(k), cv, MUL, ADD)
                cp(hist, st[:, TT:TT + 3])
                return cv

            def group_norm(ybank, eps_t, gw, gb):
                ysb = SC.bf(TT)
                cp(ysb, ybank, eng="act")
                ysq = SC.bf(TT)
                act(ysq, ybank, AF.Square)
                pm = PS.get(4)
                mm(pm, bd64_bf, ysb)
                pq = PS.get(4)
                mm(pq, bd64_bf, ysq)
                mean = SC.f32(TT)
                cp(mean, pm, eng="act")
                var = SC.f32(TT)
                stt(var, mean, -1.0, mean, MUL, MUL)
                tt(var, pq, var, ADD)
                ts(var, var, 0.0, ALU.max)
                rsqrt(var, var, 1.0, eps_t)
                yc = SC.f32(TT)
                tt(yc, ybank, mean, SUB)
                tt(yc, yc, var, MUL)
                ts(yc, yc, gw, MUL, gb, ADD)
                SC.free(ysb, ysq, mean, var)
                return yc

            tri_b4 = bc_mid(C("tri"), 4)
            mbias_b4 = bc_mid(C("mbias"), 4)

            for ti in range(NT):
                t0 = ti * TT
                xsl = xT[:, :, t0:t0 + TT]
                rmsnorm_tile(xsl, PVc("g_mix"), hT, sqt, rstd)
                yb = [PS.fixed(0), PS.fixed(1)]

                slot = next_slot()
                if not EN("A"):
                    release()
                if EN("A"):
                    for c in range(2):
                        pg_ = proj(slot, c * 128)
                        px_ = proj(slot, 256 + c * 128)
                        gt = SC.f32(TT)
                        cp(gt, pg_, eng="act")
                        cv = conv_stage(px_, histA[c], c, lambda k: PVc("lru_cw", k * 2 + c, k * 2 + c + 1),
                                        PVc("lru_cb", c, c + 1))
                        cvb = SC.bf(TT)
                        cp(cvb, cv, eng="act")
                        pr = PS.get(4)
                        mm(pr, smallw[:, c * 128:(c + 1) * 128], cvb)
                        pi_ = PS.get(4)
                        mm(pi_, smallw[:, 256 + c * 128:256 + (c + 1) * 128], cvb)
                        tr = SC.f32(TT)
                        act(tr, pr, AF.Tanh, scale=0.5, bias=DVc("hbr", c, c + 1))
                        ti_ = SC.f32(TT)
                        act(ti_, pi_, AF.Tanh, scale=0.5, bias=DVc("hbi", c, c + 1))
                        a_ = SC.f32(TT)
                        act(a_, tr, AF.Exp, scale=DVc("hcA", c, c + 1), bias=DVc("hcA", c, c + 1))
                        s_ = SC.f32(TT)
                        act(s_, tr, AF.Exp, scale=DVc("cA", c, c + 1), bias=DVc("cA", c, c + 1))
                        act(s_, s_, AF.Ln, scale=-1.0, bias=one_c)
                        act(s_, s_, AF.Exp, scale=0.5)
                        stt(ti_, ti_, 1.0, cv, ADD, MUL)
                        stt(ti_, ti_, 0.5, s_, MUL, MUL)
                        scan(tr, a_, ti_, hprev[c])
                        cp(hprev[c], tr[:, TT - 1:TT])
                        act(s_, gt, AF.Square)
                        ts(s_, s_, 0.044715, MUL, 1.0, ADD)
                        tt(s_, s_, gt, MUL)
                        act(s_, s_, AF.Tanh, scale=0.7978845608028654)
                        stt(s_, s_, 1.0, gt, ADD, MUL)
                        stt(yT[:, c, :], s_, 0.5, tr, MUL, MUL)
                        SC.free(gt, cv, cvb, tr, ti_, a_, s_)
                    release()

                slot1 = next_slot()
                slot2 = next_slot()
                if not EN("B"):
                    release()
                    release()
                if EN("B"):
                    rope = SC.f32(8 * TT)
                    rope3 = r3(rope, 8)
                    for i8 in range(8):
                        ld(rope3[:, i8, :], rope_d[ti, :, i8, :])
                    qk = [[SC.bf(TT) for _ in range(2)] for _ in range(2)]
                    for is_k in range(2 if "Q" not in flags.get("Bskip", "") else 0):
                        for c in range(2):
                            bk = proj(slot1, is_k * 256 + c * 128)
                            xb = SC.bf(TT)
                            cp(xb, bk, eng="act")
                            p2 = PS.get(4)
                            mm(p2, swap_bf, xb)
                            t1 = SC.f32(TT)
                            tt(t1, bk, rope3[:, 4 * is_k + 2 * c + 0, :], MUL)
                            t2 = SC.f32(TT)
                            tt(t2, p2, rope3[:, 4 * is_k + 2 * c + 1, :], MUL)
                            tt(qk[is_k][c], t1, t2, ADD)
                            SC.free(xb, t1, t2)
                    SC.free(rope)
                    release()
                    qT_, kT_ = qk
                    dbg("B_q0", qT_[0]); dbg("B_k0", kT_[0]); dbg("B_hT", hT)
                    ktok = SC.bf(4 * 256)
                    ktok3 = r3(ktok, 4)
                    vt = SC.bf(4 * 256)
                    vt3 = r3(vt, 4)
                    for tc in range(4):
                        tcs = slice(tc * 128, (tc + 1) * 128)
                        if "T" not in flags.get("Bskip", ""):
                            pt = PS.get(1, BF16)
                            for c in range(2):
                                tpose(pt[:, c * 128:(c + 1) * 128], kT_[c][:, tcs], ident_bf)
                            cp(ktok3[:, tc, :], pt, eng="act")
                        if "v" in flags.get("Bskip", ""):
                            continue
                        bv = PS.get(2)
                        for k in range(8):
                            mm(bv, hT[:, k, tcs], slot2[:, k, 0:256], start=(k == 0), stop=(k == 7))
                        cp(vt3[:, tc, :], bv, eng="act")
                        bv4 = r4(vt3[:, tc, :], 2, 2)
                        for hh in range(2 if "V" not in flags.get("Bskip", "") else 0):
                            ts(VpadV[:, tc, :, hh, hh * 64:(hh + 1) * 64], bv4[:, :, hh, :], 1.0, MUL)
                    sg = []
                    for c in range(2 if "G" not in flags.get("Bskip", "") else 0):
                        bk = proj(slot2, 256 + c * 128)
                        tg_ = SC.f32(TT)
                        act(tg_, bk, AF.Tanh, scale=0.5)
                        s = SC.f32(TT)
                        stt(s, tg_, 1.0, bk, ADD, MUL)
                        SC.free(tg_)
                        sg.append(s)
                    release()
                    sms = []
                    for tc in range(4):
                        tcs = slice(tc * 128, (tc + 1) * 128)
                        sm = SC.bf(TT)
                        sm4 = r4(sm, 2, 2)
                        for hh in range(2):
                            r0 = hh * 64
                            sbh = PS.get(2)
                            for c in range(2):
                                mm(sbh[:, c * 128:(c + 1) * 128], kT_[c][r0:r0 + 64, tcs], qT_[c][r0:r0 + 64, tcs])
                            tt(sm4[:, :, hh, :], r3(sbh, 2), r3(tri4[:, 0:256], 2), MUL)
                        sms.append(sm)
                    for tc in range(4):
                        tcs = slice(tc * 128, (tc + 1) * 128)
                        sm = sms[tc]
                        for c in range(2):
                            ycol = yb[c][:, tcs]
                            mm(ycol, SB_bf[c], qT_[c][:, tcs], start=True, stop=False)
                            for hh in range(2):
                                h = 2 * c + hh
                                mm(ycol, Vpad[:, tc, h, :], sm[:, h * 128:(h + 1) * 128], start=False, stop=(hh == 1))
                            pS = PS.get(1)
                            mm(pS, ktok3[:, tc, c * 128:(c + 1) * 128], vt3[:, tc, c * 128:(c + 1) * 128])
                            tmp = SC.f32(128)
                            stt(tmp, pS, C("g128", c, c + 1), C("bd"), MUL, MUL)
                            stt(SB_f[c], SB_f[c], C("g128", c, c + 1), tmp, MUL, ADD)
                            cp(SB_bf[c], SB_f[c], eng="act")
                            SC.free(tmp)
                        SC.free(sm)
                    if flags.get("dbg"):
                        dbg("B_vt", vt); dbg("B_ktok", ktok); dbg("B_sg0", sg[0]); dbg("B_Vpad", Vpad)
                    for c in range(2 if flags.get("Bstage", 3) >= 3 else 0):
                        ydb = SC.f32(TT)
                        cp(ydb, yb[c])
                        dbg("B_y%d" % c, ydb)
                        SC.free(ydb)
                        yc = group_norm(yb[c], eps5, PVc("ret_gw", c, c + 1), PVc("ret_gb", c, c + 1))
                        dbg("B_yn%d" % c, yc)
                        stt(yT[:, 2 + c, :], yc, 0.5, sg[c], MUL, MUL)
                        SC.free(yc)
                    SC.free(ktok, vt, *sg, *qT_, *kT_)
                elif False:
                    pass

                slot3 = next_slot()
                slot4 = next_slot()
                if not EN("C"):
                    release()
                    release()
                if EN("C"):
                    def shiftC(idx, bk):
                        st = stage[idx % 2]
                        cp(st[:, 2:3], histC[idx])
                        cp(st[:, 3:3 + TT], bk, eng="act")
                        o = SC.f32(TT)
                        ts(o, st[:, 3:3 + TT], DVc("omm", idx, idx + 1), MUL)
                        stt(o, st[:, 2:2 + TT], PVc("rw_mu", idx, idx + 1), o, MUL, ADD)
                        cp(histC[idx], st[:, 2 + TT:3 + TT])
                        return o

                    rf = [shiftC(c, proj(slot3, c * 128)) for c in range(2)]
                    kf = [shiftC(2 + c, proj(slot3, 256 + c * 128)) for c in range(2)]
                    release()
                    vf = [shiftC(4 + c, proj(slot4, c * 128)) for c in range(2)]
                    lo6 = shiftC(6, proj(slot4, 256))
                    lo7 = shiftC(7, proj(slot4, 384))
                    release()
                    tw = SC.bf(TT)
                    act(tw[0:64], lo6[0:64], AF.Tanh)
                    al = SC.bf(TT)
                    cp(al[64:128], lo6[64:128], eng="act")
                    sgl = SC.bf(TT)
                    act(lo7, lo7, AF.Tanh, scale=0.5)
                    ts(sgl, lo7, 0.5, MUL, 0.5, ADD)
                    SC.free(lo6, lo7)
                    gT_, bon, Wt, ARt, Ktl, Btl, vb, ARb = [], [], [], [], [], [], [], []
                    for c in range(2):
                        th = SC.f32(TT)
                        pw = PS.get(4)
                        mm(pw, smallw[0:64, 512 + c * 128:512 + (c + 1) * 128], tw[0:64])
                        logw = SC.f32(TT)
                        act(th, pw, AF.Tanh, scale=0.5, bias=DVc("hw0", c, c + 1))
                        ts(logw, th, -0.3032653298563167, MUL, -0.3032653298563167, ADD)
                        pa = PS.get(4)
                        mm(pa, smallw[64:128, 768 + c * 128:768 + (c + 1) * 128], al[64:128])
                        a_ = SC.f32(TT)
                        act(th, pa, AF.Tanh, scale=0.5, bias=DVc("ha0", c, c + 1))
                        ts(a_, th, 0.5, MUL, 0.5, ADD)
                        pg_ = PS.get(4)
                        mm(pg_, smallw[:, 1024 + c * 128:1024 + (c + 1) * 128], sgl)
                        g_ = SC.f32(TT)
                        cp(g_, pg_, eng="act")
                        gT_.append(g_)
                        kq = SC.f32(TT)
                        ts(kq, kf[c], PVc("rw_kk", c, c + 1), MUL)
                        ksq = SC.bf(TT)
                        act(ksq, kq, AF.Square)
                        pk = PS.get(4)
                        mm(pk, bd1_bf, ksq)
                        rsqrt(th, pk, 1.0, eps12)
                        tt(kq, kq, th, MUL)
                        SC.free(ksq)
                        ts(th, a_, PVc("rw_ka", c, c + 1), MUL, DVc("omka", c, c + 1), ADD)
                        kp = SC.f32(TT)
                        tt(kp, kf[c], th, MUL)
                        tt(th, a_, kq, MUL)
                        SC.free(a_, kf[c])
                        rk = SC.bf(TT)
                        stt(rk, rf[c], PVc("rw_rk", c, c + 1), kp, MUL, MUL)
                        pbn = PS.get(4)
                        mm(pbn, bd1_bf, rk)
                        bo = SC.f32(TT)
                        tt(bo, pbn, vf[c], MUL)
                        bon.append(bo)
                        SC.free(rk)
                        cum = SC.f32(TT)
                        scan(cum, C("reset"), logw, 0.0)
                        wt = SC.f32(TT)
                        act(wt, cum, AF.Exp)
                        wcol = SC.f32(4)
                        cp(wcol, r3(wt, 4)[:, :, 127])
                        Wt.append(wcol)
                        wi = SC.f32(TT)
                        act(wi, cum, AF.Exp, scale=-1.0)
                        tt(cum, cum, logw, SUB)
                        act(cum, cum, AF.Exp)
                        SC.free(logw)
                        art = SC.bf(2 * TT)
                        art4 = r4(art, 4, 2)
                        ARb.append(art)
                        tt(art4[:, :, 0, :], r3(kq, 4), r3(cum, 4), MUL)
                        tt(art4[:, :, 1, :], r3(rf[c], 4), r3(wt, 4), MUL)
                        ARt.append(art4)
                        kt_ = SC.bf(TT)
                        tt(kt_, kp, wi, MUL)
                        bt_ = SC.bf(TT)
                        tt(bt_, th, wi, MUL)
                        Ktl.append(kt_)
                        Btl.append(bt_)
                        v_ = SC.bf(TT)
                        cp(v_, vf[c], eng="act")
                        vb.append(v_)
                        SC.free(th, kq, kp, cum, wi, rf[c], vf[c], wt)
                    SC.free(tw, al, sgl)
                    Vt = SC.bf(4 * 256)
                    Vt3 = r3(Vt, 4)
                    Ktok = SC.bf(4 * 256)
                    Ktok3 = r3(Ktok, 4)
                    Btok = SC.bf(4 * 256)
                    Btok3 = r3(Btok, 4)
                    for tc in range(4):
                        tcs = slice(tc * 128, (tc + 1) * 128)
                        for src, dst, pad in ((vb, Vt3, True), (Ktl, Ktok3, False), (Btl, Btok3, False)):
                            pt = PS.get(1, BF16)
                            for c in range(2):
                                tpose(pt[:, c * 128:(c + 1) * 128], src[c][:, tcs], ident_bf)
                            cp(dst[:, tc, :], pt, eng="act")
                            if pad:
                                pt4 = r4(dst[:, tc, :], 2, 2)
                                for hh in range(2):
                                    ts(VpadV[:, tc, :, hh, hh * 64:(hh + 1) * 64], pt4[:, :, hh, :], 1.0, MUL)
                    SC.free(*vb)
                    uset = 0
                    Ckeep = []
                    for tc in range(4):
                        tcs = slice(tc * 128, (tc + 1) * 128)
                        Mxs = []
                        L4 = SC.bf(TT)
                        P4 = SC.bf(TT)
                        for h in range(4):
                            c, hh = h // 2, h % 2
                            r0 = hh * 64
                            arh = ARt[c][r0:r0 + 64, tc].v(lambda a: a.rearrange("p a b -> p (a b)"))
                            pM = PS.get(4)
                            mm(pM[:, 0:256], Btl[c][r0:r0 + 64, tcs], arh)
                            mm(pM[:, 256:512], Ktl[c][r0:r0 + 64, tcs], arh)
                            pL = PS.get(1)
                            mm(pL, ARt[c][r0:r0 + 64, tc, 0, :], Btl[c][r0:r0 + 64, tcs])
                            Mx = SC.bf(TT)
                            tt(Mx, pM, C("maskM"), MUL)
                            tt(L4[:, h * 128:(h + 1) * 128], pL, C("mlow"), MUL)
                            tt(P4[:, h * 128:(h + 1) * 128], ident_bf, Mx[:, 0:128], SUB)
                            Mxs.append(Mx)
                        Ckeep.append([Mxs, P4, L4, None])
                    SC.free(*Ktl, *Btl)
                    for lev in range(1, 7):
                        for tc in range(4):
                            Mxs, P4, L4, A4 = Ckeep[tc]

                            def Aof(h, Mxs=Mxs, A4=A4):
                                return Mxs[h][:, 0:128] if A4 is None else A4[:, h * 128:(h + 1) * 128]
                            bankL = PS.get(4)
                            for h in range(4):
                                mm(bankL[:, h * 128:(h + 1) * 128], Aof(h), L4[:, h * 128:(h + 1) * 128])
                            L4n = SC.bf(TT)
                            cp(L4n, bankL, eng="act")
                            A4n = None
                            if lev < 6:
                                bankA = PS.get(4)
                                for h in range(4):
                                    mm(bankA[:, h * 128:(h + 1) * 128], L4[:, h * 128:(h + 1) * 128], Aof(h))
                                A4n = SC.bf(TT)
                                cp(A4n, bankA)
                            bankP = PS.get(4)
                            for h in range(4):
                                mm(bankP[:, h * 128:(h + 1) * 128], L4n[:, h * 128:(h + 1) * 128],
                                   P4[:, h * 128:(h + 1) * 128])
                            P4n = SC.bf(TT)
                            tt(P4n, bankP, P4, ADD)
                            SC.free(P4, L4)
                            if A4 is not None:
                                SC.free(A4)
                            Ckeep[tc] = [Mxs, P4n, L4n, A4n]
                    for tc in range(4):
                        SC.free(Ckeep[tc][2])
                        Ckeep[tc] = (Ckeep[tc][0], Ckeep[tc][1])
                    for tc in range(4):
                        tcs = slice(tc * 128, (tc + 1) * 128)
                        Mxs, P4 = Ckeep[tc]
                        for c in range(2):
                            up = Upad[uset % 2]
                            uset += 1
                            pXs, Xbs = [], []
                            for hh in range(2):
                                h = 2 * c + hh
                                r0 = hh * 64
                                Mx = Mxs[h]
                                pX = PS.get(1)[:, 0:64]
                                mm(pX, ARt[c][:, tc, 0, :], SC_bf[c][:, r0:r0 + 64], start=True, stop=False)
                                mm(pX, Mx[:, 256:384], Vt3[:, tc, h * 64:(h + 1) * 64], start=False, stop=True)
                                pXs.append(pX)
                            for hh in range(2):
                                Xb = SC.bf(64)
                                if hh == 0:
                                    cp(Xb, pXs[hh], eng="act")
                                else:
                                    cp(Xb, pXs[hh])
                                Xbs.append(Xb)
                            pUs = []
                            for hh in range(2):
                                h = 2 * c + hh
                                pU = PS.get(1)[:, 0:64]
                                mm(pU, P4[:, h * 128:(h + 1) * 128], Xbs[hh])
                                pUs.append(pU)
                            for hh in range(2):
                                r0 = hh * 64
                                if hh == 0:
                                    act(up[hh][:, r0:r0 + 64], pUs[hh], AF.Copy, scale=-1.0)
                                else:
                                    ts(up[hh][:, r0:r0 + 64], pUs[hh], -1.0, MUL)
                            SC.free(*Xbs)
                            ycol = yb[c][:, tcs]
                            mm(ycol, SC_bf[c], ARt[c][:, tc, 1, :], start=True, stop=False)
                            for hh in range(2):
                                h = 2 * c + hh
                                mm(ycol, Vpad[:, tc, h, :], Mxs[h][:, 384:512], start=False, stop=False)
                                mm(ycol, up[hh], Mxs[h][:, 128:256], start=False, stop=(hh == 1))
                            pS = PS.get(1)
                            kc_ = Ktok3[:, tc, c * 128:(c + 1) * 128]
                            bc_ = Btok3[:, tc, c * 128:(c + 1) * 128]
                            mm(pS, kc_, Vpad[:, tc, 2 * c, :], start=True, stop=False)
                            mm(pS, kc_, Vpad[:, tc, 2 * c + 1, :], start=False, stop=False)
                            mm(pS, bc_, up[0], start=False, stop=False)
                            mm(pS, bc_, up[1], start=False, stop=True)
                            wc = Wt[c][:, tc:tc + 1]
                            tmp = SC.f32(128)
                            stt(tmp, pS, wc, C("bd"), MUL, MUL)
                            stt(SC_f[c], SC_f[c], wc, tmp, MUL, ADD)
                            cp(SC_bf[c], SC_f[c], eng="act")
                            SC.free(tmp)
                        SC.free(P4, *Mxs)
                    for c in range(2):
                        yc = group_norm(yb[c], eps64, PVc("rw_gw", c, c + 1), PVc("rw_gb", c, c + 1))
                        tt(yc, yc, bon[c], ADD)
                        tt(yT[:, 4 + c, :], yc, gT_[c], MUL)
                        SC.free(yc)
                    for lst in (gT_, bon, Wt):
                        SC.free(*lst)
                    SC.free(Vt, Ktok, Btok)
                    SC.free(*ARb)

                slot5 = next_slot()
                slot6 = next_slot()
                slot7 = next_slot()
                if not EN("D"):
                    release()
                    release()
                    release()
                if EN("D"):
                    sz = []
                    for c in range(2):
                        bk = proj(slot5, c * 128)
                        tg_ = SC.f32(TT)
                        act(tg_, bk, AF.Tanh, scale=0.5)
                        s = SC.f32(TT)
                        stt(s, tg_, 1.0, bk, ADD, MUL)
                        SC.free(tg_)
                        sz.append(s)

                    def convD(idx, bk):
                        cv = conv_stage(bk, histD[idx], idx, lambda k: DVc("sdcwh", k * 6 + idx, k * 6 + idx + 1),
                                        DVc("sdcbh", idx, idx + 1))
                        th = SC.f32(TT)
                        act(th, cv, AF.Tanh)
                        return cv, th

                    xsf, xsb, BT_, CT_ = [], [], [], []
                    for c in range(2):
                        cv, th = convD(c, proj(slot5, 256 + c * 128))
                        stt(cv, th, 1.0, cv, ADD, MUL)
                        b_ = SC.bf(TT)
                        cp(b_, cv, eng="act")
                        xsf.append(cv)
                        xsb.append(b_)
                        SC.free(th)
                    release()
                    for g in range(2):
                        cv, th = convD(2 + g, proj(slot6, g * 128))
                        b_ = SC.bf(TT)
                        stt(b_, th, 1.0, cv, ADD, MUL)
                        BT_.append(b_)
                        SC.free(cv, th)
                    for g in range(2):
                        cv, th = convD(4 + g, proj(slot6, 256 + g * 128))
                        b_ = SC.bf(TT)
                        stt(b_, th, 1.0, cv, ADD, MUL)
                        CT_.append(b_)
                        SC.free(cv, th)
                    release()
                    pdt = PS.get(1)
                    for tc in range(4):
                        for k in range(8):
                            mm(pdt[:, tc * 4:(tc + 1) * 4], hT[:, k, tc * 128:(tc + 1) * 128], slot7[:, k, 0:4],
                               start=(k == 0), stop=(k == 7))
                    release()
                    dt = SC.f32(16)
                    tt(r3(dt, 4), r3(pdt[:, 0:16], 4), bc_mid(PVc("sd_dtb"), 4), ADD)
                    act(dt, dt, AF.Exp)
                    act(dt, dt, AF.Ln, scale=1.0, bias=one_c)
                    la = SC.f32(16)
                    tt(r3(la, 4), r3(dt, 4), bc_mid(DVc("aneg"), 4), MUL)
                    pc = PS.get(1)
                    mm(pc[:, 0:16], C("tri"), la)
                    cum = SC.f32(16)
                    cp(cum, pc[:, 0:16], eng="act")
                    Btok = SC.bf(4 * 256)
                    Btok3 = r3(Btok, 4)
                    Dkeep = []
                    for tc in range(4):
                        tcs = slice(tc * 128, (tc + 1) * 128)
                        h4 = slice(tc * 4, (tc + 1) * 4)
                        px = PS.get(1, BF16)
                        for c in range(2):
                            tpose(px[:, c * 128:(c + 1) * 128], xsb[c][:, tcs], ident_bf)
                        pb = PS.get(1, BF16)
                        for g in range(2):
                            tpose(pb[:, g * 128:(g + 1) * 128], BT_[g][:, tcs], ident_bf)
                        cp(Btok3[:, tc, :], pb, eng="act")
                        lat = SC.f32(TT)
                        tt(r3(lat, 4), tri_b4, bc_last(la[:, h4], 128), MUL)
                        pcb = PS.get(4)
                        mm(pcb, ones_f, lat)
                        dif = SC.f32(TT)
                        tt(r3(dif, 4), r3(pcb, 4), bc_last(cum[:, h4], 128), SUB)
                        tt(r3(dif, 4), r3(dif, 4), mbias_b4, ADD)
                        act(dif, dif, AF.Exp)
                        E4 = SC.f32(TT)
                        act(E4, pcb, AF.Exp)
                        E43 = r3(E4, 4)
                        we = SC.f32(4)
                        tt(we, r3(pcb, 4)[:, :, 127], cum[:, h4], SUB)
                        act(we, we, AF.Exp)
                        tt(we, we, dt[:, h4], MUL)
                        px4 = r4(px, 2, 2)
                        dt22 = r3(dt[:, h4], 2)
                        for hh in range(2):
                            tt(VpadV[:, tc, :, hh, hh * 64:(hh + 1) * 64], px4[:, :, hh, :],
                               bc_last(dt22[:, :, hh], 64), MUL)
                        vw = SC.bf(256)
                        tt(r3(vw, 4), r3(px, 4), bc_last(we, 64), MUL)
                        psc = PS.get(2)
                        for g in range(2):
                            mm(psc[:, g * 128:(g + 1) * 128], BT_[g][:, tcs], CT_[g][:, tcs])
                        sm = SC.bf(TT)
                        dif4 = r4(dif, 2, 2)
                        sm4 = r4(sm, 2, 2)
                        Cs = SC.bf(TT)
                        Cs4 = r4(Cs, 2, 2)
                        E44 = r4(E4, 2, 2)
                        for g in range(2):
                            tt(sm4[:, g], bc_mid(psc[:, g * 128:(g + 1) * 128], 2), dif4[:, g], MUL)
                            tt(Cs4[:, g], bc_mid(CT_[g][:, tcs], 2), E44[:, g], MUL)
                        etot = SC.f32(4)
                        cp(etot, E43[:, :, 127], eng="act")
                        Dkeep.append((sm, Cs, vw, etot))
                        SC.free(lat, dif, E4, we)
                    for tc in range(4):
                        tcs = slice(tc * 128, (tc + 1) * 128)
                        sm, Cs, vw, etot = Dkeep[tc]
                        for g in range(2):
                            ycol = yb[g][:, tcs]
                            for hh in range(2):
                                h = 2 * g + hh
                                mm(ycol, SpadD[g][hh], Cs[:, h * 128:(h + 1) * 128], start=(hh == 0), stop=False)
                                mm(ycol, Vpad[:, tc, h, :], sm[:, h * 128:(h + 1) * 128], start=False, stop=(hh == 1))
                            pS = PS.get(1)
                            mm(pS, Btok3[:, tc, g * 128:(g + 1) * 128], vw[:, g * 128:(g + 1) * 128])
                            sd3 = r3(SD_f[g], 2)
                            tt(sd3, sd3, bc_last(etot[:, 2 * g:2 * g + 2], 64), MUL)
                            tt(SD_f[g], SD_f[g], pS, ADD)
                            for hh in range(2):
                                cp(SpadD[g][hh][:, hh * 64:(hh + 1) * 64], SD_f[g][:, hh * 64:(hh + 1) * 64], eng="act")
                        SC.free(vw, sm, Cs, etot)
                    for g in range(2):
                        y1 = SC.f32(TT)
                        stt(y1, xsf[g], PVc("sd_d", g, g + 1), yb[g], MUL, ADD)
                        stt(y1, y1, 0.5, sz[g], MUL, MUL)
                        ysq = SC.bf(TT)
                        act(ysq, y1, AF.Square)
                        pm = PS.get(4)
                        mm(pm, ones128_bf, ysq)
                        r_ = SC.f32(TT)
                        rsqrt(r_, pm, 1.0, eps6)
                        stt(yT[:, 6 + g, :], y1, PVc("sd_nw", g, g + 1), r_, MUL, MUL)
                        SC.free(y1, ysq, r_)
                    SC.free(dt, la, cum, Btok, *sz, *xsf, *xsb, *BT_, *CT_)

                so = [next_slot(), next_slot()]
                for d in range(8):
                    bk = PS.get(4)
                    w = so[d // 4]
                    for k in range(8):
                        mm(bk, w[:, k, (d % 4) * 128:(d % 4 + 1) * 128], yT[:, k, :], start=(k == 0), stop=(k == 7))
                    tt(xT[:, d, t0:t0 + TT], xT[:, d, t0:t0 + TT], bk, ADD)
                    if d % 4 == 3:
                        release()
            dbg("yT", yT)
            PHASE1["scratch_peak"] = max(PHASE1.get("scratch_peak", 0), SC.peak)
            PHASE1["scratch_n"] = SC.n
            Pg.barrier()
            AR.reset(m)


        for sq in range(NSEQ):
            phase0(sq)
            for l in range(L):
                layer_setup(l)
                if EN("mix"):
                    phase_mix(l)
                if EN("attn"):
                    phase_attn(l)
                if EN("ffn"):
                    phase_ffn(l)
            phase_final(sq)
        Pg.wait_all_on("sp")
        build.info = dict(n_ops=Pg.n_ops, arena_peak=AR.peak, arena_n=AR.n, **PHASE1)

        with nc.Block() as block:
            @block.tensor
            def _(e):
                Pg.replay("pe", e)

            @block.scalar
            def _(e):
                Pg.replay("act", e)

            @block.vector
            def _(e):
                Pg.replay("dve", e)

            @block.gpsimd
            def _(e):
                Pg.replay("pool", e)

            @block.sync
            def _(e):
                Pg.replay("sp", e)
    return nc


def make_in_maps(inputs, L, S, nseq_per_core, n_cores):
    f = lambda a: np.ascontiguousarray(np.asarray(a, dtype=np.float32))
    shared = {
        "cb": make_consts(f(inputs["norm_final"])),
        "rope": make_rope(S),
        "pv": make_pvec(inputs, L),
        "smallw": make_smallw(inputs, L),
        "w_in": f(inputs["w_in"]),
        "w_out": f(inputs["w_out"]),
        "mem_wq": f(inputs["mem_wq"]),
        "mem_wk": f(inputs["mem_wk"]),
        "mem_wv": f(inputs["mem_wv"]),
        "mem_wo": f(inputs["mem_wo"]),
        "ffn_w_in": f(inputs["ffn_w_in"]),
        "ffn_w_out": f(inputs["ffn_w_out"]),
    }
    x = f(inputs["x"])
    mem = f(inputs["mem"])
    maps = []
    for c in range(n_cores):
        b0 = c * nseq_per_core
        m = dict(shared)
        m["x"] = np.ascontiguousarray(x[b0:b0 + nseq_per_core].reshape(nseq_per_core * S, D))
        m["mem"] = np.ascontiguousarray(mem[b0:b0 + nseq_per_core].reshape(nseq_per_core * MEMT, D))
        maps.append(m)
    return maps


def kernel(**inputs):
    x = np.asarray(inputs["x"])
    B, S, _ = x.shape
    L = np.asarray(inputs["w_in"]).shape[0]
    n_cores = N_CORES if B % N_CORES == 0 else 1
    nseq = B // n_cores
    nc = build(L, S, nseq)
    maps = make_in_maps(inputs, L, S, nseq, n_cores)
    res = run_bass_kernel_spmd(nc, maps, core_ids=list(range(n_cores)))
    outs = [np.asarray(r["out"]).reshape(nseq, S, D) for r in res.results]
    return np.concatenate(outs, axis=0).astype(np.float32)
```
